# Optimizing a Trainium2 kernel written in Bass

```python
import math
import jax, jax.numpy as jnp
from jax import lax
import numpy as np

D_MODEL = 1024
BATCH = 8
SEQ = 2048
DEPTH = 1

GRID_W = 64
NA_HEADS = 8
NA_HEAD_DIM = 64
NA_WIN_ROWS = 8
NA_WIN_COLS = 16
NA_WIDTH = NA_HEADS * NA_HEAD_DIM
DN_HEADS = 8
DN_HEAD_DIM = 64
DN_WIDTH = DN_HEADS * DN_HEAD_DIM
DN_CONV = 5
DN_CHUNK = 64
N_GROUPS = 4
EXPERTS_PER_GROUP = 8
N_EXPERTS = N_GROUPS * EXPERTS_PER_GROUP
TOP_K = 2
D_EXPERT = 256
DEEPNORM_ALPHA = (2.0 * DEPTH) ** 0.25
DEEPNORM_BETA = (8.0 * DEPTH) ** -0.25
LN_EPS = 1e-5
RMS_EPS = 1e-6

IN_SPLITS = (NA_WIDTH, NA_WIDTH, NA_WIDTH, DN_WIDTH, DN_WIDTH, DN_WIDTH, DN_WIDTH,
             DN_HEADS, DN_HEADS, DN_HEADS, DN_HEADS, D_MODEL, D_MODEL)
D_IN = sum(IN_SPLITS)
VALUE_SLOTS = (2, 5)

kernel_name = "hybrid_natten_gdn_hiermoe_deepnorm"


def layer_norm(x, g, b):
    xf = x.astype(jnp.float32)
    mu = jnp.mean(xf, -1, keepdims=True)
    var = jnp.mean(jnp.square(xf - mu), -1, keepdims=True)
    return ((xf - mu) * lax.rsqrt(var + LN_EPS) * g + b).astype(x.dtype)


def l2norm(a):
    return a * lax.rsqrt(jnp.sum(a * a, -1, keepdims=True) + RMS_EPS)


def neighborhood_attention(q, k, v, rpb):
    B, T, _ = q.shape
    rows = T // GRID_W
    kr = min(NA_WIN_ROWS, rows)
    to_grid = lambda a: a.reshape(B, rows, GRID_W, NA_HEADS, NA_HEAD_DIM).transpose(0, 3, 1, 2, 4)
    qg, kg, vg = to_grid(q), to_grid(k), to_grid(v)
    r = np.arange(rows)
    row_start = np.clip(r - kr // 2, 0, rows - kr)
    row_idx = row_start[:, None] + np.arange(kr)[None, :]
    dr_idx = row_idx - r[:, None] + (NA_WIN_ROWS - 1)
    c = np.arange(GRID_W)
    col_start = np.clip(c - NA_WIN_COLS // 2, 0, GRID_W - NA_WIN_COLS)
    col_mask = (c[None, :] >= col_start[:, None]) & (c[None, :] < col_start[:, None] + NA_WIN_COLS)
    dc_idx = np.clip(c[None, :] - c[:, None], -(NA_WIN_COLS - 1), NA_WIN_COLS - 1) + (NA_WIN_COLS - 1)
    k_blk = kg[:, :, row_idx]
    v_blk = vg[:, :, row_idx]
    scale = NA_HEAD_DIM ** -0.5
    s = jnp.einsum('bhrcd,bhrjkd->bhrcjk', qg * scale, k_blk).astype(jnp.float32)
    bias = rpb[:, dr_idx[:, None, :, None], dc_idx[None, :, None, :]]
    s = s + bias[None].astype(jnp.float32)
    s = jnp.where(col_mask[:, None, :], s, -1e30)
    p = jax.nn.softmax(s.reshape(B, NA_HEADS, rows, GRID_W, kr * GRID_W), axis=-1)
    p = p.reshape(s.shape).astype(v.dtype)
    o = jnp.einsum('bhrcjk,bhrjkd->bhrcd', p, v_blk)
    return o.transpose(0, 2, 3, 1, 4).reshape(B, T, NA_WIDTH)


def short_conv(x, w):
    y = lax.conv_general_dilated(x, w[:, None, :].astype(x.dtype), window_strides=(1,),
                                 padding=[(DN_CONV // 2, DN_CONV // 2)],
                                 dimension_numbers=('NWC', 'WIO', 'NWC'),
                                 feature_group_count=x.shape[-1])
    return jax.nn.silu(y)


def chunk_gated_delta(q, k, v, beta, g):
    B, H, T, dk = q.shape
    dv = v.shape[-1]
    c = DN_CHUNK
    n = T // c
    q = q * dk ** -0.5
    blk = lambda a: a.reshape(B, H, n, c, *a.shape[3:])
    q, k, v, beta, g = blk(q), blk(k), blk(v), blk(beta), blk(g)
    gc = jnp.cumsum(g, axis=-1)
    incl = np.tril(np.ones((c, c), bool))
    strict = np.tril(np.ones((c, c), bool), -1)
    decay = jnp.exp(jnp.where(incl, gc[..., :, None] - gc[..., None, :], -jnp.inf))
    kb = k * beta[..., None]
    kk = jnp.einsum('bhnid,bhnjd->bhnij', kb, k) * decay
    a_mat = jnp.where(strict, kk, 0.0) + np.eye(c, dtype=np.float32)
    rhs = jnp.concatenate([v * beta[..., None], kb * jnp.exp(gc)[..., None]], axis=-1)
    sol = lax.linalg.triangular_solve(a_mat, rhs, left_side=True, lower=True, unit_diagonal=True)
    u, w = sol[..., :dv], sol[..., dv:]
    intra = jnp.einsum('bhnid,bhnjd->bhnij', q, k) * decay
    q_dec = q * jnp.exp(gc)[..., None]
    g_last = gc[..., -1]
    k_dec = k * jnp.exp(g_last[..., None] - gc)[..., None]

    def step(S, xs):
        qe, ke, u_c, w_c, a_c, gl = xs
        v_new = u_c - jnp.einsum('bhik,bhkv->bhiv', w_c, S)
        o = jnp.einsum('bhik,bhkv->bhiv', qe, S) + jnp.einsum('bhij,bhjv->bhiv', a_c, v_new)
        S = S * jnp.exp(gl)[..., None, None] + jnp.einsum('bhik,bhiv->bhkv', ke, v_new)
        return S, o

    xs = tuple(jnp.moveaxis(a, 2, 0) for a in (q_dec, k_dec, u, w, intra, g_last))
    S0 = jnp.zeros((B, H, dk, dv), jnp.float32)
    _, o = lax.scan(step, S0, xs)
    return jnp.moveaxis(o, 0, 2).reshape(B, H, T, dv)


def gated_deltanet_bidir(q, k, v, z, beta_f, beta_b, a_f, a_b, conv_w,
                         a_log_f, a_log_b, dt_bias_f, dt_bias_b, norm_w):
    B, T, _ = q.shape
    qkv = short_conv(jnp.concatenate([q, k, v], axis=-1), conv_w)
    q, k, v = jnp.split(qkv, 3, axis=-1)
    heads = lambda a: a.reshape(B, T, DN_HEADS, DN_HEAD_DIM).transpose(0, 2, 1, 3).astype(jnp.float32)
    q, k, v, zh = heads(q), heads(k), heads(v), heads(z)
    q, k = l2norm(q), l2norm(k)

    def gates(beta_raw, a_raw, a_log, dt_bias):
        bt = jax.nn.sigmoid(beta_raw.astype(jnp.float32))
        gl = -jnp.exp(a_log.astype(jnp.float32)) * jax.nn.softplus(
            a_raw.astype(jnp.float32) + dt_bias.astype(jnp.float32))
        return bt.transpose(0, 2, 1), gl.transpose(0, 2, 1)

    bf, gf = gates(beta_f, a_f, a_log_f, dt_bias_f)
    bb, gb = gates(beta_b, a_b, a_log_b, dt_bias_b)
    flip = lambda a: jnp.flip(a, axis=2)
    o_fwd = chunk_gated_delta(q, k, v, bf, gf)
    o_bwd = flip(chunk_gated_delta(flip(q), flip(k), flip(v), flip(bb), flip(gb)))
    o = o_fwd + o_bwd
    o = o * lax.rsqrt(jnp.mean(o * o, -1, keepdims=True) + RMS_EPS) * norm_w.astype(jnp.float32)
    o = o * jax.nn.silu(zh)
    return o.transpose(0, 2, 1, 3).reshape(B, T, DN_WIDTH).astype(z.dtype)


def token_mixer(x, w_in, na_rpb, dn_conv_w, dn_a_log_f, dn_a_log_b, dn_dt_bias_f, dn_dt_bias_b,
                dn_norm_w, w_proj_na, w_proj_dn, w_out):
    h = jnp.einsum('btd,de->bte', x, w_in)
    offs = np.cumsum(IN_SPLITS)[:-1].tolist()
    (q_na, k_na, v_na, q_dn, k_dn, v_dn, z_dn, b_f, b_b, a_f, a_b,
     gate_na, gate_dn) = jnp.split(h, offs, axis=-1)
    y_na = neighborhood_attention(q_na, k_na, v_na, na_rpb) @ w_proj_na
    y_dn = gated_deltanet_bidir(q_dn, k_dn, v_dn, z_dn, b_f, b_b, a_f, a_b, dn_conv_w,
                                dn_a_log_f, dn_a_log_b, dn_dt_bias_f, dn_dt_bias_b, dn_norm_w) @ w_proj_dn
    merged = jax.nn.sigmoid(gate_na) * y_na + jax.nn.sigmoid(gate_dn) * y_dn
    return merged @ w_out


def hierarchical_moe(x, w_router_group, b_router_group, w_router_expert, b_router_expert,
                     w_expert_gate_up, w_expert_down):
    B, T, D = x.shape
    N = B * T
    xt = x.reshape(N, D)
    group_logits = (xt @ w_router_group + b_router_group).astype(jnp.float32)
    group_p, group_idx = lax.top_k(jax.nn.softmax(group_logits, axis=-1), 1)
    expert_logits = (xt @ w_router_expert + b_router_expert).astype(jnp.float32)
    expert_logits = expert_logits.reshape(N, N_GROUPS, EXPERTS_PER_GROUP)
    sel = jnp.take_along_axis(expert_logits, group_idx[:, :, None], axis=1)[:, 0]
    top_l, top_i = lax.top_k(sel, TOP_K)
    top_p = jax.nn.softmax(top_l, axis=-1) * group_p
    expert_id = group_idx * EXPERTS_PER_GROUP + top_i
    combine = jnp.sum(jax.nn.one_hot(expert_id, N_EXPERTS, dtype=jnp.float32) * top_p[..., None], axis=1)
    hgu = jnp.einsum('nd,edf->nef', xt, w_expert_gate_up)
    hg, hu = jnp.split(hgu, 2, axis=-1)
    hid = jax.nn.silu(hg) * hu * combine[:, :, None].astype(x.dtype)
    y = jnp.einsum('nef,efd->nd', hid, w_expert_down)
    return y.reshape(B, T, D)


def setup_inputs(seed: int = 0) -> dict:
    key = jax.random.key(seed)
    ks = jax.random.split(key, 24)
    nrm = lambda k, shape, s: jax.random.normal(k, shape, jnp.float32) * s
    col_scale = jnp.concatenate([
        jnp.full((w,), DEEPNORM_BETA if i in VALUE_SLOTS else 1.0, jnp.float32)
        for i, w in enumerate(IN_SPLITS)])
    x = nrm(ks[0], (BATCH, SEQ, D_MODEL), 1.0)
    w_in = nrm(ks[1], (DEPTH, D_MODEL, D_IN), D_MODEL ** -0.5) * col_scale
    na_rpb = nrm(ks[2], (DEPTH, NA_HEADS, 2 * NA_WIN_ROWS - 1, 2 * NA_WIN_COLS - 1), 0.5)
    dn_conv_w = nrm(ks[3], (DEPTH, DN_CONV, 3 * DN_WIDTH), DN_CONV ** -0.5)
    dn_a_log_f = jnp.log(jax.random.uniform(ks[4], (DEPTH, DN_HEADS), jnp.float32, 1.0, 16.0))
    dn_a_log_b = jnp.log(jax.random.uniform(ks[5], (DEPTH, DN_HEADS), jnp.float32, 1.0, 16.0))

    def dt_bias(k):
        u = jax.random.uniform(k, (DEPTH, DN_HEADS), jnp.float32)
        dt = jnp.exp(u * (math.log(0.1) - math.log(0.001)) + math.log(0.001))
        return dt + jnp.log(-jnp.expm1(-dt))

    dn_dt_bias_f = dt_bias(ks[6])
    dn_dt_bias_b = dt_bias(ks[7])
    dn_norm_w = 1.0 + nrm(ks[8], (DEPTH, DN_HEAD_DIM), 0.02)
    w_proj_na = nrm(ks[9], (DEPTH, NA_WIDTH, D_MODEL), NA_WIDTH ** -0.5 * DEEPNORM_BETA)
    w_proj_dn = nrm(ks[10], (DEPTH, DN_WIDTH, D_MODEL), DN_WIDTH ** -0.5 * DEEPNORM_BETA)
    w_out = nrm(ks[11], (DEPTH, D_MODEL, D_MODEL), D_MODEL ** -0.5 * DEEPNORM_BETA)
    ln1_g = 1.0 + nrm(ks[12], (DEPTH, D_MODEL), 0.02)
    ln1_b = nrm(ks[13], (DEPTH, D_MODEL), 0.02)
    w_router_group = nrm(ks[14], (DEPTH, D_MODEL, N_GROUPS), D_MODEL ** -0.5)
    b_router_group = nrm(ks[15], (DEPTH, N_GROUPS), 0.01)
    w_router_expert = nrm(ks[16], (DEPTH, D_MODEL, N_EXPERTS), D_MODEL ** -0.5)
    b_router_expert = nrm(ks[17], (DEPTH, N_EXPERTS), 0.01)
    w_expert_gate_up = nrm(ks[18], (DEPTH, N_EXPERTS, D_MODEL, 2 * D_EXPERT), D_MODEL ** -0.5)
    w_expert_down = nrm(ks[19], (DEPTH, N_EXPERTS, D_EXPERT, D_MODEL), D_EXPERT ** -0.5 * DEEPNORM_BETA)
    ln2_g = 1.0 + nrm(ks[20], (DEPTH, D_MODEL), 0.02)
    ln2_b = nrm(ks[21], (DEPTH, D_MODEL), 0.02)
    return {"x": x, "w_in": w_in, "na_rpb": na_rpb, "dn_conv_w": dn_conv_w,
            "dn_a_log_f": dn_a_log_f, "dn_a_log_b": dn_a_log_b,
            "dn_dt_bias_f": dn_dt_bias_f, "dn_dt_bias_b": dn_dt_bias_b, "dn_norm_w": dn_norm_w,
            "w_proj_na": w_proj_na, "w_proj_dn": w_proj_dn, "w_out": w_out,
            "ln1_g": ln1_g, "ln1_b": ln1_b,
            "w_router_group": w_router_group, "b_router_group": b_router_group,
            "w_router_expert": w_router_expert, "b_router_expert": b_router_expert,
            "w_expert_gate_up": w_expert_gate_up, "w_expert_down": w_expert_down,
            "ln2_g": ln2_g, "ln2_b": ln2_b}


def reference(x, w_in, na_rpb, dn_conv_w, dn_a_log_f, dn_a_log_b, dn_dt_bias_f, dn_dt_bias_b,
              dn_norm_w, w_proj_na, w_proj_dn, w_out, ln1_g, ln1_b,
              w_router_group, b_router_group, w_router_expert, b_router_expert,
              w_expert_gate_up, w_expert_down, ln2_g, ln2_b):
    for l in range(DEPTH):
        mix = token_mixer(x, w_in[l], na_rpb[l], dn_conv_w[l], dn_a_log_f[l], dn_a_log_b[l],
                          dn_dt_bias_f[l], dn_dt_bias_b[l], dn_norm_w[l],
                          w_proj_na[l], w_proj_dn[l], w_out[l])
        x = layer_norm(DEEPNORM_ALPHA * x + mix, ln1_g[l], ln1_b[l])
        ffn = hierarchical_moe(x, w_router_group[l], b_router_group[l], w_router_expert[l],
                               b_router_expert[l], w_expert_gate_up[l], w_expert_down[l])
        x = layer_norm(DEEPNORM_ALPHA * x + ffn, ln2_g[l], ln2_b[l])
    return x
```

```python
from contextlib import ExitStack
import numpy as np
import concourse.bass as bass
import concourse.mybir as mybir
from concourse.bass_utils import run_bass_kernel_spmd

F32 = mybir.dt.float32
BF16 = mybir.dt.bfloat16
AF = mybir.ActivationFunctionType
ALU = mybir.AluOpType
AX = mybir.AxisListType

ENGS = ("tensor", "vector", "scalar", "gpsimd", "sync")
SAME_ENGINE_SYNC = True
NEG = -30000.0
T = 2048
DM = 1024
ARENA_KB = 207


class Buf:
    __slots__ = ("name", "w", "r", "dsem", "dcnt", "excl")

    def __init__(self, name, excl=False):
        self.name = name
        self.excl = excl
        self.w = None
        self.r = []
        self.dsem = None
        self.dcnt = 0


class Op:
    __slots__ = ("eng", "idx", "fn", "waits", "signal", "sigval", "dma", "dsem", "dval")

    def __init__(self, eng, idx, fn):
        self.eng = eng
        self.idx = idx
        self.fn = fn
        self.waits = []
        self.signal = False
        self.sigval = None
        self.dma = False
        self.dsem = None
        self.dval = 0


class Sched:
    def __init__(self, nc, stack):
        self.nc = nc
        self.stack = stack
        self.ops = {e: [] for e in ENGS}
        self.seen = {e: {p: -1 for p in ENGS} for e in ENGS}
        self.seen_dma = {e: {} for e in ENGS}
        self.nsem = 0

    def new_sem(self, name):
        self.nsem += 1
        return self.stack.enter_context(self.nc.semaphore(f"{name}_{self.nsem}"))

    def _dep(self, op, dep):
        if dep is None or dep is op:
            return
        if dep.dma:
            key = id(dep.dsem)
            if self.seen_dma[op.eng].get(key, 0) >= dep.dval:
                return
            self.seen_dma[op.eng][key] = dep.dval
            op.waits.append(dep)
            return
        if dep.eng == op.eng:
            if dep.eng == "tensor" or not SAME_ENGINE_SYNC:
                return
        if self.seen[op.eng][dep.eng] >= dep.idx:
            return
        self.seen[op.eng][dep.eng] = dep.idx
        dep.signal = True
        op.waits.append(dep)

    def _deps(self, o, deps):
        best = {}
        for dep in deps:
            if dep is None or dep is o:
                continue
            key = ("d", id(dep.dsem)) if dep.dma else ("e", dep.eng)
            val = dep.dval if dep.dma else dep.idx
            if key not in best or val > best[key][0]:
                best[key] = (val, dep)
        for _, dep in best.values():
            self._dep(o, dep)

    def op(self, eng, fn, reads=(), writes=()):
        lst = self.ops[eng]
        o = Op(eng, len(lst), fn)
        ex = [b for b in reads if b.excl and b not in writes]
        if ex:
            reads = [b for b in reads if not b.excl]
            writes = list(writes) + ex
        deps = [b.w for b in reads]
        for b in writes:
            deps.append(b.w)
            deps.extend(b.r)
        self._deps(o, deps)
        for b in reads:
            b.r.append(o)
        for b in writes:
            b.w = o
            b.r = []
        lst.append(o)
        return o

    def dma(self, eng, fn, reads=(), writes=(), sem_buf=None):
        lst = self.ops[eng]
        o = Op(eng, len(lst), fn)
        o.dma = True
        sb = sem_buf or (writes[0] if writes else reads[0])
        if sb.dsem is None:
            sb.dsem = self.new_sem("d_" + sb.name)
        deps = [b.w for b in reads]
        for b in writes:
            if b.w is not None and not (b.w.dma and b.w.dsem is sb.dsem):
                deps.append(b.w)
            deps.extend(b.r)
        self._deps(o, deps)
        sb.dcnt += 16
        o.dsem = sb.dsem
        o.dval = sb.dcnt
        for b in reads:
            b.r.append(o)
        for b in writes:
            b.w = o
            b.r = []
        lst.append(o)
        return o

    def barrier(self):
        lasts = []
        for e in ENGS:
            for o in reversed(self.ops[e]):
                if not o.dma:
                    lasts.append(o)
                    break
        latest = {}
        for e in ENGS:
            for o in self.ops[e]:
                if o.dma:
                    latest[id(o.dsem)] = o
        for e in ENGS:
            o = Op(e, len(self.ops[e]), None)
            for d in lasts:
                self._dep(o, d)
            for d in latest.values():
                self._dep(o, d)
            self.ops[e].append(o)

    def emit(self):
        nc = self.nc
        CH = 1000
        esems = {}
        for e in ENGS:
            n = sum(1 for o in self.ops[e] if o.signal)
            esems[e] = [self.new_sem(f"e_{e}_{i}") for i in range(n // CH + 1)]
            c = 0
            for o in self.ops[e]:
                if o.signal:
                    o.sigval = c
                    c += 1

        def run(e, h):
            for o in self.ops[e]:
                for d in o.waits:
                    if d.dma:
                        h.wait_ge(d.dsem, d.dval)
                    else:
                        h.wait_ge(esems[d.eng][d.sigval // CH], d.sigval % CH + 1)
                if o.fn is None:
                    if o.signal:
                        h.nop().then_inc(esems[e][o.sigval // CH], 1)
                    continue
                ins = o.fn(h)
                if o.dma:
                    ins.then_inc(o.dsem, 16)
                elif o.signal:
                    ins.then_inc(esems[e][o.sigval // CH], 1)

        with nc.Block() as block:
            @block.tensor
            def _(h):
                run("tensor", h)

            @block.vector
            def _(h):
                run("vector", h)

            @block.scalar
            def _(h):
                run("scalar", h)

            @block.gpsimd
            def _(h):
                run("gpsimd", h)

            @block.sync
            def _(h):
                run("sync", h)


class Ring:
    def __init__(self, name, views):
        self.views = views
        self.bufs = [Buf(f"{name}{i}") for i in range(len(views))]
        self.i = 0

    def next(self):
        k = self.i % len(self.views)
        self.i += 1
        return self.views[k], self.bufs[k]


def _rs(r):
    return min(max(r - 4, 0), 24)


def na_blocks(i):
    res = []
    for j in range(16):
        valid = [[_rs(2 * i + b) <= 2 * j + a < _rs(2 * i + b) + 8 for b in (0, 1)] for a in (0, 1)]
        if not (valid[0][0] or valid[0][1] or valid[1][0] or valid[1][1]):
            continue
        delta = j - i
        if all(valid[0]) and all(valid[1]):
            var = delta + 3
        elif delta == -2 and valid == [[True, False], [True, True]]:
            var = 7
        elif delta == 2 and valid == [[False, True], [False, False]]:
            var = 8
        else:
            raise AssertionError((i, j, valid))
        assert 0 <= var < 9
        res.append((j, var))
    return res


def na_bias_tiles(rpb):
    c = np.arange(64)
    col_start = np.clip(c - 8, 0, 48)
    col_mask = (c[None, :] >= col_start[:, None]) & (c[None, :] < col_start[:, None] + 16)
    dc_idx = np.clip(c[None, :] - c[:, None], -15, 15) + 15
    tiles = np.full((8, 9, 128, 128), NEG, np.float32)
    for v in range(9):
        for a in (0, 1):
            for b in (0, 1):
                if v < 7:
                    delta, ok = v - 3, True
                elif v == 7:
                    delta, ok = -2, not (a == 0 and b == 1)
                else:
                    delta, ok = 2, (a == 0 and b == 1)
                dr = 2 * delta + a - b + 7
                if not ok or not (0 <= dr <= 14):
                    continue
                vals = rpb[:, dr][:, dc_idx]
                vals = np.where(col_mask[None], vals, np.float32(NEG))
                tiles[:, v, a * 64:(a + 1) * 64, b * 64:(b + 1) * 64] = vals.transpose(0, 2, 1)
    return np.ascontiguousarray(tiles.transpose(2, 0, 1, 3)).reshape(128, 72 * 128)


def dn_consts():
    p = np.arange(128)
    same = (p[:, None] // 64) == (p[None, :] // 64)
    A1 = (same & (p[:, None] <= p[None, :])).astype(np.float32)
    A2 = (same & (p[:, None] > p[None, :])).astype(np.float32)
    ones = same.astype(np.float32)
    out = {
        "A1": A1, "A2": A2, "A1T": A1.T.copy(), "A2T": A2.T.copy(),
        "C3f": ones - A1, "C3b": ones - A1.T,
        "HA0": np.repeat((p < 64).astype(np.float32)[:, None], 128, 1),
        "HA1": np.repeat((p >= 64).astype(np.float32)[:, None], 128, 1),
        "NEGSf": np.where(same & (p[None, :] < p[:, None]), 0.0, NEG).astype(np.float32),
        "NEG2f": np.where(same & (p[:, None] <= p[None, :]), 0.0, NEG).astype(np.float32),
        "NEGSb": np.where(same & (p[None, :] > p[:, None]), 0.0, NEG).astype(np.float32),
        "NEG2b": np.where(same & (p[:, None] >= p[None, :]), 0.0, NEG).astype(np.float32),
        "IDENT": np.eye(128, dtype=np.float32),
        "H": ones,
    }
    names = ["A1", "A2", "A1T", "A2T", "C3f", "C3b", "HA0", "HA1", "NEGSf", "NEG2f", "NEGSb", "NEG2b", "IDENT", "H"]
    return names, np.concatenate([out[n] for n in names], axis=1)


CONST_NAMES, CONST_ARR = dn_consts()
NCONST = len(CONST_NAMES)

OFF_QNA, OFF_KNA, OFF_VNA = 0, 512, 1024
OFF_QDN, OFF_KDN, OFF_VDN, OFF_ZDN = 1536, 2048, 2560, 3072
OFF_G = 3584
OFF_GNA, OFF_GDN = 3616, 4640


class Region:
    def __init__(self, ranges):
        self.free = [[int(a * 1024), int(b * 1024)] for a, b in ranges]

    def alloc(self, nbytes):
        nbytes = (nbytes + 63) // 64 * 64
        for r in self.free:
            if r[1] - r[0] >= nbytes:
                off = r[0]
                r[0] += nbytes
                return off
        raise MemoryError(f"arena region exhausted: need {nbytes}, free {self.free}")


import os
LP = int(os.environ.get("LOCAL_PARTS", "9"))
SP = int(os.environ.get("SCAN_PARTS", "9"))
def mm_(h, out, **kw):
    return h.matmul(out, skip_group_check=True, **kw)


ALPHA = 2.0 ** 0.25
LN_EPS = 1e-5
RMS_EPS = 1e-6


def build_program(stage=99, dbg=()):
    nc = bass.Bass("TRN2", target_bir_lowering=False)
    D = {}

    def din(name, shape, dt=F32):
        D[name] = nc.dram_tensor(name, list(shape), dt, kind="ExternalInput").ap()

    def dout(name, shape, dt=F32):
        D[name] = nc.dram_tensor(name, list(shape), dt, kind="ExternalOutput").ap()

    din("x", [T, DM]); din("xT", [DM, T]); din("w_in", [DM, 5664]); din("nab", [128, 72 * 128])
    din("consts", [128, NCONST * 128])
    din("convT", [128, 12 * 5]); din("gvec", [1, 32]); din("norm_w", [1, 64])
    din("w_proj_na", [512, DM]); din("w_proj_dn", [512, DM]); din("w_out", [DM, DM])
    din("ln1", [1, 2 * DM]); din("ln2", [1, 2 * DM])
    din("w_r", [DM, 36]); din("b_r", [1, 36])
    din("w_gu", [32, DM, 512]); din("w_dn", [32, 256, DM])
    dout("out", [T, DM])
    for name, shape, dt in dbg:
        dout(name, shape, dt)

    with ExitStack() as st:
        S = Sched(nc, st)
        arena = st.enter_context(nc.sbuf_tensor("arena", [128, ARENA_KB * 256], F32))
        psum = st.enter_context(nc.psum_tensor("psum", [128, 4096], F32))

        def tile(reg, dims, dt=F32):
            n = 1
            for d_ in dims:
                n *= d_
            nbytes = n * (4 if dt == F32 else 2)
            off = reg.alloc(nbytes)
            v = arena[:, off // 4:(off + nbytes + 3) // 4]
            if dt != F32:
                v = v.bitcast(dt)[:, 0:n]
            if len(dims) > 1:
                names = " ".join(f"d{i}" for i in range(len(dims)))
                kw = {f"d{i}": dims[i] for i in range(1, len(dims))}
                v = v.rearrange(f"p ({names}) -> p {names}", **kw)
            return v

        def bank(b, n=512, off=0):
            return psum[:, b * 512 + off:b * 512 + off + n]

        BANKB = [Buf(f"bank{b}", excl=True) for b in range(8)]

        def bank_ring(name, banks):
            r = Ring(name, [bank(b) for b in banks])
            r.bufs = [BANKB[b] for b in banks]
            return r

        def v3(ap, h):
            return ap.rearrange("p (h j) -> p h j", h=h)

        R0 = Region([(0, 10)])
        RN = Region([(26, 206)])
        xT = tile(RN, [8, T], BF16)
        cst = tile(RN, [NCONST * 128])
        B_cst = Buf("cst")
        S.dma("sync", lambda h: h.dma_start(out=cst, in_=D["consts"]), writes=[B_cst])
        cb = tile(R0, [NCONST * 128], BF16)
        identf = tile(R0, [128])
        B_cb = Buf("cb")
        S.op("vector", lambda h: h.tensor_copy(out=cb, in_=cst), reads=[B_cst], writes=[B_cb])
        i_id = CONST_NAMES.index("IDENT")
        S.op("vector", lambda h: h.tensor_copy(out=identf, in_=cst[:, i_id * 128:(i_id + 1) * 128]), reads=[B_cst], writes=[B_cb])
        CBn = {n: cb[:, i * 128:(i + 1) * 128] for i, n in enumerate(CONST_NAMES)}
        C = CBn
        B_cst = B_cb
        identb = CBn["IDENT"]

        onaT = tile(Region([(10, 26)]), [4, T], BF16)
        B_onaT = Buf("onaT")

        evac_flip = [0]

        def copy_alt(out, in_, reads, writes, scale=None):
            evac_flip[0] ^= 1
            if evac_flip[0]:
                if scale is None:
                    S.op("scalar", lambda h: h.copy(out=out, in_=in_), reads=reads, writes=writes)
                else:
                    S.op("scalar", lambda h: h.mul(out=out, in_=in_, mul=scale), reads=reads, writes=writes)
            else:
                if scale is None:
                    S.op("vector", lambda h: h.tensor_copy(out=out, in_=in_), reads=reads, writes=writes)
                else:
                    S.op("vector", lambda h: h.tensor_scalar(out=out, in0=in_, scalar1=scale, scalar2=None, op0=ALU.mult),
                         reads=reads, writes=writes)

        class WLoader:
            def __init__(self, reg, nslots=2, ncols=256):
                self.ncols = ncols
                self.wst = Ring("wst", [tile(reg, [8, ncols]) for _ in range(nslots)])
                self.wbf = Ring("wbf", [tile(reg, [8, ncols], BF16) for _ in range(nslots)])

            def load(self, src, col0, ncols=None, kchunks=8, cast_eng="gpsimd", dest=None):
                ncols = ncols or self.ncols
                sv, sb_ = self.wst.next()
                if dest is None:
                    wv, wb = self.wbf.next()
                    wv = wv[:, 0:kchunks, 0:ncols]
                else:
                    wv, wb = dest
                S.dma("sync", lambda h: h.dma_start(out=sv[:, 0:kchunks, 0:ncols],
                                                    in_=src[:, col0:col0 + ncols].rearrange("(c p) n -> p c n", p=128)),
                      writes=[sb_])
                if cast_eng == "scalar":
                    S.op("scalar", lambda h: h.copy(out=wv, in_=sv[:, 0:kchunks, 0:ncols]), reads=[sb_], writes=[wb])
                else:
                    S.op(cast_eng, lambda h: h.tensor_copy(out=wv, in_=sv[:, 0:kchunks, 0:ncols]), reads=[sb_], writes=[wb])
                return wv, wb

        def proj_fm(pring, wv, wb, ncols, evac, act, B_act, kchunks=8):
            for fc in range(ncols // 128):
                for tb in range(4):
                    ps, pb = pring.next()
                    for c in range(kchunks):
                        S.op("tensor", lambda h, ps=ps, c=c, fc=fc, tb=tb: mm_(h,
                            ps, lhsT=wv[:, c, fc * 128:(fc + 1) * 128], rhs=act[:, c, tb * 512:(tb + 1) * 512],
                            start=(c == 0), stop=(c == kchunks - 1)), reads=[wb, B_act], writes=[pb])
                    evac(ps, pb, fc, tb)

        def proj_tm(pring, wv, wb, ncols, evac, act, B_act, kchunks=8):
            for t in range(16):
                ps, pb = pring.next()
                for c in range(kchunks):
                    S.op("tensor", lambda h, ps=ps, c=c, t=t: mm_(h,
                        ps[:, 0:ncols], lhsT=act[:, c, t * 128:(t + 1) * 128], rhs=wv[:, c, 0:ncols],
                        start=(c == 0), stop=(c == kchunks - 1)), reads=[wb, B_act], writes=[pb])
                evac(ps, pb, t)

        def load_xT(xT, B_xT, ring):
            for c in range(8):
                sv, sb_ = ring.next()
                svf = sv.rearrange("p c n -> p (c n)") if len(sv.shape) == 3 else sv
                S.dma("sync", lambda h, svf=svf, c=c: h.dma_start(out=svf[:, 0:T], in_=D["xT"][c * 128:(c + 1) * 128, :]),
                      writes=[sb_])
                if c % 2 == 0:
                    S.op("vector", lambda h, svf=svf, c=c: h.tensor_copy(out=xT[:, c, :], in_=svf[:, 0:T]), reads=[sb_], writes=[B_xT])
                else:
                    S.op("scalar", lambda h, svf=svf, c=c: h.copy(out=xT[:, c, :], in_=svf[:, 0:T]), reads=[sb_], writes=[B_xT])

        B_xT = Buf("xT")
        qnaT = tile(RN, [4, T], BF16)
        knaT = tile(RN, [4, T], BF16)
        B_qnaT, B_knaT = Buf("qnaT"), Buf("knaT")
        vaug = tile(RN, [16, 8, 128], BF16)
        B_vaug = Buf("vaug")
        nab = tile(RN, [8, 9, 128], BF16)
        B_nab = Buf("nab")
        pT_ring = Ring("pT", [tile(RN, [640], BF16) for _ in range(3)])
        rden_ring = Ring("rden", [tile(RN, [128]) for _ in range(2)])
        WL = WLoader(RN)
        xst_ring = Ring("xst", [tile(RN, [T]) for _ in range(2)])
        pin_ring = bank_ring("pin", [0, 1, 2])

        load_xT(xT, B_xT, xst_ring)

        nabf = nab.rearrange("p h v q -> p (h v q)")
        for k in range(6):
            sv, sb_ = WL.wst.next()
            svf = sv.rearrange("p c n -> p (c n)")
            w = 12 * 128
            S.dma("sync", lambda h, svf=svf, k=k, w=w: h.dma_start(out=svf[:, 0:w], in_=D["nab"][:, k * w:(k + 1) * w]), writes=[sb_])
            S.op("gpsimd", lambda h, svf=svf, k=k, w=w: h.tensor_copy(out=nabf[:, k * w:(k + 1) * w], in_=svf[:, 0:w]), reads=[sb_], writes=[B_nab])

        for g in range(2):
            wv, wb = WL.load(D["w_in"], OFF_QNA + g * 256)
            proj_fm(pin_ring, wv, wb, 256, lambda ps, pb, fc, tb, g=g: copy_alt(
                qnaT[:, g * 2 + fc, tb * 512:(tb + 1) * 512], ps, [pb], [B_qnaT], scale=0.125), xT, B_xT)
        for g in range(2):
            wv, wb = WL.load(D["w_in"], OFF_KNA + g * 256)
            proj_fm(pin_ring, wv, wb, 256, lambda ps, pb, fc, tb, g=g: copy_alt(
                knaT[:, g * 2 + fc, tb * 512:(tb + 1) * 512], ps, [pb], [B_knaT]), xT, B_xT)
        S.op("gpsimd", lambda h: h.memset(vaug[:, :, :, 64:128], 1.0), writes=[B_vaug])
        for g in range(2):
            wv, wb = WL.load(D["w_in"], OFF_VNA + g * 256)
            proj_tm(pin_ring, wv, wb, 256, lambda ps, pb, t, g=g: copy_alt(
                vaug[:, t, g * 4:(g + 1) * 4, 0:64], v3(ps[:, 0:256], 4), [pb], [B_vaug]), xT, B_xT)

        sc_ring = Ring("sc", [psum[:, 1024:2048], psum[:, 2048:3072], psum[:, 3072:4096]])
        sc_ring.bufs = [BANKB[2], BANKB[4], BANKB[6]]
        sc_b2 = [BANKB[3], BANKB[5], BANKB[7]]
        po_ring = bank_ring("po", [0, 1])
        def na_scores(i, hd):
            blocks = na_blocks(i)
            nb = len(blocks)
            hc, hp = hd // 2, (hd % 2) * 64
            sc, scb = sc_ring.next()
            scb2 = sc_b2[sc_ring.bufs.index(scb)]
            for bi, (j, var) in enumerate(blocks):
                S.op("tensor", lambda h, bi=bi, j=j: mm_(h,
                    sc[:, bi * 128:(bi + 1) * 128], lhsT=knaT[hp:hp + 64, hc, j * 128:(j + 1) * 128],
                    rhs=qnaT[hp:hp + 64, hc, i * 128:(i + 1) * 128], start=True, stop=False),
                    reads=[B_knaT, B_qnaT], writes=[scb if bi < 4 else scb2])
                S.op("tensor", lambda h, bi=bi, var=var: mm_(h,
                    sc[:, bi * 128:(bi + 1) * 128], lhsT=identb, rhs=nab[:, hd, var, :], start=False, stop=True),
                    reads=[B_cb, B_nab], writes=[scb if bi < 4 else scb2])
            pT, pTb = pT_ring.next()
            n0 = min(nb, 4) * 128
            S.op("scalar", lambda h: h.activation(out=pT[:, 0:n0], in_=sc[:, 0:n0], func=AF.Exp), reads=[scb], writes=[pTb])
            if nb > 4:
                S.op("scalar", lambda h: h.activation(out=pT[:, 512:640], in_=sc[:, 512:640], func=AF.Exp), reads=[scb2], writes=[pTb])
            return blocks, pT, pTb

        def na_pv(i, hd, blocks, pT, pTb):
            nb = len(blocks)
            hc, hp = hd // 2, (hd % 2) * 64
            po, pob = po_ring.next()
            for bi, (j, var) in enumerate(blocks):
                S.op("tensor", lambda h, bi=bi, j=j: mm_(h,
                    po[:, 0:128], lhsT=vaug[:, j, hd, :], rhs=pT[:, bi * 128:(bi + 1) * 128],
                    start=(bi == 0), stop=(bi == nb - 1)), reads=[B_vaug, pTb], writes=[pob])
            rd, rdb = rden_ring.next()
            S.op("vector", lambda h: h.reciprocal(out=rd[64:128, :], in_=po[64:128, 0:128]), reads=[pob], writes=[rdb])
            S.op("vector", lambda h: h.tensor_tensor(
                out=onaT[hp:hp + 64, hc, i * 128:(i + 1) * 128], in0=po[0:64, 0:128], in1=rd[64:128, :], op=ALU.mult),
                reads=[pob, rdb], writes=[B_onaT])

        na_items = [(i, hd) for i in range(16) for hd in range(8)]
        pend = []
        for k_, (i, hd) in enumerate(na_items):
            pend.append((i, hd) + na_scores(i, hd))
            if len(pend) > 1:
                na_pv(*pend.pop(0))
        while pend:
            na_pv(*pend.pop(0))

        if "dbg_onaT" in D:
            S.dma("sync", lambda h: h.dma_start(out=D["dbg_onaT"], in_=onaT), reads=[B_onaT], writes=[Buf("dbg1")])
        S.barrier()

        if stage >= 2:
            RR = Region([(58, 142)])
            RT = Region([(142, 207)])
            qT = tile(RR, [4, T], BF16); kT = tile(RR, [4, T], BF16)
            ktok = tile(RR, [16, 512], BF16); vtok = tile(RR, [16, 512], BF16)
            B_qT, B_kT, B_ktok, B_vtok = Buf("qT"), Buf("kT"), Buf("ktok"), Buf("vtok")
            graw = tile(RR, [16, 32]); B_graw = Buf("graw")
            gv = tile(RR, [32]); nega = tile(RR, [16]); convT = tile(RR, [12, 5])
            tx = tile(RR, [16, 16]); tax = tile(RR, [16, 16]); te = tile(RR, [16, 16]); tsp = tile(RR, [16, 16])
            betat = tile(RR, [16, 16]); gt = tile(RR, [16, 16])
            gD = tile(RR, [2, 128]); betaD = tile(RR, [2, 128]); nbetaD = tile(RR, [2, 128])
            egc = tile(RR, [2, 128]); egd = tile(RR, [2, 128]); bg = tile(RR, [2, 128])
            EGL = tile(RR, [2, 2, 128])
            gDh = tile(RR, [2, 128], BF16); gDl = tile(RR, [2, 128], BF16); gDhf = tile(RR, [2, 128]); gDlf = tile(RR, [2, 128])
            B_g = Buf("gates")
            B_small = Buf("dnsmall")
            WL = WLoader(RT)
            cst_ring = Ring("cst", [tile(RT, [T + 4], BF16) for _ in range(2)])
            diagw = tile(RT, [12, 5, 128], BF16); B_diag = Buf("diagw")
            sil_ring = Ring("sil", [tile(RT, [T], BF16) for _ in range(2)])
            sq_ring = Ring("sq", [tile(RT, [512], BF16) for _ in range(2)])
            tmpf_ring = Ring("tmpf", [tile(RT, [512]) for _ in range(2)])

            S.dma("sync", lambda h: h.dma_start(out=gv, in_=D["gvec"].partition_broadcast(128)), writes=[B_small])
            S.dma("sync", lambda h: h.dma_start(out=convT.rearrange("p a b -> p (a b)"), in_=D["convT"]), writes=[B_small])
            for cs_ in cst_ring.views:
                pass
            for k_, (cs_, csb_) in enumerate(zip(cst_ring.views, cst_ring.bufs)):
                S.op("gpsimd", lambda h, cs_=cs_: h.memset(cs_[:, 0:2], 0.0), writes=[csb_])
                S.op("gpsimd", lambda h, cs_=cs_: h.memset(cs_[:, T + 2:T + 4], 0.0), writes=[csb_])

            wv, wb = WL.load(D["w_in"], OFF_G, ncols=32)
            proj_tm(pin_ring, wv, wb, 32, lambda ps, pb, t: copy_alt(graw[:, t, :], ps[:, 0:32], [pb], [B_graw]), xT, B_xT)
            G_ = [B_graw, B_small, B_g]
            S.op("scalar", lambda h: h.activation(out=betat, in_=graw[:, :, 0:16], func=AF.Sigmoid), reads=G_, writes=[B_g])
            S.op("vector", lambda h: h.tensor_tensor(out=tx, in0=graw[:, :, 16:32],
                                                     in1=gv[:, 16:32].unsqueeze(1).broadcast_to([128, 16, 16]), op=ALU.add),
                 reads=G_, writes=[B_g])
            S.op("vector", lambda h: h.scalar_tensor_tensor(out=tax, in0=tx, scalar=-1.0, in1=tx, op0=ALU.mult, op1=ALU.max), reads=G_, writes=[B_g])
            S.op("scalar", lambda h: h.activation(out=te, in_=tax, func=AF.Exp, scale=-1.0), reads=G_, writes=[B_g])
            S.op("scalar", lambda h: h.activation(out=te, in_=te, func=AF.Ln, bias=1.0), reads=G_, writes=[B_g])
            S.op("vector", lambda h: h.scalar_tensor_tensor(out=tsp, in0=tx, scalar=0.0, in1=te, op0=ALU.max, op1=ALU.add),
                 reads=G_, writes=[B_g])
            S.op("scalar", lambda h: h.activation(out=nega, in_=gv[:, 0:16], func=AF.Exp), reads=G_, writes=[B_g])
            S.op("vector", lambda h: h.tensor_scalar(out=nega, in0=nega, scalar1=-1.0, scalar2=None, op0=ALU.mult), reads=G_, writes=[B_g])
            S.op("vector", lambda h: h.tensor_tensor(out=gt, in0=tsp, in1=nega.unsqueeze(1).broadcast_to([128, 16, 16]), op=ALU.mult),
                 reads=G_, writes=[B_g])
            for d in range(2):
                S.op("vector", lambda h, d=d: h.tensor_copy(out=gD[:, d, :].rearrange("p (t h) -> p t h", t=16),
                                                            in_=gt[:, :, d * 8:(d + 1) * 8]), reads=G_, writes=[B_g])
                S.op("vector", lambda h, d=d: h.tensor_copy(out=betaD[:, d, :].rearrange("p (t h) -> p t h", t=16),
                                                            in_=betat[:, :, d * 8:(d + 1) * 8]), reads=G_, writes=[B_g])
            S.op("vector", lambda h: h.tensor_scalar(out=nbetaD, in0=betaD, scalar1=-1.0, scalar2=None, op0=ALU.mult), reads=G_, writes=[B_g])
            S.op("vector", lambda h: h.tensor_copy(out=gDh, in_=gD), reads=G_, writes=[B_g])
            S.op("vector", lambda h: h.tensor_copy(out=gDhf, in_=gDh), reads=G_, writes=[B_g])
            S.op("vector", lambda h: h.tensor_tensor(out=gDlf, in0=gD, in1=gDhf, op=ALU.subtract), reads=G_, writes=[B_g])
            S.op("vector", lambda h: h.tensor_copy(out=gDl, in_=gDlf), reads=G_, writes=[B_g])
            for d in range(2):
                C1 = C["A1"] if d == 0 else C["A1T"]
                C3 = C["C3f"] if d == 0 else C["C3b"]
                for lhs, dst in ((C1, egc[:, d, :]), (C3, egd[:, d, :]), (C["HA0"], EGL[:, 0, d, :]), (C["HA1"], EGL[:, 1, d, :])):
                    ps, pb = pin_ring.next()
                    S.op("tensor", lambda h, ps=ps, lhs=lhs, d=d: mm_(h, ps[:, 0:128], lhsT=lhs, rhs=gDh[:, d, :], start=True, stop=False),
                         reads=[B_cst, B_g], writes=[pb])
                    S.op("tensor", lambda h, ps=ps, lhs=lhs, d=d: mm_(h, ps[:, 0:128], lhsT=lhs, rhs=gDl[:, d, :], start=False, stop=True),
                         reads=[B_cst, B_g], writes=[pb])
                    S.op("scalar", lambda h, ps=ps, dst=dst: h.activation(out=dst, in_=ps[:, 0:128], func=AF.Exp), reads=[pb], writes=[B_g])
            S.op("vector", lambda h: h.tensor_tensor(out=bg, in0=betaD, in1=egc, op=ALU.mult), reads=G_, writes=[B_g])

            for ch12 in range(12):
                S.op("vector", lambda h, ch12=ch12: h.tensor_tensor(
                    out=diagw[:, ch12, :, :], in0=identb.unsqueeze(1).broadcast_to([128, 5, 128]),
                    in1=convT[:, ch12, :].unsqueeze(2).broadcast_to([128, 5, 128]), op=ALU.mult), reads=[B_cb, B_small], writes=[B_diag])
            chunks = [(grp, kind, off, g, fc) for grp, (off, kind) in enumerate([(OFF_QDN, "q"), (OFF_KDN, "k"), (OFF_VDN, "v")])
                      for g in range(2) for fc in range(2)]
            conv_ring = bank_ring("cv", [4, 5])
            aux_ring = bank_ring("aux", [6, 7])
            pin4 = bank_ring("pin4", [0, 1, 2, 3])
            wcur = {}
            cs_of, sil_of = {}, {}

            def st_A(ci):
                grp, kind, off, g, fc = chunks[ci]
                if fc == 0:
                    wcur[(grp, g)] = WL.load(D["w_in"], off + g * 256)
                wv, wb = wcur[(grp, g)]
                cs, csb = cst_ring.next()
                cs_of[ci] = (cs, csb)
                for tb in range(4):
                    ps, pb = pin4.next()
                    for c in range(8):
                        S.op("tensor", lambda h, ps=ps, c=c, tb=tb: mm_(h,
                            ps, lhsT=wv[:, c, fc * 128:(fc + 1) * 128], rhs=xT[:, c, tb * 512:(tb + 1) * 512],
                            start=(c == 0), stop=(c == 7)), reads=[wb, B_xT], writes=[pb])
                    copy_alt(cs[:, 2 + tb * 512:2 + (tb + 1) * 512], ps, [pb], [csb])

            def st_B(ci):
                grp, kind, off, g, fc = chunks[ci]
                ch12 = grp * 4 + g * 2 + fc
                cs, csb = cs_of[ci]
                sl, slb = sil_ring.next()
                sil_of[ci] = (sl, slb)
                for tb in range(4):
                    ps, pb = conv_ring.next()
                    for tau in range(5):
                        S.op("tensor", lambda h, ps=ps, tau=tau, tb=tb: mm_(h,
                            ps, lhsT=diagw[:, ch12, tau, :], rhs=cs[:, tb * 512 + tau:tb * 512 + tau + 512],
                            start=(tau == 0), stop=(tau == 4)), reads=[B_diag, csb], writes=[pb])
                    S.op("scalar", lambda h, ps=ps, tb=tb: h.activation(out=sl[:, tb * 512:(tb + 1) * 512], in_=ps, func=AF.Silu),
                         reads=[pb], writes=[slb])

            def st_C(ci):
                grp, kind, off, g, fc = chunks[ci]
                fcg = g * 2 + fc
                if kind == "v":
                    return
                sl, slb = sil_of[ci]
                for tb in range(4):
                    sq_, sqb_ = sq_ring.next()
                    S.op("scalar", lambda h, sq_=sq_, tb=tb: h.activation(out=sq_, in_=sl[:, tb * 512:(tb + 1) * 512], func=AF.Square),
                         reads=[slb], writes=[sqb_])
                    ps, pb = aux_ring.next()
                    S.op("tensor", lambda h, ps=ps, sq_=sq_: mm_(h, ps, lhsT=CBn["H"], rhs=sq_, start=True, stop=True),
                         reads=[B_cb, sqb_], writes=[pb])
                    tf_, tfb_ = tmpf_ring.next()
                    S.op("scalar", lambda h, ps=ps, tf_=tf_: h.activation(out=tf_, in_=ps, func=AF.Sqrt, bias=RMS_EPS), reads=[pb], writes=[tfb_])
                    S.op("vector", lambda h, tf_=tf_: h.reciprocal(out=tf_, in_=tf_), reads=[tfb_], writes=[tfb_])
                    if kind == "q":
                        S.op("vector", lambda h, tf_=tf_, tb=tb: h.scalar_tensor_tensor(
                            out=qT[:, fcg, tb * 512:(tb + 1) * 512], in0=sl[:, tb * 512:(tb + 1) * 512], scalar=0.125, in1=tf_,
                            op0=ALU.mult, op1=ALU.mult), reads=[slb, tfb_], writes=[B_qT])
                    else:
                        S.op("vector", lambda h, tf_=tf_, tb=tb: h.tensor_tensor(
                            out=kT[:, fcg, tb * 512:(tb + 1) * 512], in0=sl[:, tb * 512:(tb + 1) * 512], in1=tf_, op=ALU.mult),
                            reads=[slb, tfb_], writes=[B_kT])

            def st_D(ci):
                grp, kind, off, g, fc = chunks[ci]
                fcg = g * 2 + fc
                if kind == "q":
                    return
                if kind == "v":
                    sl, slb = sil_of[ci]
                    src_, srcb, dst_, dstb = (lambda a, b: sl[:, a:b]), slb, vtok, B_vtok
                else:
                    src_, srcb, dst_, dstb = (lambda a, b: kT[:, fcg, a:b]), B_kT, ktok, B_ktok
                for t4 in range(4):
                    ps, pb = aux_ring.next()
                    for u in range(4):
                        tt_ = t4 * 4 + u
                        S.op("tensor", lambda h, ps=ps, u=u, tt_=tt_: mm_(h,
                            ps[:, u * 128:(u + 1) * 128], lhsT=src_(tt_ * 128, (tt_ + 1) * 128), rhs=identb,
                            start=True, stop=True), reads=[srcb, B_cb], writes=[pb])
                    copy_alt(dst_[:, t4 * 4:(t4 + 1) * 4, fcg * 128:(fcg + 1) * 128], v3(ps, 4), [pb], [dstb])

            nch = len(chunks)
            for step in range(nch + 3):
                if 0 <= step - 3 < nch:
                    st_D(step - 3)
                if 0 <= step - 2 < nch:
                    st_C(step - 2)
                if 0 <= step - 1 < nch:
                    st_B(step - 1)
                if step < nch:
                    st_A(step)
            S.barrier()

        if stage >= 3:
            RO = Region([(26, 42)])
            RM = Region([(42, 58), (142, 207)])
            o_dn = tile(RO, [16, 512], BF16)
            B_odn = [Buf(f"odn{t}") for t in range(16)]
            TMP = []
            for d_ in range(2):
                TMP.append(dict(
                    rhsDh=tile(RM, [8, 128], BF16), rhsDl=tile(RM, [8, 128], BF16), B_rhsD=Buf(f"rhsD{d_}"),
                    Eb=tile(RM, [8, 128]), B_E=Buf(f"E{d_}"),
                    P_ring=Ring(f"P{d_}", [tile(RM, [8, 128], BF16) for _ in range(2)]),
                    PT_ring=Ring(f"PT{d_}", [tile(RM, [8, 128], BF16) for _ in range(2)]),
                    X=tile(RM, [8, 128], BF16), B_X=Buf(f"X{d_}"),
                    vb=tile(RM, [512], BF16), kbe=tile(RM, [512], BF16), B_vk=Buf(f"vbkbe{d_}")))
            sets = []
            for k_ in range(4):
                sets.append(dict(WT=tile(RM, [8, 128], BF16), U=tile(RM, [512], BF16), IT=tile(RM, [8, 128], BF16),
                                 KD=tile(RM, [512], BF16), b=Buf(f"set{k_}")))
            Sst = tile(RM, [2, 4, 64]); Sbf = tile(RM, [2, 4, 64], BF16)
            B_S = [Buf("S0"), Buf("S1")]; B_Sbf = [Buf("Sbf0"), Buf("Sbf1")]
            vnew = tile(RM, [2, 512], BF16); B_vnew = [Buf("vn0"), Buf("vn1")]
            ot_ring = Ring("ot", [tile(RM, [512]) for _ in range(2)])
            lring = bank_ring("lps", [0, 1, 2, 3])
            sring = bank_ring("sps", [4, 5, 6, 7])
            EGL5 = EGL.rearrange("q a d (t hc hp) -> q a d t hc hp", t=16, hc=4, hp=2)

            S.op("gpsimd", lambda h: h.memset(Sst, 0.0), writes=B_S)
            S.op("gpsimd", lambda h: h.memset(Sbf, 0.0), writes=B_Sbf)

            def local(t, d, st_):
                T_ = TMP[d]
                rhsDh, rhsDl, B_rhsD, Eb, B_E = T_["rhsDh"], T_["rhsDl"], T_["B_rhsD"], T_["Eb"], T_["B_E"]
                P_ring, PT_ring, X, B_X, vb, kbe, B_vk = T_["P_ring"], T_["PT_ring"], T_["X"], T_["B_X"], T_["vb"], T_["kbe"], T_["B_vk"]
                C1 = C["A1"] if d == 0 else C["A1T"]
                C2 = C["A2"] if d == 0 else C["A2T"]
                NEGS = CBn["NEGSf"] if d == 0 else CBn["NEGSb"]
                NEG2 = CBn["NEG2f"] if d == 0 else CBn["NEG2b"]
                col = lambda h_: t * 8 + h_
                sb_ = st_["b"]

                def decay(Cl, Cr, NEGm):
                    if os.environ.get("RHSD_ACT", "0") == "1":
                        for h_ in range(8):
                            for dst, src in ((rhsDh, gDhf), (rhsDl, gDlf)):
                                S.op("scalar", lambda h, dst=dst, src=src, h_=h_: h.activation(
                                    out=dst[:, h_, :], in_=Cr, func=AF.Identity, scale=src[:, d, col(h_):col(h_) + 1]),
                                    reads=[B_cst, B_g], writes=[B_rhsD])
                    else:
                        for dst, src, eng_ in ((rhsDh, gDhf, "gpsimd"), (rhsDl, gDlf, "vector")):
                            S.op(eng_, lambda h, dst=dst, src=src: h.tensor_tensor(
                                out=dst, in0=Cr.unsqueeze(1).broadcast_to([128, 8, 128]),
                                in1=src[:, d, t * 8:(t + 1) * 8].unsqueeze(2).broadcast_to([128, 8, 128]), op=ALU.mult),
                                reads=[B_cst, B_g], writes=[B_rhsD])
                    for half in range(2):
                        ps, pb = lring.next()
                        for hh in range(4):
                            h_ = half * 4 + hh
                            S.op("tensor", lambda h, ps=ps, hh=hh: mm_(h, ps[:, hh * 128:(hh + 1) * 128], lhsT=identb, rhs=NEGm,
                                                                       start=True, stop=False), reads=[B_cb], writes=[pb])
                            S.op("tensor", lambda h, ps=ps, hh=hh, h_=h_: mm_(h, ps[:, hh * 128:(hh + 1) * 128], lhsT=Cl, rhs=rhsDh[:, h_, :],
                                                                              start=False, stop=False), reads=[B_cst, B_rhsD], writes=[pb])
                            S.op("tensor", lambda h, ps=ps, hh=hh, h_=h_: mm_(h, ps[:, hh * 128:(hh + 1) * 128], lhsT=Cl, rhs=rhsDl[:, h_, :],
                                                                              start=False, stop=True), reads=[B_cst, B_rhsD], writes=[pb])
                        S.op("scalar", lambda h, ps=ps, half=half: h.activation(out=Eb[:, half * 4:(half + 1) * 4, :], in_=v3(ps, 4), func=AF.Exp),
                             reads=[pb], writes=[B_E])

                decay(C1, C2, NEGS)
                yield
                Pm, Pmb = P_ring.next()
                for par in range(2):
                    ps, pb = lring.next()
                    hp = par * 64
                    for hh in range(4):
                        S.op("tensor", lambda h, ps=ps, hh=hh, hp=hp: mm_(h,
                            ps[:, hh * 128:(hh + 1) * 128], lhsT=kT[hp:hp + 64, hh, t * 128:(t + 1) * 128],
                            rhs=kT[hp:hp + 64, hh, t * 128:(t + 1) * 128], start=True, stop=True), reads=[B_kT], writes=[pb])
                    for hh in range(4):
                        h_ = hh * 2 + par
                        S.op("vector", lambda h, ps=ps, hh=hh, h_=h_, Pm=Pm: h.scalar_tensor_tensor(
                            out=Pm[:, h_, :], in0=ps[:, hh * 128:(hh + 1) * 128], scalar=nbetaD[:, d, col(h_):col(h_) + 1],
                            in1=Eb[:, h_, :], op0=ALU.mult, op1=ALU.mult), reads=[pb, B_g, B_E], writes=[Pmb])
                yield
                PTm, PTmb = PT_ring.next()
                for half in range(2):
                    ps, pb = lring.next()
                    for hh in range(4):
                        h_ = half * 4 + hh
                        S.op("tensor", lambda h, ps=ps, hh=hh, h_=h_, Pm=Pm: mm_(h, ps[:, hh * 128:(hh + 1) * 128], lhsT=Pm[:, h_, :], rhs=identb,
                                                                                      start=True, stop=True), reads=[Pmb, B_cb], writes=[pb])
                    S.op("scalar", lambda h, ps=ps, half=half, PTm=PTm: h.copy(out=PTm[:, half * 4:(half + 1) * 4, :], in_=v3(ps, 4)),
                         reads=[pb], writes=[PTmb])
                    S.op("vector", lambda h, ps=ps, half=half: h.tensor_tensor(
                        out=X[:, half * 4:(half + 1) * 4, :], in0=v3(ps, 4), in1=identf.unsqueeze(1).broadcast_to([128, 4, 128]), op=ALU.add),
                        reads=[pb, B_cst], writes=[B_X])
                yield
                for m in range(6):
                    last = (m == 5)
                    if not last:
                        Pn, Pnb = P_ring.next()
                        PTn, PTnb = PT_ring.next()
                    for half in range(2):
                        hs = [half * 4 + hh for hh in range(4)]
                        if not last:
                            psA, pbA = lring.next()
                            for hh, h_ in enumerate(hs):
                                S.op("tensor", lambda h, psA=psA, hh=hh, h_=h_, Pm=Pm, PTm=PTm: mm_(h,
                                    psA[:, hh * 128:(hh + 1) * 128], lhsT=PTm[:, h_, :], rhs=Pm[:, h_, :], start=True, stop=True),
                                    reads=[Pmb, PTmb], writes=[pbA])
                            S.op("scalar", lambda h, psA=psA, half=half, Pn=Pn: h.copy(out=Pn[:, half * 4:(half + 1) * 4, :], in_=v3(psA, 4)),
                                 reads=[pbA], writes=[Pnb])
                            psB, pbB = lring.next()
                            for hh, h_ in enumerate(hs):
                                S.op("tensor", lambda h, psB=psB, hh=hh, h_=h_, Pm=Pm, PTm=PTm: mm_(h,
                                    psB[:, hh * 128:(hh + 1) * 128], lhsT=Pm[:, h_, :], rhs=PTm[:, h_, :], start=True, stop=True),
                                    reads=[Pmb, PTmb], writes=[pbB])
                            S.op("scalar", lambda h, psB=psB, half=half, PTn=PTn: h.copy(out=PTn[:, half * 4:(half + 1) * 4, :], in_=v3(psB, 4)),
                                 reads=[pbB], writes=[PTnb])
                        if m >= 1:
                            psC, pbC = lring.next()
                            for hh, h_ in enumerate(hs):
                                S.op("tensor", lambda h, psC=psC, hh=hh, h_=h_, Pm=Pm: mm_(h,
                                    psC[:, hh * 128:(hh + 1) * 128], lhsT=Pm[:, h_, :], rhs=X[:, h_, :], start=True, stop=True),
                                    reads=[Pmb, B_X], writes=[pbC])
                            S.op("vector", lambda h, psC=psC, half=half: h.tensor_tensor(
                                out=X[:, half * 4:(half + 1) * 4, :], in0=v3(psC, 4), in1=X[:, half * 4:(half + 1) * 4, :], op=ALU.add),
                                reads=[pbC, B_X], writes=[B_X])
                    if not last:
                        Pm, Pmb, PTm, PTmb = Pn, Pnb, PTn, PTnb
                    yield
                for dst, src, sc_, wbuf in ((vb, vtok, betaD, B_vk), (kbe, ktok, bg, B_vk), (st_["KD"], ktok, egd, sb_)):
                    S.op("gpsimd", lambda h, dst=dst, src=src, sc_=sc_: h.tensor_tensor(
                        out=v3(dst, 8), in0=v3(src[:, t, :], 8),
                        in1=sc_[:, d, t * 8:(t + 1) * 8].unsqueeze(2).broadcast_to([128, 8, 64]), op=ALU.mult),
                        reads=[B_vtok, B_ktok, B_g], writes=[wbuf])
                psU, pbU = lring.next()
                for h_ in range(8):
                    S.op("tensor", lambda h, psU=psU, h_=h_: mm_(h, psU[:, h_ * 64:(h_ + 1) * 64], lhsT=X[:, h_, :], rhs=vb[:, h_ * 64:(h_ + 1) * 64],
                                                                      start=True, stop=True), reads=[B_X, B_vk], writes=[pbU])
                S.op("scalar", lambda h, psU=psU: h.copy(out=st_["U"], in_=psU), reads=[pbU], writes=[sb_])
                WT4 = st_["WT"].rearrange("p (hc hp) c -> p hc hp c", hp=2)
                for half in range(2):
                    psW, pbW = lring.next()
                    for hh in range(4):
                        h_ = half * 4 + hh
                        hc = h_ // 2
                        S.op("tensor", lambda h, psW=psW, hh=hh, h_=h_, hc=hc: mm_(h,
                            psW[:, hh * 128:(hh + 1) * 128], lhsT=kbe[:, hc * 128:(hc + 1) * 128], rhs=X[:, h_, :], start=True, stop=True),
                            reads=[B_vk, B_X], writes=[pbW])
                    pw4 = psW.rearrange("p (hc hp c) -> p hc hp c", hc=2, hp=2)
                    for p_ in range(2):
                        S.op("vector", lambda h, pw4=pw4, p_=p_, half=half: h.tensor_copy(
                            out=WT4[p_ * 64:(p_ + 1) * 64, half * 2:(half + 1) * 2, p_, :], in_=pw4[p_ * 64:(p_ + 1) * 64, :, p_, :]),
                            reads=[pbW], writes=[sb_])
                yield
                decay(C2, C1, NEG2)
                IT4 = st_["IT"].rearrange("p (hc hp) c -> p hc hp c", hp=2)
                Eb4 = Eb.rearrange("p (hc hp) c -> p hc hp c", hp=2)
                for par in range(2):
                    ps, pb = lring.next()
                    hp = par * 64
                    for hh in range(4):
                        S.op("tensor", lambda h, ps=ps, hh=hh, hp=hp: mm_(h,
                            ps[:, hh * 128:(hh + 1) * 128], lhsT=kT[hp:hp + 64, hh, t * 128:(t + 1) * 128],
                            rhs=qT[hp:hp + 64, hh, t * 128:(t + 1) * 128], start=True, stop=True), reads=[B_kT, B_qT], writes=[pb])
                    S.op("vector", lambda h, ps=ps, par=par: h.tensor_tensor(
                        out=IT4[:, :, par, :], in0=v3(ps, 4), in1=Eb4[:, :, par, :], op=ALU.mult),
                        reads=[pb, B_E], writes=[sb_])

            def scan_d(tt, k, d):
                if True:
                    a = k if d == 0 else 1 - k
                    t = tt if d == 0 else 15 - tt
                    st_ = sets[(tt % 2) * 2 + d]
                    sb_ = st_["b"]
                    r0 = 64 * a
                    rows = slice(r0, r0 + 64)
                    pe_, peb = sring.next()
                    po_, pob_ = sring.next()
                    pbk = [(pe_, peb), (po_, pob_)]
                    for par in range(2):
                        hp = par * 64
                        bk, bkb = pbk[par]
                        for hc in range(4):
                            h_ = hc * 2 + par
                            S.op("tensor", lambda h, bk=bk, h_=h_, hc=hc, hp=hp: mm_(h,
                                bk[:, hc * 64:(hc + 1) * 64], lhsT=st_["WT"][hp:hp + 64, h_, :], rhs=Sbf[hp:hp + 64, d, hc, :],
                                start=True, stop=True), reads=[sb_, B_Sbf[d]], writes=[bkb])
                        for hc in range(4):
                            S.op("tensor", lambda h, bk=bk, hc=hc, hp=hp: mm_(h,
                                bk[:, 256 + hc * 64:256 + (hc + 1) * 64], lhsT=qT[hp:hp + 64, hc, t * 128:(t + 1) * 128], rhs=Sbf[hp:hp + 64, d, hc, :],
                                start=True, stop=True), reads=[B_qT, B_Sbf[d]], writes=[bkb])
                    vnew5 = vnew.rearrange("p d (hc hp v) -> p d hc hp v", hc=4, hp=2)
                    U4 = st_["U"].rearrange("p (hc hp v) -> p hc hp v", hc=4, hp=2)
                    for par in range(2):
                        bk, bkb = pbk[par]
                        S.op("vector", lambda h, bk=bk, par=par: h.tensor_tensor(
                            out=vnew5[rows, d, :, par, :], in0=U4[rows, :, par, :], in1=v3(bk[rows, 0:256], 4), op=ALU.subtract),
                            reads=[sb_, bkb], writes=[B_vnew[d]])
                    yield
                    pi, pib = sring.next()
                    for h_ in range(8):
                        S.op("tensor", lambda h, pi=pi, h_=h_: mm_(h,
                            pi[:, h_ * 64:(h_ + 1) * 64], lhsT=st_["IT"][rows, h_, :], rhs=vnew[rows, d, h_ * 64:(h_ + 1) * 64],
                            start=True, stop=True), reads=[sb_, B_vnew[d]], writes=[pib])
                    ot, otb = ot_ring.next()
                    ot4 = ot.rearrange("p (hc hp v) -> p hc hp v", hc=4, hp=2)
                    egc5 = egc.rearrange("p d (t hc hp) -> p d t hc hp", t=16, hc=4, hp=2)
                    for par in range(2):
                        bk, bkb = pbk[par]
                        S.op("vector", lambda h, ot4=ot4, bk=bk, par=par: h.tensor_tensor(
                            out=ot4[rows, :, par, :], in0=v3(bk[rows, 256:512], 4),
                            in1=egc5[rows, d, t, :, par].unsqueeze(2).broadcast_to([64, 4, 64]), op=ALU.mult),
                            reads=[bkb, B_g], writes=[otb])
                    first = (d == 0) == (t < 8)
                    if first:
                        S.op("vector", lambda h, ot=ot, pi=pi: h.tensor_tensor(out=o_dn[rows, t, :], in0=ot[rows, :], in1=pi[rows, :], op=ALU.add),
                             reads=[otb, pib], writes=[B_odn[t]])
                    else:
                        S.op("vector", lambda h, ot=ot, pi=pi: h.tensor_tensor(out=ot[rows, :], in0=ot[rows, :], in1=pi[rows, :], op=ALU.add),
                             reads=[otb, pib], writes=[otb])
                        S.op("gpsimd", lambda h, ot=ot: h.tensor_tensor(out=o_dn[rows, t, :], in0=o_dn[rows, t, :], in1=ot[rows, :], op=ALU.add),
                             reads=[otb, B_odn[t]], writes=[B_odn[t]])
                    yield
                    psu, psub = sring.next()
                    for h_ in range(8):
                        hc = h_ // 2
                        S.op("tensor", lambda h, psu=psu, h_=h_, hc=hc: mm_(h,
                            psu[:, h_ * 64:(h_ + 1) * 64], lhsT=st_["KD"][rows, hc * 128:(hc + 1) * 128], rhs=vnew[rows, d, h_ * 64:(h_ + 1) * 64],
                            start=True, stop=True), reads=[sb_, B_vnew[d]], writes=[psub])
                    psu4 = psu.rearrange("q (hc hp v) -> q hc hp v", hc=4, hp=2)
                    for p_ in range(2):
                        pr = slice(p_ * 64, (p_ + 1) * 64)
                        S.op("vector", lambda h, pr=pr, p_=p_: h.tensor_tensor(
                            out=Sst[pr, d, :, :], in0=Sst[pr, d, :, :],
                            in1=EGL5[pr, a, d, t, :, p_].unsqueeze(2).broadcast_to([64, 4, 64]), op=ALU.mult),
                            reads=[B_S[d], B_g], writes=[B_S[d]])
                        S.op("vector", lambda h, pr=pr, p_=p_, psu4=psu4: h.tensor_tensor(
                            out=Sst[pr, d, :, :], in0=Sst[pr, d, :, :], in1=psu4[pr, :, p_, :], op=ALU.add),
                            reads=[B_S[d], psub], writes=[B_S[d]])
                        S.op("scalar", lambda h, pr=pr: h.copy(out=Sbf[pr, d, :, :], in_=Sst[pr, d, :, :]), reads=[B_S[d]], writes=[B_Sbf[d]])

            import itertools

            def drive(gens):
                live = list(gens)
                while live:
                    for g_ in list(live):
                        try:
                            next(g_)
                        except StopIteration:
                            live.remove(g_)

            def scan_gen(tt):
                return itertools.chain(scan_d(tt, 0, 0), scan_d(tt, 0, 1), scan_d(tt, 1, 0), scan_d(tt, 1, 1))

            for tt in range(16):
                gens = [local(tt, 0, sets[(tt % 2) * 2 + 0]), local(15 - tt, 1, sets[(tt % 2) * 2 + 1])]
                if tt > 0:
                    gens.append(scan_gen(tt - 1))
                drive(gens)
            drive([scan_gen(15)])

            if "dbg_odn" in D:
                S.dma("sync", lambda h: h.dma_start(out=D["dbg_odn"], in_=o_dn), reads=B_odn, writes=[Buf("dbg2")])
            S.barrier()


        def layer_norm_tile(r, gb, out, st, junk, Bs_r, B_gb, B_out, B_st, B_junk, eng_mul="vector"):
            S.op("vector", lambda h: h.memset(st[:, 0:2], 0.0), writes=[B_st])
            S.op("scalar", lambda h: h.activation(out=junk, in_=r, func=AF.Identity, accum_out=st[:, 0:1]), reads=Bs_r + [B_st], writes=[B_junk, B_st])
            S.op("scalar", lambda h: h.activation(out=junk, in_=r, func=AF.Square, accum_out=st[:, 1:2]), reads=Bs_r + [B_st], writes=[B_junk, B_st])
            S.op("vector", lambda h: h.tensor_scalar(out=st[:, 2:3], in0=st[:, 0:1], scalar1=1.0 / DM, scalar2=None, op0=ALU.mult), reads=[B_st], writes=[B_st])
            S.op("vector", lambda h: h.tensor_tensor(out=st[:, 3:4], in0=st[:, 2:3], in1=st[:, 2:3], op=ALU.mult), reads=[B_st], writes=[B_st])
            S.op("vector", lambda h: h.scalar_tensor_tensor(out=st[:, 4:5], in0=st[:, 1:2], scalar=1.0 / DM, in1=st[:, 3:4], op0=ALU.mult, op1=ALU.subtract),
                 reads=[B_st], writes=[B_st])
            S.op("scalar", lambda h: h.activation(out=st[:, 5:6], in_=st[:, 4:5], func=AF.Sqrt, bias=LN_EPS), reads=[B_st], writes=[B_st])
            S.op("vector", lambda h: h.reciprocal(out=st[:, 6:7], in_=st[:, 5:6]), reads=[B_st], writes=[B_st])
            S.op("vector", lambda h: h.tensor_scalar(out=out, in0=r, scalar1=st[:, 2:3], scalar2=st[:, 6:7], op0=ALU.subtract, op1=ALU.mult),
                 reads=Bs_r + [B_st], writes=[B_out])
            S.op(eng_mul, lambda h: h.tensor_tensor(out=out, in0=out, in1=gb[:, 0:DM], op=ALU.mult), reads=[B_out, B_gb], writes=[B_out])
            S.op(eng_mul, lambda h: h.tensor_tensor(out=out, in0=out, in1=gb[:, DM:2 * DM], op=ALU.add), reads=[B_out, B_gb], writes=[B_out])

        if stage >= 5:
            RP = Region([(58, 174)])
            mergedT = tile(Region([(174, 206)]), [8, T], BF16); B_mT = Buf("mergedT")
            xT2 = tile(RP, [8, T], BF16); B_xT2 = Buf("xT2")
            odnT = tile(RP, [4, T], BF16); B_odnT = Buf("odnT")
            wz = tile(RP, [8, 512], BF16); B_wz = Buf("wz")
            WL = WLoader(RP, nslots=3)
            normw = tile(RP, [64]); B_nw = Buf("normw")
            sz_ring = Ring("sz", [tile(RP, [512]) for _ in range(2)])
            sq_ring = Ring("sq", [tile(RP, [512]) for _ in range(2)])
            of_ring = Ring("of", [tile(RP, [512], BF16) for _ in range(2)])
            ss_ring = Ring("ss", [tile(RP, [16]) for _ in range(2)])
            tA_ring = Ring("tA", [tile(RP, [512]) for _ in range(2)])
            tB_ring = Ring("tB", [tile(RP, [512]) for _ in range(2)])
            pz_ring = bank_ring("pz", [0, 1])
            pt_ring = bank_ring("pt", [2, 3])
            pm_ring = bank_ring("pm", [4, 5, 6, 7])

            load_xT(xT2, B_xT2, WL.wst)
            S.dma("sync", lambda h: h.dma_start(out=normw, in_=D["norm_w"].partition_broadcast(128)), writes=[B_nw])
            for g in range(2):
                WL.load(D["w_in"], OFF_ZDN + g * 256, dest=(wz[:, :, g * 256:(g + 1) * 256], B_wz))

            def dn_post(t):
                psz, pzb = pz_ring.next()
                for c in range(8):
                    S.op("tensor", lambda h, c=c: mm_(h, psz, lhsT=xT2[:, c, t * 128:(t + 1) * 128], rhs=wz[:, c, :],
                                                      start=(c == 0), stop=(c == 7)), reads=[B_xT2, B_wz], writes=[pzb])
                yield
                sz, szb = sz_ring.next()
                S.op("scalar", lambda h: h.activation(out=sz, in_=psz, func=AF.Silu), reads=[pzb], writes=[szb])
                sq, sqb_ = sq_ring.next()
                ss, ssb = ss_ring.next()
                o_t = o_dn[:, t, :]
                S.op("vector", lambda h: h.tensor_tensor(out=sq, in0=o_t, in1=o_t, op=ALU.mult), reads=[B_odn[t]], writes=[sqb_])
                yield
                S.op("vector", lambda h: h.tensor_reduce(out=ss[:, 0:8], in_=v3(sq, 8), axis=AX.X, op=ALU.add), reads=[sqb_], writes=[ssb])
                S.op("vector", lambda h: h.tensor_scalar(out=ss[:, 0:8], in0=ss[:, 0:8], scalar1=1.0 / 64, scalar2=RMS_EPS, op0=ALU.mult, op1=ALU.add),
                     reads=[ssb], writes=[ssb])
                yield
                S.op("scalar", lambda h: h.activation(out=ss[:, 8:16], in_=ss[:, 0:8], func=AF.Sqrt), reads=[ssb], writes=[ssb])
                yield
                S.op("vector", lambda h: h.reciprocal(out=ss[:, 8:16], in_=ss[:, 8:16]), reads=[ssb], writes=[ssb])
                S.op("vector", lambda h: h.tensor_tensor(out=v3(sq, 8), in0=v3(o_t, 8), in1=ss[:, 8:16].unsqueeze(2).broadcast_to([128, 8, 64]), op=ALU.mult),
                     reads=[B_odn[t], ssb], writes=[sqb_])
                S.op("vector", lambda h: h.tensor_tensor(out=v3(sq, 8), in0=v3(sq, 8), in1=normw.unsqueeze(1).broadcast_to([128, 8, 64]), op=ALU.mult),
                     reads=[B_nw], writes=[sqb_])
                yield
                of, ofb = of_ring.next()
                S.op("vector", lambda h: h.tensor_tensor(out=of, in0=sq, in1=sz, op=ALU.mult), reads=[sqb_, szb], writes=[ofb])
                yield
                pst, ptb = pt_ring.next()
                for fc in range(4):
                    S.op("tensor", lambda h, fc=fc: mm_(h, pst[:, fc * 128:(fc + 1) * 128], lhsT=of[:, fc * 128:(fc + 1) * 128], rhs=identb,
                                                        start=True, stop=True), reads=[ofb, B_cb], writes=[ptb])
                S.op("scalar", lambda h: h.copy(out=odnT[:, :, t * 128:(t + 1) * 128], in_=v3(pst, 4)), reads=[ptb], writes=[B_odnT])

            def merged_block(part, wp, wpb, wg, wgb, fc, fcg, tb, oT, B_oT):
                ps1, p1b = pm_ring.next()
                for c in range(4):
                    S.op("tensor", lambda h, c=c: mm_(h, ps1, lhsT=wp[:, c, fc * 128:(fc + 1) * 128], rhs=oT[:, c, tb * 512:(tb + 1) * 512],
                                                      start=(c == 0), stop=(c == 3)), reads=[wpb, B_oT], writes=[p1b])
                ps2, p2b = pm_ring.next()
                for c in range(8):
                    S.op("tensor", lambda h, c=c: mm_(h, ps2, lhsT=wg[:, c, fc * 128:(fc + 1) * 128], rhs=xT2[:, c, tb * 512:(tb + 1) * 512],
                                                      start=(c == 0), stop=(c == 7)), reads=[wgb, B_xT2], writes=[p2b])
                tA, tAb = tA_ring.next()
                S.op("scalar", lambda h: h.activation(out=tA, in_=ps2, func=AF.Sigmoid), reads=[p2b], writes=[tAb])
                dst = mergedT[:, fcg, tb * 512:(tb + 1) * 512]
                if part == 0:
                    S.op("vector", lambda h: h.tensor_tensor(out=dst, in0=ps1, in1=tA, op=ALU.mult), reads=[p1b, tAb], writes=[B_mT])
                else:
                    tB, tBb = tB_ring.next()
                    S.op("vector", lambda h: h.tensor_tensor(out=tB, in0=ps1, in1=tA, op=ALU.mult), reads=[p1b, tAb], writes=[tBb])
                    S.op("vector", lambda h: h.tensor_tensor(out=dst, in0=dst, in1=tB, op=ALU.add), reads=[tBb, B_mT], writes=[B_mT])

            def p3a_part(part):
                for g in range(4):
                    wp, wpb = WL.load(D["w_proj_na"] if part == 0 else D["w_proj_dn"], g * 256, kchunks=4, cast_eng="scalar")
                    wg, wgb = WL.load(D["w_in"], (OFF_GNA if part == 0 else OFF_GDN) + g * 256, cast_eng="scalar")
                    for fc in range(2):
                        for tb in range(4):
                            merged_block(part, wp, wpb, wg, wgb, fc, g * 2 + fc, tb,
                                         onaT if part == 0 else odnT, B_onaT if part == 0 else B_odnT)
                            yield

            def dn_post_all():
                for t in range(16):
                    yield from dn_post(t)

            def drive3(gens):
                live = list(gens)
                while live:
                    for g_ in list(live):
                        try:
                            next(g_)
                        except StopIteration:
                            live.remove(g_)

            drive3([dn_post_all(), p3a_part(0)])
            if "dbg_odnT" in D:
                S.dma("sync", lambda h: h.dma_start(out=D["dbg_odnT"], in_=odnT), reads=[B_odnT], writes=[Buf("dbg3")])
            drive3([p3a_part(1)])
            S.barrier()

        if stage >= 6:
            RB = Region([(10, 174)])
            acc = tile(RB, [16, DM]); B_acct = [Buf(f"acc{t}") for t in range(16)]
            x1T = tile(RB, [8, T], BF16); B_x1T = Buf("x1T")
            comb = tile(RB, [16, 32]); B_comb = Buf("comb")
            wout = tile(RB, [8, DM], BF16); B_wout = Buf("wout")
            ln1gb = tile(RB, [2 * DM]); B_ln1 = Buf("ln1gb")
            wrf = tile(RB, [8, 36]); wrh = tile(RB, [8, 36], BF16); wrl = tile(RB, [8, 36], BF16); brb = tile(RB, [36]); B_wr = Buf("wr")
            WLb = WLoader(RB, nslots=2, ncols=128)
            xt_ring = Ring("xt", [tile(RB, [DM]) for _ in range(2)])
            r_ring = Ring("r", [tile(RB, [DM]) for _ in range(1)])
            x1_ring = Ring("x1", [tile(RB, [DM]) for _ in range(1)])
            x1h_ring = Ring("x1h", [tile(RB, [DM], BF16) for _ in range(2)])
            x1l_ring = Ring("x1l", [tile(RB, [DM], BF16) for _ in range(2)])
            x1Tl_ring = Ring("x1Tl", [tile(RB, [8, 128], BF16) for _ in range(1)])
            st_ring = Ring("st", [tile(RB, [8]) for _ in range(2)])
            sm_ring = Ring("sm", [tile(RB, [128]) for _ in range(2)])
            pm2_ring = Ring("pm2", [psum[:, 0:1024], psum[:, 1024:2048]])
            pm2_bufs = [[BANKB[0], BANKB[1]], [BANKB[2], BANKB[3]]]
            pth_b = [BANKB[4], BANKB[5]]
            ptl_b = [BANKB[6], BANKB[7]]
            pth = psum[:, 2048:3072]
            ptl = psum[:, 3072:4096]

            for g in range(8):
                WLb.load(D["w_out"], g * 128, dest=(wout[:, :, g * 128:(g + 1) * 128], B_wout), cast_eng="scalar")
            S.dma("sync", lambda h: h.dma_start(out=ln1gb, in_=D["ln1"].partition_broadcast(128)), writes=[B_ln1])
            S.dma("sync", lambda h: h.dma_start(out=brb, in_=D["b_r"].partition_broadcast(128)), writes=[B_wr])
            S.dma("sync", lambda h: h.dma_start(out=wrf, in_=D["w_r"].rearrange("(c p) n -> p c n", p=128)), writes=[B_wr])
            S.op("vector", lambda h: h.tensor_copy(out=wrh, in_=wrf), reads=[B_wr], writes=[B_wr])
            S.op("vector", lambda h: h.tensor_tensor(out=wrl, in0=wrf, in1=wrh, op=ALU.subtract), reads=[B_wr], writes=[B_wr])

            x1h_of = {}

            def p3b_A(t, k):
                xt, xtb = xt_ring.next()
                S.dma("sync", lambda h: h.dma_start(out=xt, in_=D["x"][t * 128:(t + 1) * 128, :]), writes=[xtb])
                psm = pm2_ring.views[k % 2]
                pmb = pm2_bufs[k % 2]
                r, rb = r_ring.next()
                for half in range(2):
                    for c in range(8):
                        S.op("tensor", lambda h, c=c, half=half: mm_(h, psm[:, half * 512:(half + 1) * 512], lhsT=mergedT[:, c, t * 128:(t + 1) * 128],
                                                                     rhs=wout[:, c, half * 512:(half + 1) * 512], start=(c == 0), stop=(c == 7)),
                             reads=[B_mT, B_wout], writes=[pmb[half]])
                    S.op("vector", lambda h, half=half: h.scalar_tensor_tensor(
                        out=r[:, half * 512:(half + 1) * 512], in0=xt[:, half * 512:(half + 1) * 512], scalar=ALPHA,
                        in1=psm[:, half * 512:(half + 1) * 512], op0=ALU.mult, op1=ALU.add), reads=[xtb, pmb[half]], writes=[rb])
                    yield
                x1, x1b = x1_ring.next()
                st_, stb = st_ring.next()
                S.op("vector", lambda h: h.memset(st_[:, 0:2], 0.0), writes=[stb])
                S.op("scalar", lambda h: h.activation(out=x1, in_=r, func=AF.Identity, accum_out=st_[:, 0:1]), reads=[rb, stb], writes=[x1b, stb])
                yield
                S.op("scalar", lambda h: h.activation(out=x1, in_=r, func=AF.Square, accum_out=st_[:, 1:2]), reads=[rb, stb], writes=[x1b, stb])
                yield
                S.op("vector", lambda h: h.tensor_scalar(out=st_[:, 2:3], in0=st_[:, 0:1], scalar1=1.0 / DM, scalar2=None, op0=ALU.mult), reads=[stb], writes=[stb])
                S.op("vector", lambda h: h.tensor_tensor(out=st_[:, 3:4], in0=st_[:, 2:3], in1=st_[:, 2:3], op=ALU.mult), reads=[stb], writes=[stb])
                yield
                S.op("vector", lambda h: h.scalar_tensor_tensor(out=st_[:, 4:5], in0=st_[:, 1:2], scalar=1.0 / DM, in1=st_[:, 3:4], op0=ALU.mult, op1=ALU.subtract),
                     reads=[stb], writes=[stb])
                S.op("scalar", lambda h: h.activation(out=st_[:, 5:6], in_=st_[:, 4:5], func=AF.Sqrt, bias=LN_EPS), reads=[stb], writes=[stb])
                yield
                S.op("vector", lambda h: h.reciprocal(out=st_[:, 6:7], in_=st_[:, 5:6]), reads=[stb], writes=[stb])
                S.op("vector", lambda h: h.scalar_tensor_tensor(out=st_[:, 7:8], in0=st_[:, 2:3], scalar=-1.0, in1=st_[:, 6:7], op0=ALU.mult, op1=ALU.mult),
                     reads=[stb], writes=[stb])
                yield
                S.op("scalar", lambda h: h.activation(out=x1, in_=r, func=AF.Identity, scale=st_[:, 6:7], bias=st_[:, 7:8]), reads=[rb, stb], writes=[x1b])
                yield
                S.op("vector", lambda h: h.tensor_tensor(out=x1, in0=x1, in1=ln1gb[:, 0:DM], op=ALU.mult), reads=[x1b, B_ln1], writes=[x1b])
                yield
                S.op("vector", lambda h: h.tensor_tensor(out=x1, in0=x1, in1=ln1gb[:, DM:2 * DM], op=ALU.add), reads=[x1b, B_ln1], writes=[x1b])
                yield
                S.op("scalar", lambda h: h.mul(out=acc[:, t, :], in_=x1, mul=ALPHA), reads=[x1b], writes=[B_acct[t]])
                x1h, x1hb = x1h_ring.next()
                x1l, x1lb = x1l_ring.next()
                S.op("scalar", lambda h: h.copy(out=x1h, in_=x1), reads=[x1b], writes=[x1hb])
                yield
                S.op("vector", lambda h: h.tensor_tensor(out=x1l, in0=x1, in1=x1h, op=ALU.subtract), reads=[x1b, x1hb], writes=[x1lb])
                x1h_of[t] = (x1h, x1hb, x1l, x1lb)
                yield

            def p3b_B(t):
                x1h, x1hb, x1l, x1lb = x1h_of[t]
                for c in range(8):
                    S.op("tensor", lambda h, c=c: mm_(h, pth[:, c * 128:(c + 1) * 128], lhsT=x1h[:, c * 128:(c + 1) * 128], rhs=identb, start=True, stop=True),
                         reads=[x1hb, B_cb], writes=[pth_b[c // 4]])
                for hb in range(2):
                    S.op("scalar", lambda h, hb=hb: h.copy(out=x1T[:, hb * 4:(hb + 1) * 4, t * 128:(t + 1) * 128], in_=v3(pth[:, hb * 512:(hb + 1) * 512], 4)),
                         reads=[pth_b[hb]], writes=[B_x1T])
                yield
                for c in range(8):
                    S.op("tensor", lambda h, c=c: mm_(h, ptl[:, c * 128:(c + 1) * 128], lhsT=x1l[:, c * 128:(c + 1) * 128], rhs=identb, start=True, stop=True),
                         reads=[x1lb, B_cb], writes=[ptl_b[c // 4]])
                x1Tl, x1Tlb = x1Tl_ring.next()
                for hb in range(2):
                    S.op("vector", lambda h, hb=hb: h.tensor_copy(out=x1Tl[:, hb * 4:(hb + 1) * 4, :], in_=v3(ptl[:, hb * 512:(hb + 1) * 512], 4)),
                         reads=[ptl_b[hb]], writes=[x1Tlb])
                yield
                psr = ptl[:, 512:548]
                n_ = 0
                for c in range(8):
                    for (lh, rh, lb_) in ((x1T[:, c, t * 128:(t + 1) * 128], wrh[:, c, :], B_x1T), (x1T[:, c, t * 128:(t + 1) * 128], wrl[:, c, :], B_x1T),
                                          (x1Tl[:, c, :], wrh[:, c, :], x1Tlb)):
                        S.op("tensor", lambda h, lh=lh, rh=rh, n_=n_: mm_(h, psr, lhsT=lh, rhs=rh, start=(n_ == 0), stop=(n_ == 23)),
                             reads=[lb_, B_wr], writes=[ptl_b[1]])
                        n_ += 1
                yield
                sm, smb = sm_ring.next()
                R_ = [smb]
                lg = sm[:, 0:36]
                S.op("vector", lambda h: h.tensor_tensor(out=lg, in0=psr, in1=brb, op=ALU.add), reads=[ptl_b[1], B_wr], writes=R_)
                m_, negm, eg, sgs, gp, og = sm[:, 36:37], sm[:, 37:38], sm[:, 38:42], sm[:, 42:43], sm[:, 43:44], sm[:, 44:48]
                tmp32, sel, m1, mask1 = sm[:, 48:80], sm[:, 80:88], sm[:, 88:89], sm[:, 89:97]
                sel2, m2, mask2, dif, e21, den, p1, p2, c8 = (sm[:, 97:105], sm[:, 105:106], sm[:, 106:114], sm[:, 114:115], sm[:, 115:116],
                                                               sm[:, 116:117], sm[:, 117:118], sm[:, 118:119], sm[:, 119:127])
                V = lambda fn: S.op("vector", fn, reads=R_, writes=R_)
                A_ = lambda fn: S.op("scalar", fn, reads=R_, writes=R_)
                V(lambda h: h.tensor_reduce(out=m_, in_=lg[:, 0:4], axis=AX.X, op=ALU.max))
                V(lambda h: h.tensor_scalar(out=negm, in0=m_, scalar1=-1.0, scalar2=None, op0=ALU.mult))
                V(lambda h: h.memset(sgs, 0.0))
                yield
                A_(lambda h: h.activation(out=eg, in_=lg[:, 0:4], func=AF.Exp, bias=negm, accum_out=sgs))
                yield
                V(lambda h: h.reciprocal(out=gp, in_=sgs))
                V(lambda h: h.tensor_scalar(out=og, in0=lg[:, 0:4], scalar1=m_, scalar2=None, op0=ALU.is_equal))
                yield
                V(lambda h: h.tensor_tensor(out=v3(tmp32, 4), in0=v3(lg[:, 4:36], 4), in1=og.unsqueeze(2).broadcast_to([128, 4, 8]), op=ALU.mult))
                V(lambda h: h.tensor_reduce(out=sel, in_=tmp32.rearrange("p (g e) -> p e g", g=4), axis=AX.X, op=ALU.add))
                yield
                V(lambda h: h.tensor_reduce(out=m1, in_=sel, axis=AX.X, op=ALU.max))
                V(lambda h: h.tensor_scalar(out=mask1, in0=sel, scalar1=m1, scalar2=None, op0=ALU.is_equal))
                yield
                V(lambda h: h.scalar_tensor_tensor(out=sel2, in0=mask1, scalar=-1e30, in1=sel, op0=ALU.mult, op1=ALU.add))
                V(lambda h: h.tensor_reduce(out=m2, in_=sel2, axis=AX.X, op=ALU.max))
                yield
                V(lambda h: h.tensor_scalar(out=mask2, in0=sel2, scalar1=m2, scalar2=None, op0=ALU.is_equal))
                V(lambda h: h.tensor_tensor(out=dif, in0=m2, in1=m1, op=ALU.subtract))
                yield
                A_(lambda h: h.activation(out=e21, in_=dif, func=AF.Exp))
                yield
                V(lambda h: h.tensor_scalar(out=den, in0=e21, scalar1=1.0, scalar2=None, op0=ALU.add))
                V(lambda h: h.reciprocal(out=p1, in_=den))
                yield
                V(lambda h: h.tensor_tensor(out=p2, in0=e21, in1=p1, op=ALU.mult))
                V(lambda h: h.tensor_tensor(out=p1, in0=p1, in1=gp, op=ALU.mult))
                yield
                V(lambda h: h.tensor_tensor(out=p2, in0=p2, in1=gp, op=ALU.mult))
                V(lambda h: h.tensor_scalar(out=c8, in0=mask1, scalar1=p1, scalar2=None, op0=ALU.mult))
                yield
                V(lambda h: h.scalar_tensor_tensor(out=c8, in0=mask2, scalar=p2, in1=c8, op0=ALU.mult, op1=ALU.add))
                V(lambda h: h.tensor_copy(out=v3(tmp32, 4), in_=c8.unsqueeze(1).broadcast_to([128, 4, 8])))
                yield
                S.op("vector", lambda h: h.tensor_tensor(out=v3(comb[:, t, :], 4), in0=v3(tmp32, 4), in1=og.unsqueeze(2).broadcast_to([128, 4, 8]), op=ALU.mult),
                     reads=R_, writes=[B_comb])
                yield

            def drive2(gens):
                live = list(gens)
                while live:
                    for g_ in list(live):
                        try:
                            next(g_)
                        except StopIteration:
                            live.remove(g_)

            for t in range(17):
                gens = []
                if t < 16:
                    gens.append(p3b_A(t, t))
                if t >= 1:
                    gens.append(p3b_B(t - 1))
                drive2(gens)
            if "dbg_x1" in D:
                S.dma("sync", lambda h: h.dma_start(out=D["dbg_x1"], in_=acc), reads=B_acct, writes=[Buf("dbg4")])
            if "dbg_comb" in D:
                S.dma("sync", lambda h: h.dma_start(out=D["dbg_comb"], in_=comb), reads=[B_comb], writes=[Buf("dbg5")])
            S.barrier()

        if stage >= 7:
            RE = Region([(108, 207)])
            NSLOT = 4
            wgu_v = [tile(RE, [8, 512], BF16) for _ in range(NSLOT)]
            wdn_v = [tile(RE, [2, DM], BF16) for _ in range(NSLOT)]
            wexp_b = [Buf(f"wexp{i}") for i in range(NSLOT)]
            stg_ring = Ring("stg", [tile(RE, [2048]) for _ in range(2)])
            ytmp_ring = Ring("ytmp", [tile(RE, [DM]) for _ in range(3)])
            sgm_ring = Ring("sgm", [tile(RE, [256]) for _ in range(3)])
            hid_ring = Ring("hid", [tile(RE, [256], BF16) for _ in range(3)])
            hidT_ring = Ring("hidT", [tile(RE, [2, 128], BF16) for _ in range(3)])
            ln2gb = tile(RE, [2 * DM]); B_ln2 = Buf("ln2gb")
            outst_ring = Ring("outst", [tile(RE, [DM]) for _ in range(2)])
            st2_ring = Ring("st2", [tile(RE, [8]) for _ in range(2)])
            ph_ring = bank_ring("ph", [0, 1, 2])
            pT2_ring = Ring("pT2", [bank(3, 256, 0), bank(3, 256, 256)])
            pT2_ring.bufs = [BANKB[3], BANKB[3]]
            y_views = [psum[:, 2048:3072], psum[:, 3072:4096]]
            y_bufs = [[BANKB[4], BANKB[5]], [BANKB[6], BANKB[7]]]
            S.dma("sync", lambda h: h.dma_start(out=ln2gb, in_=D["ln2"].partition_broadcast(128)), writes=[B_ln2])

            def load_expert(e, slot):
                wb_ = wexp_b[slot]
                if os.environ.get("MOE_NODMA", "") == "1" and e >= 4:
                    return
                for half in range(2):
                    sv, sb_ = stg_ring.next()
                    S.dma("sync", lambda h, sv=sv, half=half: h.dma_start(
                        out=v3(sv, 8), in_=D["w_gu"][e, :, half * 256:(half + 1) * 256].rearrange("(c p) n -> p c n", p=128)), writes=[sb_])
                    S.op("gpsimd", lambda h, sv=sv, half=half: h.tensor_copy(out=wgu_v[slot][:, :, half * 256:(half + 1) * 256], in_=v3(sv, 8)),
                         reads=[sb_], writes=[wb_])
                sv, sb_ = stg_ring.next()
                S.dma("sync", lambda h, sv=sv: h.dma_start(out=v3(sv, 2), in_=D["w_dn"][e].rearrange("(c p) n -> p c n", p=128)), writes=[sb_])
                S.op("gpsimd", lambda h, sv=sv: h.tensor_copy(out=wdn_v[slot], in_=v3(sv, 2)), reads=[sb_], writes=[wb_])

            def hgu(t, e, slot):
                ps, pb = ph_ring.next()
                for c in range(8):
                    S.op("tensor", lambda h, c=c: mm_(h, ps, lhsT=x1T[:, c, t * 128:(t + 1) * 128], rhs=wgu_v[slot][:, c, :],
                                                      start=(c == 0), stop=(c == 7)), reads=[B_x1T, wexp_b[slot]], writes=[pb])
                return ps, pb

            def stage_b(t, e, ps, pb):
                sg, sgb = sgm_ring.next()
                S.op("scalar", lambda h: h.activation(out=sg, in_=ps[:, 0:256], func=AF.Silu), reads=[pb], writes=[sgb])
                hid, hidb = hid_ring.next()
                S.op("vector", lambda h: h.scalar_tensor_tensor(out=hid, in0=ps[:, 256:512], scalar=comb[:, t, e:e + 1], in1=sg,
                                                                op0=ALU.mult, op1=ALU.mult), reads=[pb, B_comb, sgb], writes=[hidb])
                pT, pTb = pT2_ring.next()
                for f in range(2):
                    S.op("tensor", lambda h, f=f: mm_(h, pT[:, f * 128:(f + 1) * 128], lhsT=hid[:, f * 128:(f + 1) * 128], rhs=identb, start=True, stop=True),
                         reads=[hidb, B_cb], writes=[pTb])
                hT, hTb = hidT_ring.next()
                S.op("scalar", lambda h: h.copy(out=hT, in_=v3(pT[:, 0:256], 2)), reads=[pTb], writes=[hTb])
                return hT, hTb

            def stage_c(slot, hT, hTb, yv, yb, first, last):
                for half in range(2):
                    for f in range(2):
                        S.op("tensor", lambda h, f=f, half=half: mm_(h, yv[:, half * 512:(half + 1) * 512], lhsT=hT[:, f, :],
                                                                     rhs=wdn_v[slot][:, f, half * 512:(half + 1) * 512],
                                                                     start=(first and f == 0), stop=(last and f == 1)),
                             reads=[hTb, wexp_b[slot]], writes=[yb[half]])

            def flush_copy(t, yv, yb):
                yt, ytb = ytmp_ring.next()
                S.op("scalar", lambda h: h.copy(out=yt[:, 0:512], in_=yv[:, 0:512]), reads=[yb[0]], writes=[ytb])
                S.op("vector", lambda h: h.tensor_copy(out=yt[:, 512:1024], in_=yv[:, 512:1024]), reads=[yb[1]], writes=[ytb])
                return t, yt, ytb

            def flush_add(t, yt, ytb):
                S.op("vector", lambda h: h.tensor_tensor(out=acc[:, t, :], in0=acc[:, t, :], in1=yt, op=ALU.add), reads=[ytb, B_acct[t]], writes=[B_acct[t]])

            def final_tile(t):
                ot_, otb_ = outst_ring.next()
                st_, stb = st2_ring.next()
                layer_norm_tile(acc[:, t, :], ln2gb, ot_, st_, ot_, [B_acct[t]], B_ln2, otb_, stb, otb_)
                S.dma("sync", lambda h: h.dma_start(out=D["out"][t * 128:(t + 1) * 128, :], in_=ot_), reads=[otb_], writes=[Buf(f"outd{t}")], sem_buf=otb_)

            NE = int(os.environ.get("N_EXPERTS", "32"))
            load_expert(0, 0)
            load_expert(1, 1)
            items = [(grp, t, j) for grp in range(NE // 2) for t in range(16) for j in range(2)]
            n_it = len(items)
            Hs, Ts = {}, {}
            yk = 0
            pending = []
            pending2 = []
            fin_cnt = [0] * 16
            for step in range(n_it + 4):
                if step < n_it:
                    grp, t_, j_ = items[step]
                    if t_ == 2 and j_ == 0 and grp + 1 < NE // 2:
                        load_expert(2 * grp + 2, (2 * grp + 2) % NSLOT)
                        load_expert(2 * grp + 3, (2 * grp + 3) % NSLOT)
                    Hs[step] = hgu(t_, 2 * grp + j_, (2 * grp + j_) % NSLOT)
                i1_ = step - 1
                if 0 <= i1_ < n_it:
                    grp, t_, j_ = items[i1_]
                    Ts[i1_] = stage_b(t_, 2 * grp + j_, Hs[i1_][0], Hs[i1_][1])
                    del Hs[i1_]
                for args in pending2:
                    flush_add(*args)
                    fin_cnt[args[0]] += 1
                    if fin_cnt[args[0]] == NE // 2:
                        final_tile(args[0])
                pending2 = [flush_copy(*args) for args in pending]
                pending = []
                i2_ = step - 2
                if 0 <= i2_ < n_it:
                    grp, t_, j_ = items[i2_]
                    yv, yb = y_views[yk % 2], y_bufs[yk % 2]
                    stage_c((2 * grp + j_) % NSLOT, Ts[i2_][0], Ts[i2_][1], yv, yb, j_ == 0, j_ == 1)
                    del Ts[i2_]
                    if j_ == 1:
                        pending.append((t_, yv, yb))
                        yk += 1

            S.barrier()

        build_program.last_sched = S
        S.emit()
    return nc


def make_in_maps(inputs):
    f = lambda a: np.ascontiguousarray(np.asarray(a, dtype=np.float32))
    x = f(inputs["x"])
    w_in = f(inputs["w_in"])[0]
    shared = {
        "w_in": w_in,
        "nab": na_bias_tiles(f(inputs["na_rpb"])[0]),
        "consts": np.ascontiguousarray(CONST_ARR),
        "convT": np.ascontiguousarray(f(inputs["dn_conv_w"])[0].T.reshape(12, 128, 5).transpose(1, 0, 2).reshape(128, 60)),
        "gvec": np.concatenate([f(inputs["dn_a_log_f"])[0], f(inputs["dn_a_log_b"])[0],
                                f(inputs["dn_dt_bias_f"])[0], f(inputs["dn_dt_bias_b"])[0]])[None, :].copy(),
        "norm_w": f(inputs["dn_norm_w"]).reshape(1, 64),
        "w_proj_na": f(inputs["w_proj_na"])[0], "w_proj_dn": f(inputs["w_proj_dn"])[0], "w_out": f(inputs["w_out"])[0],
        "ln1": np.concatenate([f(inputs["ln1_g"])[0], f(inputs["ln1_b"])[0]])[None, :].copy(),
        "ln2": np.concatenate([f(inputs["ln2_g"])[0], f(inputs["ln2_b"])[0]])[None, :].copy(),
        "w_r": np.ascontiguousarray(np.concatenate([f(inputs["w_router_group"])[0], f(inputs["w_router_expert"])[0]], axis=1)),
        "b_r": np.concatenate([f(inputs["b_router_group"])[0], f(inputs["b_router_expert"])[0]])[None, :].copy(),
        "w_gu": f(inputs["w_expert_gate_up"])[0], "w_dn": f(inputs["w_expert_down"])[0],
    }
    maps = []
    for b in range(8):
        m = dict(shared)
        m["x"] = np.ascontiguousarray(x[b])
        m["xT"] = np.ascontiguousarray(x[b].T)
        maps.append(m)
    return maps


def kernel(**inputs):
    nc = build_program()
    in_maps = make_in_maps(inputs)
    res = run_bass_kernel_spmd(nc, in_maps, core_ids=list(range(8)))
    return np.stack([np.asarray(r["out"], dtype=np.float32) for r in res.results], axis=0)
```

```python
from contextlib import ExitStack
import numpy as np
import concourse.bass as bass
import concourse.mybir as mybir
from concourse.bass_utils import run_bass_kernel_spmd

F32 = mybir.dt.float32
BF16 = mybir.dt.bfloat16
AF = mybir.ActivationFunctionType
ALU = mybir.AluOpType
AX = mybir.AxisListType

ENGS = ("tensor", "vector", "scalar", "gpsimd", "sync")
SAME_ENGINE_SYNC = True
NEG = -30000.0
T = 2048
DM = 1024
ARENA_KB = 207


class Buf:
    __slots__ = ("name", "w", "r", "dsem", "dcnt", "excl")

    def __init__(self, name, excl=False):
        self.name = name
        self.excl = excl
        self.w = None
        self.r = []
        self.dsem = None
        self.dcnt = 0


class Op:
    __slots__ = ("eng", "idx", "fn", "waits", "signal", "sigval", "dma", "dsem", "dval")

    def __init__(self, eng, idx, fn):
        self.eng = eng
        self.idx = idx
        self.fn = fn
        self.waits = []
        self.signal = False
        self.sigval = None
        self.dma = False
        self.dsem = None
        self.dval = 0


class Sched:
    def __init__(self, nc, stack):
        self.nc = nc
        self.stack = stack
        self.ops = {e: [] for e in ENGS}
        self.seen = {e: {p: -1 for p in ENGS} for e in ENGS}
        self.seen_dma = {e: {} for e in ENGS}
        self.nsem = 0

    def new_sem(self, name):
        self.nsem += 1
        return self.stack.enter_context(self.nc.semaphore(f"{name}_{self.nsem}"))

    def _dep(self, op, dep):
        if dep is None or dep is op:
            return
        if dep.dma:
            key = id(dep.dsem)
            if self.seen_dma[op.eng].get(key, 0) >= dep.dval:
                return
            self.seen_dma[op.eng][key] = dep.dval
            op.waits.append(dep)
            return
        if dep.eng == op.eng:
            if dep.eng == "tensor" or not SAME_ENGINE_SYNC:
                return
        if self.seen[op.eng][dep.eng] >= dep.idx:
            return
        self.seen[op.eng][dep.eng] = dep.idx
        dep.signal = True
        op.waits.append(dep)

    def _deps(self, o, deps):
        best = {}
        for dep in deps:
            if dep is None or dep is o:
                continue
            key = ("d", id(dep.dsem)) if dep.dma else ("e", dep.eng)
            val = dep.dval if dep.dma else dep.idx
            if key not in best or val > best[key][0]:
                best[key] = (val, dep)
        for _, dep in best.values():
            self._dep(o, dep)

    def op(self, eng, fn, reads=(), writes=()):
        lst = self.ops[eng]
        o = Op(eng, len(lst), fn)
        ex = [b for b in reads if b.excl and b not in writes]
        if ex:
            reads = [b for b in reads if not b.excl]
            writes = list(writes) + ex
        deps = [b.w for b in reads]
        for b in writes:
            deps.append(b.w)
            deps.extend(b.r)
        self._deps(o, deps)
        for b in reads:
            b.r.append(o)
        for b in writes:
            b.w = o
            b.r = []
        lst.append(o)
        return o

    def dma(self, eng, fn, reads=(), writes=(), sem_buf=None):
        lst = self.ops[eng]
        o = Op(eng, len(lst), fn)
        o.dma = True
        sb = sem_buf or (writes[0] if writes else reads[0])
        if sb.dsem is None:
            sb.dsem = self.new_sem("d_" + sb.name)
        deps = [b.w for b in reads]
        for b in writes:
            if b.w is not None and not (b.w.dma and b.w.dsem is sb.dsem):
                deps.append(b.w)
            deps.extend(b.r)
        self._deps(o, deps)
        sb.dcnt += 16
        o.dsem = sb.dsem
        o.dval = sb.dcnt
        for b in reads:
            b.r.append(o)
        for b in writes:
            b.w = o
            b.r = []
        lst.append(o)
        return o

    def barrier(self):
        lasts = []
        for e in ENGS:
            for o in reversed(self.ops[e]):
                if not o.dma:
                    lasts.append(o)
                    break
        latest = {}
        for e in ENGS:
            for o in self.ops[e]:
                if o.dma:
                    latest[id(o.dsem)] = o
        for e in ENGS:
            o = Op(e, len(self.ops[e]), None)
            for d in lasts:
                self._dep(o, d)
            for d in latest.values():
                self._dep(o, d)
            self.ops[e].append(o)

    def emit(self):
        nc = self.nc
        CH = 1000
        esems = {}
        for e in ENGS:
            n = sum(1 for o in self.ops[e] if o.signal)
            esems[e] = [self.new_sem(f"e_{e}_{i}") for i in range(n // CH + 1)]
            c = 0
            for o in self.ops[e]:
                if o.signal:
                    o.sigval = c
                    c += 1

        def run(e, h):
            for o in self.ops[e]:
                for d in o.waits:
                    if d.dma:
                        h.wait_ge(d.dsem, d.dval)
                    else:
                        h.wait_ge(esems[d.eng][d.sigval // CH], d.sigval % CH + 1)
                if o.fn is None:
                    if o.signal:
                        h.nop().then_inc(esems[e][o.sigval // CH], 1)
                    continue
                ins = o.fn(h)
                if o.dma:
                    ins.then_inc(o.dsem, 16)
                elif o.signal:
                    ins.then_inc(esems[e][o.sigval // CH], 1)

        with nc.Block() as block:
            @block.tensor
            def _(h):
                run("tensor", h)

            @block.vector
            def _(h):
                run("vector", h)

            @block.scalar
            def _(h):
                run("scalar", h)

            @block.gpsimd
            def _(h):
                run("gpsimd", h)

            @block.sync
            def _(h):
                run("sync", h)


class Ring:
    def __init__(self, name, views):
        self.views = views
        self.bufs = [Buf(f"{name}{i}") for i in range(len(views))]
        self.i = 0

    def next(self):
        k = self.i % len(self.views)
        self.i += 1
        return self.views[k], self.bufs[k]


def _rs(r):
    return min(max(r - 4, 0), 24)


def na_blocks(i):
    res = []
    for j in range(16):
        valid = [[_rs(2 * i + b) <= 2 * j + a < _rs(2 * i + b) + 8 for b in (0, 1)] for a in (0, 1)]
        if not (valid[0][0] or valid[0][1] or valid[1][0] or valid[1][1]):
            continue
        delta = j - i
        if all(valid[0]) and all(valid[1]):
            var = delta + 3
        elif delta == -2 and valid == [[True, False], [True, True]]:
            var = 7
        elif delta == 2 and valid == [[False, True], [False, False]]:
            var = 8
        else:
            raise AssertionError((i, j, valid))
        assert 0 <= var < 9
        res.append((j, var))
    return res


def na_bias_tiles(rpb):
    c = np.arange(64)
    col_start = np.clip(c - 8, 0, 48)
    col_mask = (c[None, :] >= col_start[:, None]) & (c[None, :] < col_start[:, None] + 16)
    dc_idx = np.clip(c[None, :] - c[:, None], -15, 15) + 15
    tiles = np.full((8, 9, 128, 128), NEG, np.float32)
    for v in range(9):
        for a in (0, 1):
            for b in (0, 1):
                if v < 7:
                    delta, ok = v - 3, True
                elif v == 7:
                    delta, ok = -2, not (a == 0 and b == 1)
                else:
                    delta, ok = 2, (a == 0 and b == 1)
                dr = 2 * delta + a - b + 7
                if not ok or not (0 <= dr <= 14):
                    continue
                vals = rpb[:, dr][:, dc_idx]
                vals = np.where(col_mask[None], vals, np.float32(NEG))
                tiles[:, v, a * 64:(a + 1) * 64, b * 64:(b + 1) * 64] = vals.transpose(0, 2, 1)
    return np.ascontiguousarray(tiles.transpose(2, 0, 1, 3)).reshape(128, 72 * 128)


def dn_consts():
    p = np.arange(128)
    same = (p[:, None] // 64) == (p[None, :] // 64)
    A1 = (same & (p[:, None] <= p[None, :])).astype(np.float32)
    A2 = (same & (p[:, None] > p[None, :])).astype(np.float32)
    ones = same.astype(np.float32)
    out = {
        "A1": A1, "A2": A2, "A1T": A1.T.copy(), "A2T": A2.T.copy(),
        "C3f": ones - A1, "C3b": ones - A1.T,
        "HA0": np.repeat((p < 64).astype(np.float32)[:, None], 128, 1),
        "HA1": np.repeat((p >= 64).astype(np.float32)[:, None], 128, 1),
        "NEGSf": np.where(same & (p[None, :] < p[:, None]), 0.0, NEG).astype(np.float32),
        "NEG2f": np.where(same & (p[:, None] <= p[None, :]), 0.0, NEG).astype(np.float32),
        "NEGSb": np.where(same & (p[None, :] > p[:, None]), 0.0, NEG).astype(np.float32),
        "NEG2b": np.where(same & (p[:, None] >= p[None, :]), 0.0, NEG).astype(np.float32),
        "IDENT": np.eye(128, dtype=np.float32),
        "H": ones,
    }
    names = ["A1", "A2", "A1T", "A2T", "C3f", "C3b", "HA0", "HA1", "NEGSf", "NEG2f", "NEGSb", "NEG2b", "IDENT", "H"]
    return names, np.concatenate([out[n] for n in names], axis=1)


CONST_NAMES, CONST_ARR = dn_consts()
NCONST = len(CONST_NAMES)

OFF_QNA, OFF_KNA, OFF_VNA = 0, 512, 1024
OFF_QDN, OFF_KDN, OFF_VDN, OFF_ZDN = 1536, 2048, 2560, 3072
OFF_G = 3584
OFF_GNA, OFF_GDN = 3616, 4640


class Region:
    def __init__(self, ranges):
        self.free = [[int(a * 1024), int(b * 1024)] for a, b in ranges]

    def alloc(self, nbytes):
        nbytes = (nbytes + 63) // 64 * 64
        for r in self.free:
            if r[1] - r[0] >= nbytes:
                off = r[0]
                r[0] += nbytes
                return off
        raise MemoryError(f"arena region exhausted: need {nbytes}, free {self.free}")


import os
LP = int(os.environ.get("LOCAL_PARTS", "9"))
SP = int(os.environ.get("SCAN_PARTS", "9"))
def mm_(h, out, **kw):
    return h.matmul(out, skip_group_check=True, **kw)


ALPHA = 2.0 ** 0.25
LN_EPS = 1e-5
RMS_EPS = 1e-6


def build_program(stage=99, dbg=()):
    nc = bass.Bass("TRN2", target_bir_lowering=False)
    D = {}

    def din(name, shape, dt=F32):
        D[name] = nc.dram_tensor(name, list(shape), dt, kind="ExternalInput").ap()

    def dout(name, shape, dt=F32):
        D[name] = nc.dram_tensor(name, list(shape), dt, kind="ExternalOutput").ap()

    din("x", [T, DM]); din("xT", [DM, T]); din("w_in", [DM, 5664]); din("nab", [128, 72 * 128])
    din("consts", [128, NCONST * 128])
    din("convT", [128, 12 * 5]); din("gvec", [1, 32]); din("norm_w", [1, 64])
    din("w_proj_na", [512, DM]); din("w_proj_dn", [512, DM]); din("w_out", [DM, DM])
    din("ln1", [1, 2 * DM]); din("ln2", [1, 2 * DM])
    din("w_r", [DM, 36]); din("b_r", [1, 36])
    din("w_gu", [32, DM, 512]); din("w_dn", [32, 256, DM])
    dout("out", [T, DM])
    for name, shape, dt in dbg:
        dout(name, shape, dt)

    with ExitStack() as st:
        S = Sched(nc, st)
        arena = st.enter_context(nc.sbuf_tensor("arena", [128, ARENA_KB * 256], F32))
        psum = st.enter_context(nc.psum_tensor("psum", [128, 4096], F32))

        def tile(reg, dims, dt=F32):
            n = 1
            for d_ in dims:
                n *= d_
            nbytes = n * (4 if dt == F32 else 2)
            off = reg.alloc(nbytes)
            v = arena[:, off // 4:(off + nbytes + 3) // 4]
            if dt != F32:
                v = v.bitcast(dt)[:, 0:n]
            if len(dims) > 1:
                names = " ".join(f"d{i}" for i in range(len(dims)))
                kw = {f"d{i}": dims[i] for i in range(1, len(dims))}
                v = v.rearrange(f"p ({names}) -> p {names}", **kw)
            return v

        def bank(b, n=512, off=0):
            return psum[:, b * 512 + off:b * 512 + off + n]

        BANKB = [Buf(f"bank{b}", excl=True) for b in range(8)]

        def bank_ring(name, banks):
            r = Ring(name, [bank(b) for b in banks])
            r.bufs = [BANKB[b] for b in banks]
            return r

        def v3(ap, h):
            return ap.rearrange("p (h j) -> p h j", h=h)

        R0 = Region([(0, 10)])
        RN = Region([(26, 206)])
        xT = tile(RN, [8, T], BF16)
        cst = tile(RN, [NCONST * 128])
        B_cst = Buf("cst")
        S.dma("sync", lambda h: h.dma_start(out=cst, in_=D["consts"]), writes=[B_cst])
        cb = tile(R0, [NCONST * 128], BF16)
        identf = tile(R0, [128])
        B_cb = Buf("cb")
        S.op("vector", lambda h: h.tensor_copy(out=cb, in_=cst), reads=[B_cst], writes=[B_cb])
        i_id = CONST_NAMES.index("IDENT")
        S.op("vector", lambda h: h.tensor_copy(out=identf, in_=cst[:, i_id * 128:(i_id + 1) * 128]), reads=[B_cst], writes=[B_cb])
        CBn = {n: cb[:, i * 128:(i + 1) * 128] for i, n in enumerate(CONST_NAMES)}
        C = CBn
        B_cst = B_cb
        identb = CBn["IDENT"]

        onaT = tile(Region([(10, 26)]), [4, T], BF16)
        B_onaT = Buf("onaT")

        evac_flip = [0]

        def copy_alt(out, in_, reads, writes, scale=None):
            evac_flip[0] ^= 1
            if evac_flip[0]:
                if scale is None:
                    S.op("scalar", lambda h: h.copy(out=out, in_=in_), reads=reads, writes=writes)
                else:
                    S.op("scalar", lambda h: h.mul(out=out, in_=in_, mul=scale), reads=reads, writes=writes)
            else:
                if scale is None:
                    S.op("vector", lambda h: h.tensor_copy(out=out, in_=in_), reads=reads, writes=writes)
                else:
                    S.op("vector", lambda h: h.tensor_scalar(out=out, in0=in_, scalar1=scale, scalar2=None, op0=ALU.mult),
                         reads=reads, writes=writes)

        class WLoader:
            def __init__(self, reg, nslots=2, ncols=256):
                self.ncols = ncols
                self.wst = Ring("wst", [tile(reg, [8, ncols]) for _ in range(nslots)])
                self.wbf = Ring("wbf", [tile(reg, [8, ncols], BF16) for _ in range(nslots)])

            def load(self, src, col0, ncols=None, kchunks=8, cast_eng="gpsimd", dest=None):
                ncols = ncols or self.ncols
                sv, sb_ = self.wst.next()
                if dest is None:
                    wv, wb = self.wbf.next()
                    wv = wv[:, 0:kchunks, 0:ncols]
                else:
                    wv, wb = dest
                S.dma("sync", lambda h: h.dma_start(out=sv[:, 0:kchunks, 0:ncols],
                                                    in_=src[:, col0:col0 + ncols].rearrange("(c p) n -> p c n", p=128)),
                      writes=[sb_])
                if cast_eng == "scalar":
                    S.op("scalar", lambda h: h.copy(out=wv, in_=sv[:, 0:kchunks, 0:ncols]), reads=[sb_], writes=[wb])
                else:
                    S.op(cast_eng, lambda h: h.tensor_copy(out=wv, in_=sv[:, 0:kchunks, 0:ncols]), reads=[sb_], writes=[wb])
                return wv, wb

        def proj_fm(pring, wv, wb, ncols, evac, act, B_act, kchunks=8):
            for fc in range(ncols // 128):
                for tb in range(4):
                    ps, pb = pring.next()
                    for c in range(kchunks):
                        S.op("tensor", lambda h, ps=ps, c=c, fc=fc, tb=tb: mm_(h,
                            ps, lhsT=wv[:, c, fc * 128:(fc + 1) * 128], rhs=act[:, c, tb * 512:(tb + 1) * 512],
                            start=(c == 0), stop=(c == kchunks - 1)), reads=[wb, B_act], writes=[pb])
                    evac(ps, pb, fc, tb)

        def proj_tm(pring, wv, wb, ncols, evac, act, B_act, kchunks=8):
            for t in range(16):
                ps, pb = pring.next()
                for c in range(kchunks):
                    S.op("tensor", lambda h, ps=ps, c=c, t=t: mm_(h,
                        ps[:, 0:ncols], lhsT=act[:, c, t * 128:(t + 1) * 128], rhs=wv[:, c, 0:ncols],
                        start=(c == 0), stop=(c == kchunks - 1)), reads=[wb, B_act], writes=[pb])
                evac(ps, pb, t)

        def load_xT(xT, B_xT, ring):
            for c in range(8):
                sv, sb_ = ring.next()
                svf = sv.rearrange("p c n -> p (c n)") if len(sv.shape) == 3 else sv
                S.dma("sync", lambda h, svf=svf, c=c: h.dma_start(out=svf[:, 0:T], in_=D["xT"][c * 128:(c + 1) * 128, :]),
                      writes=[sb_])
                if c % 2 == 0:
                    S.op("vector", lambda h, svf=svf, c=c: h.tensor_copy(out=xT[:, c, :], in_=svf[:, 0:T]), reads=[sb_], writes=[B_xT])
                else:
                    S.op("scalar", lambda h, svf=svf, c=c: h.copy(out=xT[:, c, :], in_=svf[:, 0:T]), reads=[sb_], writes=[B_xT])

        B_xT = Buf("xT")
        qnaT = tile(RN, [4, T], BF16)
        knaT = tile(RN, [4, T], BF16)
        B_qnaT, B_knaT = Buf("qnaT"), Buf("knaT")
        vaug = tile(RN, [16, 8, 128], BF16)
        B_vaug = Buf("vaug")
        nab = tile(RN, [8, 9, 128], BF16)
        B_nab = Buf("nab")
        pT_ring = Ring("pT", [tile(RN, [640], BF16) for _ in range(3)])
        rden_ring = Ring("rden", [tile(RN, [128]) for _ in range(2)])
        WL = WLoader(RN)
        xst_ring = Ring("xst", [tile(RN, [T]) for _ in range(2)])
        pin_ring = bank_ring("pin", [0, 1, 2])

        load_xT(xT, B_xT, xst_ring)

        nabf = nab.rearrange("p h v q -> p (h v q)")
        for k in range(6):
            sv, sb_ = WL.wst.next()
            svf = sv.rearrange("p c n -> p (c n)")
            w = 12 * 128
            S.dma("sync", lambda h, svf=svf, k=k, w=w: h.dma_start(out=svf[:, 0:w], in_=D["nab"][:, k * w:(k + 1) * w]), writes=[sb_])
            S.op("gpsimd", lambda h, svf=svf, k=k, w=w: h.tensor_copy(out=nabf[:, k * w:(k + 1) * w], in_=svf[:, 0:w]), reads=[sb_], writes=[B_nab])

        for g in range(2):
            wv, wb = WL.load(D["w_in"], OFF_QNA + g * 256)
            proj_fm(pin_ring, wv, wb, 256, lambda ps, pb, fc, tb, g=g: copy_alt(
                qnaT[:, g * 2 + fc, tb * 512:(tb + 1) * 512], ps, [pb], [B_qnaT], scale=0.125), xT, B_xT)
        for g in range(2):
            wv, wb = WL.load(D["w_in"], OFF_KNA + g * 256)
            proj_fm(pin_ring, wv, wb, 256, lambda ps, pb, fc, tb, g=g: copy_alt(
                knaT[:, g * 2 + fc, tb * 512:(tb + 1) * 512], ps, [pb], [B_knaT]), xT, B_xT)
        S.op("gpsimd", lambda h: h.memset(vaug[:, :, :, 64:128], 1.0), writes=[B_vaug])
        for g in range(2):
            wv, wb = WL.load(D["w_in"], OFF_VNA + g * 256)
            proj_tm(pin_ring, wv, wb, 256, lambda ps, pb, t, g=g: copy_alt(
                vaug[:, t, g * 4:(g + 1) * 4, 0:64], v3(ps[:, 0:256], 4), [pb], [B_vaug]), xT, B_xT)

        sc_ring = Ring("sc", [psum[:, 1024:2048], psum[:, 2048:3072], psum[:, 3072:4096]])
        sc_ring.bufs = [BANKB[2], BANKB[4], BANKB[6]]
        sc_b2 = [BANKB[3], BANKB[5], BANKB[7]]
        po_ring = bank_ring("po", [0, 1])
        def na_scores(i, hd):
            blocks = na_blocks(i)
            nb = len(blocks)
            hc, hp = hd // 2, (hd % 2) * 64
            sc, scb = sc_ring.next()
            scb2 = sc_b2[sc_ring.bufs.index(scb)]
            for bi, (j, var) in enumerate(blocks):
                S.op("tensor", lambda h, bi=bi, j=j: mm_(h,
                    sc[:, bi * 128:(bi + 1) * 128], lhsT=knaT[hp:hp + 64, hc, j * 128:(j + 1) * 128],
                    rhs=qnaT[hp:hp + 64, hc, i * 128:(i + 1) * 128], start=True, stop=False),
                    reads=[B_knaT, B_qnaT], writes=[scb if bi < 4 else scb2])
                S.op("tensor", lambda h, bi=bi, var=var: mm_(h,
                    sc[:, bi * 128:(bi + 1) * 128], lhsT=identb, rhs=nab[:, hd, var, :], start=False, stop=True),
                    reads=[B_cb, B_nab], writes=[scb if bi < 4 else scb2])
            pT, pTb = pT_ring.next()
            n0 = min(nb, 4) * 128
            S.op("scalar", lambda h: h.activation(out=pT[:, 0:n0], in_=sc[:, 0:n0], func=AF.Exp), reads=[scb], writes=[pTb])
            if nb > 4:
                S.op("scalar", lambda h: h.activation(out=pT[:, 512:640], in_=sc[:, 512:640], func=AF.Exp), reads=[scb2], writes=[pTb])
            return blocks, pT, pTb

        def na_pv(i, hd, blocks, pT, pTb):
            nb = len(blocks)
            hc, hp = hd // 2, (hd % 2) * 64
            po, pob = po_ring.next()
            for bi, (j, var) in enumerate(blocks):
                S.op("tensor", lambda h, bi=bi, j=j: mm_(h,
                    po[:, 0:128], lhsT=vaug[:, j, hd, :], rhs=pT[:, bi * 128:(bi + 1) * 128],
                    start=(bi == 0), stop=(bi == nb - 1)), reads=[B_vaug, pTb], writes=[pob])
            rd, rdb = rden_ring.next()
            S.op("vector", lambda h: h.reciprocal(out=rd[64:128, :], in_=po[64:128, 0:128]), reads=[pob], writes=[rdb])
            S.op("vector", lambda h: h.tensor_tensor(
                out=onaT[hp:hp + 64, hc, i * 128:(i + 1) * 128], in0=po[0:64, 0:128], in1=rd[64:128, :], op=ALU.mult),
                reads=[pob, rdb], writes=[B_onaT])

        na_items = [(i, hd) for i in range(16) for hd in range(8)]
        pend = []
        for k_, (i, hd) in enumerate(na_items):
            pend.append((i, hd) + na_scores(i, hd))
            if len(pend) > 1:
                na_pv(*pend.pop(0))
        while pend:
            na_pv(*pend.pop(0))

        if "dbg_onaT" in D:
            S.dma("sync", lambda h: h.dma_start(out=D["dbg_onaT"], in_=onaT), reads=[B_onaT], writes=[Buf("dbg1")])
        S.barrier()

        if stage >= 2:
            RR = Region([(58, 142)])
            RT = Region([(142, 207)])
            qT = tile(RR, [4, T], BF16); kT = tile(RR, [4, T], BF16)
            ktok = tile(RR, [16, 512], BF16); vtok = tile(RR, [16, 512], BF16)
            B_qT, B_kT, B_ktok, B_vtok = Buf("qT"), Buf("kT"), Buf("ktok"), Buf("vtok")
            graw = tile(RR, [16, 32]); B_graw = Buf("graw")
            gv = tile(RR, [32]); nega = tile(RR, [16]); convT = tile(RR, [12, 5])
            tx = tile(RR, [16, 16]); tax = tile(RR, [16, 16]); te = tile(RR, [16, 16]); tsp = tile(RR, [16, 16])
            betat = tile(RR, [16, 16]); gt = tile(RR, [16, 16])
            gD = tile(RR, [2, 128]); betaD = tile(RR, [2, 128]); nbetaD = tile(RR, [2, 128])
            egc = tile(RR, [2, 128]); egd = tile(RR, [2, 128]); bg = tile(RR, [2, 128])
            EGL = tile(RR, [2, 2, 128])
            gDh = tile(RR, [2, 128], BF16); gDl = tile(RR, [2, 128], BF16); gDhf = tile(RR, [2, 128]); gDlf = tile(RR, [2, 128])
            B_g = Buf("gates")
            B_small = Buf("dnsmall")
            WL = WLoader(RT)
            cst_ring = Ring("cst", [tile(RT, [T + 4], BF16) for _ in range(2)])
            diagw = tile(RT, [12, 5, 128], BF16); B_diag = Buf("diagw")
            sil_ring = Ring("sil", [tile(RT, [T], BF16) for _ in range(2)])
            sq_ring = Ring("sq", [tile(RT, [512], BF16) for _ in range(2)])
            tmpf_ring = Ring("tmpf", [tile(RT, [512]) for _ in range(2)])

            S.dma("sync", lambda h: h.dma_start(out=gv, in_=D["gvec"].partition_broadcast(128)), writes=[B_small])
            S.dma("sync", lambda h: h.dma_start(out=convT.rearrange("p a b -> p (a b)"), in_=D["convT"]), writes=[B_small])
            for cs_ in cst_ring.views:
                pass
            for k_, (cs_, csb_) in enumerate(zip(cst_ring.views, cst_ring.bufs)):
                S.op("gpsimd", lambda h, cs_=cs_: h.memset(cs_[:, 0:2], 0.0), writes=[csb_])
                S.op("gpsimd", lambda h, cs_=cs_: h.memset(cs_[:, T + 2:T + 4], 0.0), writes=[csb_])

            wv, wb = WL.load(D["w_in"], OFF_G, ncols=32)
            proj_tm(pin_ring, wv, wb, 32, lambda ps, pb, t: copy_alt(graw[:, t, :], ps[:, 0:32], [pb], [B_graw]), xT, B_xT)
            G_ = [B_graw, B_small, B_g]
            S.op("scalar", lambda h: h.activation(out=betat, in_=graw[:, :, 0:16], func=AF.Sigmoid), reads=G_, writes=[B_g])
            S.op("vector", lambda h: h.tensor_tensor(out=tx, in0=graw[:, :, 16:32],
                                                     in1=gv[:, 16:32].unsqueeze(1).broadcast_to([128, 16, 16]), op=ALU.add),
                 reads=G_, writes=[B_g])
            S.op("vector", lambda h: h.scalar_tensor_tensor(out=tax, in0=tx, scalar=-1.0, in1=tx, op0=ALU.mult, op1=ALU.max), reads=G_, writes=[B_g])
            S.op("scalar", lambda h: h.activation(out=te, in_=tax, func=AF.Exp, scale=-1.0), reads=G_, writes=[B_g])
            S.op("scalar", lambda h: h.activation(out=te, in_=te, func=AF.Ln, bias=1.0), reads=G_, writes=[B_g])
            S.op("vector", lambda h: h.scalar_tensor_tensor(out=tsp, in0=tx, scalar=0.0, in1=te, op0=ALU.max, op1=ALU.add),
                 reads=G_, writes=[B_g])
            S.op("scalar", lambda h: h.activation(out=nega, in_=gv[:, 0:16], func=AF.Exp), reads=G_, writes=[B_g])
            S.op("vector", lambda h: h.tensor_scalar(out=nega, in0=nega, scalar1=-1.0, scalar2=None, op0=ALU.mult), reads=G_, writes=[B_g])
            S.op("vector", lambda h: h.tensor_tensor(out=gt, in0=tsp, in1=nega.unsqueeze(1).broadcast_to([128, 16, 16]), op=ALU.mult),
                 reads=G_, writes=[B_g])
            for d in range(2):
                S.op("vector", lambda h, d=d: h.tensor_copy(out=gD[:, d, :].rearrange("p (t h) -> p t h", t=16),
                                                            in_=gt[:, :, d * 8:(d + 1) * 8]), reads=G_, writes=[B_g])
                S.op("vector", lambda h, d=d: h.tensor_copy(out=betaD[:, d, :].rearrange("p (t h) -> p t h", t=16),
                                                            in_=betat[:, :, d * 8:(d + 1) * 8]), reads=G_, writes=[B_g])
            S.op("vector", lambda h: h.tensor_scalar(out=nbetaD, in0=betaD, scalar1=-1.0, scalar2=None, op0=ALU.mult), reads=G_, writes=[B_g])
            S.op("vector", lambda h: h.tensor_copy(out=gDh, in_=gD), reads=G_, writes=[B_g])
            S.op("vector", lambda h: h.tensor_copy(out=gDhf, in_=gDh), reads=G_, writes=[B_g])
            S.op("vector", lambda h: h.tensor_tensor(out=gDlf, in0=gD, in1=gDhf, op=ALU.subtract), reads=G_, writes=[B_g])
            S.op("vector", lambda h: h.tensor_copy(out=gDl, in_=gDlf), reads=G_, writes=[B_g])
            for d in range(2):
                C1 = C["A1"] if d == 0 else C["A1T"]
                C3 = C["C3f"] if d == 0 else C["C3b"]
                for lhs, dst in ((C1, egc[:, d, :]), (C3, egd[:, d, :]), (C["HA0"], EGL[:, 0, d, :]), (C["HA1"], EGL[:, 1, d, :])):
                    ps, pb = pin_ring.next()
                    S.op("tensor", lambda h, ps=ps, lhs=lhs, d=d: mm_(h, ps[:, 0:128], lhsT=lhs, rhs=gDh[:, d, :], start=True, stop=False),
                         reads=[B_cst, B_g], writes=[pb])
                    S.op("tensor", lambda h, ps=ps, lhs=lhs, d=d: mm_(h, ps[:, 0:128], lhsT=lhs, rhs=gDl[:, d, :], start=False, stop=True),
                         reads=[B_cst, B_g], writes=[pb])
                    S.op("scalar", lambda h, ps=ps, dst=dst: h.activation(out=dst, in_=ps[:, 0:128], func=AF.Exp), reads=[pb], writes=[B_g])
            S.op("vector", lambda h: h.tensor_tensor(out=bg, in0=betaD, in1=egc, op=ALU.mult), reads=G_, writes=[B_g])

            for ch12 in range(12):
                S.op("vector", lambda h, ch12=ch12: h.tensor_tensor(
                    out=diagw[:, ch12, :, :], in0=identb.unsqueeze(1).broadcast_to([128, 5, 128]),
                    in1=convT[:, ch12, :].unsqueeze(2).broadcast_to([128, 5, 128]), op=ALU.mult), reads=[B_cb, B_small], writes=[B_diag])
            chunks = [(grp, kind, off, g, fc) for grp, (off, kind) in enumerate([(OFF_QDN, "q"), (OFF_KDN, "k"), (OFF_VDN, "v")])
                      for g in range(2) for fc in range(2)]
            conv_ring = bank_ring("cv", [4, 5])
            aux_ring = bank_ring("aux", [6, 7])
            pin4 = bank_ring("pin4", [0, 1, 2, 3])
            wcur = {}
            cs_of, sil_of = {}, {}

            def st_A(ci):
                grp, kind, off, g, fc = chunks[ci]
                if fc == 0:
                    wcur[(grp, g)] = WL.load(D["w_in"], off + g * 256)
                wv, wb = wcur[(grp, g)]
                cs, csb = cst_ring.next()
                cs_of[ci] = (cs, csb)
                for tb in range(4):
                    ps, pb = pin4.next()
                    for c in range(8):
                        S.op("tensor", lambda h, ps=ps, c=c, tb=tb: mm_(h,
                            ps, lhsT=wv[:, c, fc * 128:(fc + 1) * 128], rhs=xT[:, c, tb * 512:(tb + 1) * 512],
                            start=(c == 0), stop=(c == 7)), reads=[wb, B_xT], writes=[pb])
                    copy_alt(cs[:, 2 + tb * 512:2 + (tb + 1) * 512], ps, [pb], [csb])

            def st_B(ci):
                grp, kind, off, g, fc = chunks[ci]
                ch12 = grp * 4 + g * 2 + fc
                cs, csb = cs_of[ci]
                sl, slb = sil_ring.next()
                sil_of[ci] = (sl, slb)
                for tb in range(4):
                    ps, pb = conv_ring.next()
                    for tau in range(5):
                        S.op("tensor", lambda h, ps=ps, tau=tau, tb=tb: mm_(h,
                            ps, lhsT=diagw[:, ch12, tau, :], rhs=cs[:, tb * 512 + tau:tb * 512 + tau + 512],
                            start=(tau == 0), stop=(tau == 4)), reads=[B_diag, csb], writes=[pb])
                    S.op("scalar", lambda h, ps=ps, tb=tb: h.activation(out=sl[:, tb * 512:(tb + 1) * 512], in_=ps, func=AF.Silu),
                         reads=[pb], writes=[slb])

            def st_C(ci):
                grp, kind, off, g, fc = chunks[ci]
                fcg = g * 2 + fc
                if kind == "v":
                    return
                sl, slb = sil_of[ci]
                for tb in range(4):
                    sq_, sqb_ = sq_ring.next()
                    S.op("scalar", lambda h, sq_=sq_, tb=tb: h.activation(out=sq_, in_=sl[:, tb * 512:(tb + 1) * 512], func=AF.Square),
                         reads=[slb], writes=[sqb_])
                    ps, pb = aux_ring.next()
                    S.op("tensor", lambda h, ps=ps, sq_=sq_: mm_(h, ps, lhsT=CBn["H"], rhs=sq_, start=True, stop=True),
                         reads=[B_cb, sqb_], writes=[pb])
                    tf_, tfb_ = tmpf_ring.next()
                    S.op("scalar", lambda h, ps=ps, tf_=tf_: h.activation(out=tf_, in_=ps, func=AF.Sqrt, bias=RMS_EPS), reads=[pb], writes=[tfb_])
                    S.op("vector", lambda h, tf_=tf_: h.reciprocal(out=tf_, in_=tf_), reads=[tfb_], writes=[tfb_])
                    if kind == "q":
                        S.op("vector", lambda h, tf_=tf_, tb=tb: h.scalar_tensor_tensor(
                            out=qT[:, fcg, tb * 512:(tb + 1) * 512], in0=sl[:, tb * 512:(tb + 1) * 512], scalar=0.125, in1=tf_,
                            op0=ALU.mult, op1=ALU.mult), reads=[slb, tfb_], writes=[B_qT])
                    else:
                        S.op("vector", lambda h, tf_=tf_, tb=tb: h.tensor_tensor(
                            out=kT[:, fcg, tb * 512:(tb + 1) * 512], in0=sl[:, tb * 512:(tb + 1) * 512], in1=tf_, op=ALU.mult),
                            reads=[slb, tfb_], writes=[B_kT])

            def st_D(ci):
                grp, kind, off, g, fc = chunks[ci]
                fcg = g * 2 + fc
                if kind == "q":
                    return
                if kind == "v":
                    sl, slb = sil_of[ci]
                    src_, srcb, dst_, dstb = (lambda a, b: sl[:, a:b]), slb, vtok, B_vtok
                else:
                    src_, srcb, dst_, dstb = (lambda a, b: kT[:, fcg, a:b]), B_kT, ktok, B_ktok
                for t4 in range(4):
                    ps, pb = aux_ring.next()
                    for u in range(4):
                        tt_ = t4 * 4 + u
                        S.op("tensor", lambda h, ps=ps, u=u, tt_=tt_: mm_(h,
                            ps[:, u * 128:(u + 1) * 128], lhsT=src_(tt_ * 128, (tt_ + 1) * 128), rhs=identb,
                            start=True, stop=True), reads=[srcb, B_cb], writes=[pb])
                    copy_alt(dst_[:, t4 * 4:(t4 + 1) * 4, fcg * 128:(fcg + 1) * 128], v3(ps, 4), [pb], [dstb])

            nch = len(chunks)
            for step in range(nch + 3):
                if 0 <= step - 3 < nch:
                    st_D(step - 3)
                if 0 <= step - 2 < nch:
                    st_C(step - 2)
                if 0 <= step - 1 < nch:
                    st_B(step - 1)
                if step < nch:
                    st_A(step)
            S.barrier()

        if stage >= 3:
            RO = Region([(26, 42)])
            RM = Region([(42, 58), (142, 207)])
            o_dn = tile(RO, [16, 512], BF16)
            B_odn = [Buf(f"odn{t}") for t in range(16)]
            TMP = []
            for d_ in range(2):
                TMP.append(dict(
                    rhsDh=tile(RM, [8, 128], BF16), rhsDl=tile(RM, [8, 128], BF16), B_rhsD=Buf(f"rhsD{d_}"),
                    Eb=tile(RM, [8, 128]), B_E=Buf(f"E{d_}"),
                    P_ring=Ring(f"P{d_}", [tile(RM, [8, 128], BF16) for _ in range(2)]),
                    PT_ring=Ring(f"PT{d_}", [tile(RM, [8, 128], BF16) for _ in range(2)]),
                    X=tile(RM, [8, 128], BF16), B_X=Buf(f"X{d_}"),
                    vb=tile(RM, [512], BF16), kbe=tile(RM, [512], BF16), B_vk=Buf(f"vbkbe{d_}")))
            sets = []
            for k_ in range(4):
                sets.append(dict(WT=tile(RM, [8, 128], BF16), U=tile(RM, [512], BF16), IT=tile(RM, [8, 128], BF16),
                                 KD=tile(RM, [512], BF16), b=Buf(f"set{k_}")))
            Sst = tile(RM, [2, 4, 64]); Sbf = tile(RM, [2, 4, 64], BF16)
            B_S = [Buf("S0"), Buf("S1")]; B_Sbf = [Buf("Sbf0"), Buf("Sbf1")]
            vnew = tile(RM, [2, 512], BF16); B_vnew = [Buf("vn0"), Buf("vn1")]
            ot_ring = Ring("ot", [tile(RM, [512]) for _ in range(2)])
            lring = bank_ring("lps", [0, 1, 2, 3])
            sring = bank_ring("sps", [4, 5, 6, 7])
            EGL5 = EGL.rearrange("q a d (t hc hp) -> q a d t hc hp", t=16, hc=4, hp=2)

            S.op("gpsimd", lambda h: h.memset(Sst, 0.0), writes=B_S)
            S.op("gpsimd", lambda h: h.memset(Sbf, 0.0), writes=B_Sbf)

            def local(t, d, st_):
                T_ = TMP[d]
                rhsDh, rhsDl, B_rhsD, Eb, B_E = T_["rhsDh"], T_["rhsDl"], T_["B_rhsD"], T_["Eb"], T_["B_E"]
                P_ring, PT_ring, X, B_X, vb, kbe, B_vk = T_["P_ring"], T_["PT_ring"], T_["X"], T_["B_X"], T_["vb"], T_["kbe"], T_["B_vk"]
                C1 = C["A1"] if d == 0 else C["A1T"]
                C2 = C["A2"] if d == 0 else C["A2T"]
                NEGS = CBn["NEGSf"] if d == 0 else CBn["NEGSb"]
                NEG2 = CBn["NEG2f"] if d == 0 else CBn["NEG2b"]
                col = lambda h_: t * 8 + h_
                sb_ = st_["b"]

                def decay(Cl, Cr, NEGm):
                    if os.environ.get("RHSD_ACT", "0") == "1":
                        for h_ in range(8):
                            for dst, src in ((rhsDh, gDhf), (rhsDl, gDlf)):
                                S.op("scalar", lambda h, dst=dst, src=src, h_=h_: h.activation(
                                    out=dst[:, h_, :], in_=Cr, func=AF.Identity, scale=src[:, d, col(h_):col(h_) + 1]),
                                    reads=[B_cst, B_g], writes=[B_rhsD])
                    else:
                        for dst, src in ((rhsDh, gDhf), (rhsDl, gDlf)):
                            S.op("gpsimd", lambda h, dst=dst, src=src: h.tensor_tensor(
                                out=dst, in0=Cr.unsqueeze(1).broadcast_to([128, 8, 128]),
                                in1=src[:, d, t * 8:(t + 1) * 8].unsqueeze(2).broadcast_to([128, 8, 128]), op=ALU.mult),
                                reads=[B_cst, B_g], writes=[B_rhsD])
                    for half in range(2):
                        ps, pb = lring.next()
                        for hh in range(4):
                            h_ = half * 4 + hh
                            S.op("tensor", lambda h, ps=ps, hh=hh: mm_(h, ps[:, hh * 128:(hh + 1) * 128], lhsT=identb, rhs=NEGm,
                                                                       start=True, stop=False), reads=[B_cb], writes=[pb])
                            S.op("tensor", lambda h, ps=ps, hh=hh, h_=h_: mm_(h, ps[:, hh * 128:(hh + 1) * 128], lhsT=Cl, rhs=rhsDh[:, h_, :],
                                                                              start=False, stop=False), reads=[B_cst, B_rhsD], writes=[pb])
                            S.op("tensor", lambda h, ps=ps, hh=hh, h_=h_: mm_(h, ps[:, hh * 128:(hh + 1) * 128], lhsT=Cl, rhs=rhsDl[:, h_, :],
                                                                              start=False, stop=True), reads=[B_cst, B_rhsD], writes=[pb])
                        S.op("scalar", lambda h, ps=ps, half=half: h.activation(out=Eb[:, half * 4:(half + 1) * 4, :], in_=v3(ps, 4), func=AF.Exp),
                             reads=[pb], writes=[B_E])

                decay(C1, C2, NEGS)
                yield
                Pm, Pmb = P_ring.next()
                for par in range(2):
                    ps, pb = lring.next()
                    hp = par * 64
                    for hh in range(4):
                        S.op("tensor", lambda h, ps=ps, hh=hh, hp=hp: mm_(h,
                            ps[:, hh * 128:(hh + 1) * 128], lhsT=kT[hp:hp + 64, hh, t * 128:(t + 1) * 128],
                            rhs=kT[hp:hp + 64, hh, t * 128:(t + 1) * 128], start=True, stop=True), reads=[B_kT], writes=[pb])
                    for hh in range(4):
                        h_ = hh * 2 + par
                        S.op("vector", lambda h, ps=ps, hh=hh, h_=h_, Pm=Pm: h.scalar_tensor_tensor(
                            out=Pm[:, h_, :], in0=ps[:, hh * 128:(hh + 1) * 128], scalar=nbetaD[:, d, col(h_):col(h_) + 1],
                            in1=Eb[:, h_, :], op0=ALU.mult, op1=ALU.mult), reads=[pb, B_g, B_E], writes=[Pmb])
                yield
                PTm, PTmb = PT_ring.next()
                for half in range(2):
                    ps, pb = lring.next()
                    for hh in range(4):
                        h_ = half * 4 + hh
                        S.op("tensor", lambda h, ps=ps, hh=hh, h_=h_, Pm=Pm: mm_(h, ps[:, hh * 128:(hh + 1) * 128], lhsT=Pm[:, h_, :], rhs=identb,
                                                                                      start=True, stop=True), reads=[Pmb, B_cb], writes=[pb])
                    S.op("scalar", lambda h, ps=ps, half=half, PTm=PTm: h.copy(out=PTm[:, half * 4:(half + 1) * 4, :], in_=v3(ps, 4)),
                         reads=[pb], writes=[PTmb])
                    S.op("vector", lambda h, ps=ps, half=half: h.tensor_tensor(
                        out=X[:, half * 4:(half + 1) * 4, :], in0=v3(ps, 4), in1=identf.unsqueeze(1).broadcast_to([128, 4, 128]), op=ALU.add),
                        reads=[pb, B_cst], writes=[B_X])
                yield
                for m in range(6):
                    last = (m == 5)
                    if not last:
                        Pn, Pnb = P_ring.next()
                        PTn, PTnb = PT_ring.next()
                    for half in range(2):
                        hs = [half * 4 + hh for hh in range(4)]
                        if not last:
                            psA, pbA = lring.next()
                            for hh, h_ in enumerate(hs):
                                S.op("tensor", lambda h, psA=psA, hh=hh, h_=h_, Pm=Pm, PTm=PTm: mm_(h,
                                    psA[:, hh * 128:(hh + 1) * 128], lhsT=PTm[:, h_, :], rhs=Pm[:, h_, :], start=True, stop=True),
                                    reads=[Pmb, PTmb], writes=[pbA])
                            S.op("scalar", lambda h, psA=psA, half=half, Pn=Pn: h.copy(out=Pn[:, half * 4:(half + 1) * 4, :], in_=v3(psA, 4)),
                                 reads=[pbA], writes=[Pnb])
                            psB, pbB = lring.next()
                            for hh, h_ in enumerate(hs):
                                S.op("tensor", lambda h, psB=psB, hh=hh, h_=h_, Pm=Pm, PTm=PTm: mm_(h,
                                    psB[:, hh * 128:(hh + 1) * 128], lhsT=Pm[:, h_, :], rhs=PTm[:, h_, :], start=True, stop=True),
                                    reads=[Pmb, PTmb], writes=[pbB])
                            S.op("scalar", lambda h, psB=psB, half=half, PTn=PTn: h.copy(out=PTn[:, half * 4:(half + 1) * 4, :], in_=v3(psB, 4)),
                                 reads=[pbB], writes=[PTnb])
                        if m >= 1:
                            psC, pbC = lring.next()
                            for hh, h_ in enumerate(hs):
                                S.op("tensor", lambda h, psC=psC, hh=hh, h_=h_, Pm=Pm: mm_(h,
                                    psC[:, hh * 128:(hh + 1) * 128], lhsT=Pm[:, h_, :], rhs=X[:, h_, :], start=True, stop=True),
                                    reads=[Pmb, B_X], writes=[pbC])
                            S.op("vector", lambda h, psC=psC, half=half: h.tensor_tensor(
                                out=X[:, half * 4:(half + 1) * 4, :], in0=v3(psC, 4), in1=X[:, half * 4:(half + 1) * 4, :], op=ALU.add),
                                reads=[pbC, B_X], writes=[B_X])
                    if not last:
                        Pm, Pmb, PTm, PTmb = Pn, Pnb, PTn, PTnb
                    yield
                for dst, src, sc_, wbuf in ((vb, vtok, betaD, B_vk), (kbe, ktok, bg, B_vk), (st_["KD"], ktok, egd, sb_)):
                    S.op("gpsimd", lambda h, dst=dst, src=src, sc_=sc_: h.tensor_tensor(
                        out=v3(dst, 8), in0=v3(src[:, t, :], 8),
                        in1=sc_[:, d, t * 8:(t + 1) * 8].unsqueeze(2).broadcast_to([128, 8, 64]), op=ALU.mult),
                        reads=[B_vtok, B_ktok, B_g], writes=[wbuf])
                psU, pbU = lring.next()
                for h_ in range(8):
                    S.op("tensor", lambda h, psU=psU, h_=h_: mm_(h, psU[:, h_ * 64:(h_ + 1) * 64], lhsT=X[:, h_, :], rhs=vb[:, h_ * 64:(h_ + 1) * 64],
                                                                      start=True, stop=True), reads=[B_X, B_vk], writes=[pbU])
                S.op("scalar", lambda h, psU=psU: h.copy(out=st_["U"], in_=psU), reads=[pbU], writes=[sb_])
                WT4 = st_["WT"].rearrange("p (hc hp) c -> p hc hp c", hp=2)
                for half in range(2):
                    psW, pbW = lring.next()
                    for hh in range(4):
                        h_ = half * 4 + hh
                        hc = h_ // 2
                        S.op("tensor", lambda h, psW=psW, hh=hh, h_=h_, hc=hc: mm_(h,
                            psW[:, hh * 128:(hh + 1) * 128], lhsT=kbe[:, hc * 128:(hc + 1) * 128], rhs=X[:, h_, :], start=True, stop=True),
                            reads=[B_vk, B_X], writes=[pbW])
                    pw4 = psW.rearrange("p (hc hp c) -> p hc hp c", hc=2, hp=2)
                    for p_ in range(2):
                        S.op("vector", lambda h, pw4=pw4, p_=p_, half=half: h.tensor_copy(
                            out=WT4[p_ * 64:(p_ + 1) * 64, half * 2:(half + 1) * 2, p_, :], in_=pw4[p_ * 64:(p_ + 1) * 64, :, p_, :]),
                            reads=[pbW], writes=[sb_])
                yield
                decay(C2, C1, NEG2)
                IT4 = st_["IT"].rearrange("p (hc hp) c -> p hc hp c", hp=2)
                Eb4 = Eb.rearrange("p (hc hp) c -> p hc hp c", hp=2)
                for par in range(2):
                    ps, pb = lring.next()
                    hp = par * 64
                    for hh in range(4):
                        S.op("tensor", lambda h, ps=ps, hh=hh, hp=hp: mm_(h,
                            ps[:, hh * 128:(hh + 1) * 128], lhsT=kT[hp:hp + 64, hh, t * 128:(t + 1) * 128],
                            rhs=qT[hp:hp + 64, hh, t * 128:(t + 1) * 128], start=True, stop=True), reads=[B_kT, B_qT], writes=[pb])
                    S.op("vector", lambda h, ps=ps, par=par: h.tensor_tensor(
                        out=IT4[:, :, par, :], in0=v3(ps, 4), in1=Eb4[:, :, par, :], op=ALU.mult),
                        reads=[pb, B_E], writes=[sb_])

            def scan_d(tt, k, d):
                if True:
                    a = k if d == 0 else 1 - k
                    t = tt if d == 0 else 15 - tt
                    st_ = sets[(tt % 2) * 2 + d]
                    sb_ = st_["b"]
                    r0 = 64 * a
                    rows = slice(r0, r0 + 64)
                    pe_, peb = sring.next()
                    po_, pob_ = sring.next()
                    pbk = [(pe_, peb), (po_, pob_)]
                    for par in range(2):
                        hp = par * 64
                        bk, bkb = pbk[par]
                        for hc in range(4):
                            h_ = hc * 2 + par
                            S.op("tensor", lambda h, bk=bk, h_=h_, hc=hc, hp=hp: mm_(h,
                                bk[:, hc * 64:(hc + 1) * 64], lhsT=st_["WT"][hp:hp + 64, h_, :], rhs=Sbf[hp:hp + 64, d, hc, :],
                                start=True, stop=True), reads=[sb_, B_Sbf[d]], writes=[bkb])
                        for hc in range(4):
                            S.op("tensor", lambda h, bk=bk, hc=hc, hp=hp: mm_(h,
                                bk[:, 256 + hc * 64:256 + (hc + 1) * 64], lhsT=qT[hp:hp + 64, hc, t * 128:(t + 1) * 128], rhs=Sbf[hp:hp + 64, d, hc, :],
                                start=True, stop=True), reads=[B_qT, B_Sbf[d]], writes=[bkb])
                    vnew5 = vnew.rearrange("p d (hc hp v) -> p d hc hp v", hc=4, hp=2)
                    U4 = st_["U"].rearrange("p (hc hp v) -> p hc hp v", hc=4, hp=2)
                    for par in range(2):
                        bk, bkb = pbk[par]
                        S.op("vector", lambda h, bk=bk, par=par: h.tensor_tensor(
                            out=vnew5[rows, d, :, par, :], in0=U4[rows, :, par, :], in1=v3(bk[rows, 0:256], 4), op=ALU.subtract),
                            reads=[sb_, bkb], writes=[B_vnew[d]])
                    yield
                    pi, pib = sring.next()
                    for h_ in range(8):
                        S.op("tensor", lambda h, pi=pi, h_=h_: mm_(h,
                            pi[:, h_ * 64:(h_ + 1) * 64], lhsT=st_["IT"][rows, h_, :], rhs=vnew[rows, d, h_ * 64:(h_ + 1) * 64],
                            start=True, stop=True), reads=[sb_, B_vnew[d]], writes=[pib])
                    ot, otb = ot_ring.next()
                    ot4 = ot.rearrange("p (hc hp v) -> p hc hp v", hc=4, hp=2)
                    egc5 = egc.rearrange("p d (t hc hp) -> p d t hc hp", t=16, hc=4, hp=2)
                    for par in range(2):
                        bk, bkb = pbk[par]
                        S.op("vector", lambda h, ot4=ot4, bk=bk, par=par: h.tensor_tensor(
                            out=ot4[rows, :, par, :], in0=v3(bk[rows, 256:512], 4),
                            in1=egc5[rows, d, t, :, par].unsqueeze(2).broadcast_to([64, 4, 64]), op=ALU.mult),
                            reads=[bkb, B_g], writes=[otb])
                    first = (d == 0) == (t < 8)
                    if first:
                        S.op("vector", lambda h, ot=ot, pi=pi: h.tensor_tensor(out=o_dn[rows, t, :], in0=ot[rows, :], in1=pi[rows, :], op=ALU.add),
                             reads=[otb, pib], writes=[B_odn[t]])
                    else:
                        S.op("vector", lambda h, ot=ot, pi=pi: h.tensor_tensor(out=ot[rows, :], in0=ot[rows, :], in1=pi[rows, :], op=ALU.add),
                             reads=[otb, pib], writes=[otb])
                        S.op("gpsimd", lambda h, ot=ot: h.tensor_tensor(out=o_dn[rows, t, :], in0=o_dn[rows, t, :], in1=ot[rows, :], op=ALU.add),
                             reads=[otb, B_odn[t]], writes=[B_odn[t]])
                    yield
                    psu, psub = sring.next()
                    for h_ in range(8):
                        hc = h_ // 2
                        S.op("tensor", lambda h, psu=psu, h_=h_, hc=hc: mm_(h,
                            psu[:, h_ * 64:(h_ + 1) * 64], lhsT=st_["KD"][rows, hc * 128:(hc + 1) * 128], rhs=vnew[rows, d, h_ * 64:(h_ + 1) * 64],
                            start=True, stop=True), reads=[sb_, B_vnew[d]], writes=[psub])
                    psu4 = psu.rearrange("q (hc hp v) -> q hc hp v", hc=4, hp=2)
                    for p_ in range(2):
                        pr = slice(p_ * 64, (p_ + 1) * 64)
                        S.op("vector", lambda h, pr=pr, p_=p_: h.tensor_tensor(
                            out=Sst[pr, d, :, :], in0=Sst[pr, d, :, :],
                            in1=EGL5[pr, a, d, t, :, p_].unsqueeze(2).broadcast_to([64, 4, 64]), op=ALU.mult),
                            reads=[B_S[d], B_g], writes=[B_S[d]])
                        S.op("vector", lambda h, pr=pr, p_=p_, psu4=psu4: h.tensor_tensor(
                            out=Sst[pr, d, :, :], in0=Sst[pr, d, :, :], in1=psu4[pr, :, p_, :], op=ALU.add),
                            reads=[B_S[d], psub], writes=[B_S[d]])
                        S.op("scalar", lambda h, pr=pr: h.copy(out=Sbf[pr, d, :, :], in_=Sst[pr, d, :, :]), reads=[B_S[d]], writes=[B_Sbf[d]])

            import itertools

            def drive(gens):
                live = list(gens)
                while live:
                    for g_ in list(live):
                        try:
                            next(g_)
                        except StopIteration:
                            live.remove(g_)

            def scan_gen(tt):
                return itertools.chain(scan_d(tt, 0, 0), scan_d(tt, 0, 1), scan_d(tt, 1, 0), scan_d(tt, 1, 1))

            for tt in range(16):
                gens = [local(tt, 0, sets[(tt % 2) * 2 + 0]), local(15 - tt, 1, sets[(tt % 2) * 2 + 1])]
                if tt > 0:
                    gens.append(scan_gen(tt - 1))
                drive(gens)
            drive([scan_gen(15)])

            if "dbg_odn" in D:
                S.dma("sync", lambda h: h.dma_start(out=D["dbg_odn"], in_=o_dn), reads=B_odn, writes=[Buf("dbg2")])
            S.barrier()


        def layer_norm_tile(r, gb, out, st, junk, Bs_r, B_gb, B_out, B_st, B_junk, eng_mul="vector"):
            S.op("vector", lambda h: h.memset(st[:, 0:2], 0.0), writes=[B_st])
            S.op("scalar", lambda h: h.activation(out=junk, in_=r, func=AF.Identity, accum_out=st[:, 0:1]), reads=Bs_r + [B_st], writes=[B_junk, B_st])
            S.op("scalar", lambda h: h.activation(out=junk, in_=r, func=AF.Square, accum_out=st[:, 1:2]), reads=Bs_r + [B_st], writes=[B_junk, B_st])
            S.op("vector", lambda h: h.tensor_scalar(out=st[:, 2:3], in0=st[:, 0:1], scalar1=1.0 / DM, scalar2=None, op0=ALU.mult), reads=[B_st], writes=[B_st])
            S.op("vector", lambda h: h.tensor_tensor(out=st[:, 3:4], in0=st[:, 2:3], in1=st[:, 2:3], op=ALU.mult), reads=[B_st], writes=[B_st])
            S.op("vector", lambda h: h.scalar_tensor_tensor(out=st[:, 4:5], in0=st[:, 1:2], scalar=1.0 / DM, in1=st[:, 3:4], op0=ALU.mult, op1=ALU.subtract),
                 reads=[B_st], writes=[B_st])
            S.op("scalar", lambda h: h.activation(out=st[:, 5:6], in_=st[:, 4:5], func=AF.Sqrt, bias=LN_EPS), reads=[B_st], writes=[B_st])
            S.op("vector", lambda h: h.reciprocal(out=st[:, 6:7], in_=st[:, 5:6]), reads=[B_st], writes=[B_st])
            S.op("vector", lambda h: h.tensor_scalar(out=out, in0=r, scalar1=st[:, 2:3], scalar2=st[:, 6:7], op0=ALU.subtract, op1=ALU.mult),
                 reads=Bs_r + [B_st], writes=[B_out])
            S.op(eng_mul, lambda h: h.tensor_tensor(out=out, in0=out, in1=gb[:, 0:DM], op=ALU.mult), reads=[B_out, B_gb], writes=[B_out])
            S.op(eng_mul, lambda h: h.tensor_tensor(out=out, in0=out, in1=gb[:, DM:2 * DM], op=ALU.add), reads=[B_out, B_gb], writes=[B_out])

        if stage >= 5:
            RP = Region([(58, 174)])
            mergedT = tile(Region([(174, 206)]), [8, T], BF16); B_mT = Buf("mergedT")
            xT2 = tile(RP, [8, T], BF16); B_xT2 = Buf("xT2")
            odnT = tile(RP, [4, T], BF16); B_odnT = Buf("odnT")
            wz = tile(RP, [8, 512], BF16); B_wz = Buf("wz")
            WL = WLoader(RP, nslots=3)
            normw = tile(RP, [64]); B_nw = Buf("normw")
            sz_ring = Ring("sz", [tile(RP, [512]) for _ in range(2)])
            sq_ring = Ring("sq", [tile(RP, [512]) for _ in range(2)])
            of_ring = Ring("of", [tile(RP, [512], BF16) for _ in range(2)])
            ss_ring = Ring("ss", [tile(RP, [16]) for _ in range(2)])
            tA_ring = Ring("tA", [tile(RP, [512]) for _ in range(2)])
            tB_ring = Ring("tB", [tile(RP, [512]) for _ in range(2)])
            pz_ring = bank_ring("pz", [0, 1])
            pt_ring = bank_ring("pt", [2, 3])
            pm_ring = bank_ring("pm", [4, 5, 6, 7])

            load_xT(xT2, B_xT2, WL.wst)
            S.dma("sync", lambda h: h.dma_start(out=normw, in_=D["norm_w"].partition_broadcast(128)), writes=[B_nw])
            for g in range(2):
                WL.load(D["w_in"], OFF_ZDN + g * 256, dest=(wz[:, :, g * 256:(g + 1) * 256], B_wz))

            def dn_post(t):
                psz, pzb = pz_ring.next()
                for c in range(8):
                    S.op("tensor", lambda h, c=c: mm_(h, psz, lhsT=xT2[:, c, t * 128:(t + 1) * 128], rhs=wz[:, c, :],
                                                      start=(c == 0), stop=(c == 7)), reads=[B_xT2, B_wz], writes=[pzb])
                yield
                sz, szb = sz_ring.next()
                S.op("scalar", lambda h: h.activation(out=sz, in_=psz, func=AF.Silu), reads=[pzb], writes=[szb])
                sq, sqb_ = sq_ring.next()
                ss, ssb = ss_ring.next()
                o_t = o_dn[:, t, :]
                S.op("vector", lambda h: h.tensor_tensor(out=sq, in0=o_t, in1=o_t, op=ALU.mult), reads=[B_odn[t]], writes=[sqb_])
                yield
                S.op("vector", lambda h: h.tensor_reduce(out=ss[:, 0:8], in_=v3(sq, 8), axis=AX.X, op=ALU.add), reads=[sqb_], writes=[ssb])
                S.op("vector", lambda h: h.tensor_scalar(out=ss[:, 0:8], in0=ss[:, 0:8], scalar1=1.0 / 64, scalar2=RMS_EPS, op0=ALU.mult, op1=ALU.add),
                     reads=[ssb], writes=[ssb])
                yield
                S.op("scalar", lambda h: h.activation(out=ss[:, 8:16], in_=ss[:, 0:8], func=AF.Sqrt), reads=[ssb], writes=[ssb])
                yield
                S.op("vector", lambda h: h.reciprocal(out=ss[:, 8:16], in_=ss[:, 8:16]), reads=[ssb], writes=[ssb])
                S.op("vector", lambda h: h.tensor_tensor(out=v3(sq, 8), in0=v3(o_t, 8), in1=ss[:, 8:16].unsqueeze(2).broadcast_to([128, 8, 64]), op=ALU.mult),
                     reads=[B_odn[t], ssb], writes=[sqb_])
                S.op("vector", lambda h: h.tensor_tensor(out=v3(sq, 8), in0=v3(sq, 8), in1=normw.unsqueeze(1).broadcast_to([128, 8, 64]), op=ALU.mult),
                     reads=[B_nw], writes=[sqb_])
                yield
                of, ofb = of_ring.next()
                S.op("vector", lambda h: h.tensor_tensor(out=of, in0=sq, in1=sz, op=ALU.mult), reads=[sqb_, szb], writes=[ofb])
                yield
                pst, ptb = pt_ring.next()
                for fc in range(4):
                    S.op("tensor", lambda h, fc=fc: mm_(h, pst[:, fc * 128:(fc + 1) * 128], lhsT=of[:, fc * 128:(fc + 1) * 128], rhs=identb,
                                                        start=True, stop=True), reads=[ofb, B_cb], writes=[ptb])
                S.op("scalar", lambda h: h.copy(out=odnT[:, :, t * 128:(t + 1) * 128], in_=v3(pst, 4)), reads=[ptb], writes=[B_odnT])

            def merged_block(part, wp, wpb, wg, wgb, fc, fcg, tb, oT, B_oT):
                ps1, p1b = pm_ring.next()
                for c in range(4):
                    S.op("tensor", lambda h, c=c: mm_(h, ps1, lhsT=wp[:, c, fc * 128:(fc + 1) * 128], rhs=oT[:, c, tb * 512:(tb + 1) * 512],
                                                      start=(c == 0), stop=(c == 3)), reads=[wpb, B_oT], writes=[p1b])
                ps2, p2b = pm_ring.next()
                for c in range(8):
                    S.op("tensor", lambda h, c=c: mm_(h, ps2, lhsT=wg[:, c, fc * 128:(fc + 1) * 128], rhs=xT2[:, c, tb * 512:(tb + 1) * 512],
                                                      start=(c == 0), stop=(c == 7)), reads=[wgb, B_xT2], writes=[p2b])
                tA, tAb = tA_ring.next()
                S.op("scalar", lambda h: h.activation(out=tA, in_=ps2, func=AF.Sigmoid), reads=[p2b], writes=[tAb])
                dst = mergedT[:, fcg, tb * 512:(tb + 1) * 512]
                if part == 0:
                    S.op("vector", lambda h: h.tensor_tensor(out=dst, in0=ps1, in1=tA, op=ALU.mult), reads=[p1b, tAb], writes=[B_mT])
                else:
                    tB, tBb = tB_ring.next()
                    S.op("vector", lambda h: h.tensor_tensor(out=tB, in0=ps1, in1=tA, op=ALU.mult), reads=[p1b, tAb], writes=[tBb])
                    S.op("vector", lambda h: h.tensor_tensor(out=dst, in0=dst, in1=tB, op=ALU.add), reads=[tBb, B_mT], writes=[B_mT])

            def p3a_part(part):
                for g in range(4):
                    wp, wpb = WL.load(D["w_proj_na"] if part == 0 else D["w_proj_dn"], g * 256, kchunks=4, cast_eng="scalar")
                    wg, wgb = WL.load(D["w_in"], (OFF_GNA if part == 0 else OFF_GDN) + g * 256, cast_eng="scalar")
                    for fc in range(2):
                        for tb in range(4):
                            merged_block(part, wp, wpb, wg, wgb, fc, g * 2 + fc, tb,
                                         onaT if part == 0 else odnT, B_onaT if part == 0 else B_odnT)
                            yield

            def dn_post_all():
                for t in range(16):
                    yield from dn_post(t)

            def drive3(gens):
                live = list(gens)
                while live:
                    for g_ in list(live):
                        try:
                            next(g_)
                        except StopIteration:
                            live.remove(g_)

            drive3([dn_post_all(), p3a_part(0)])
            if "dbg_odnT" in D:
                S.dma("sync", lambda h: h.dma_start(out=D["dbg_odnT"], in_=odnT), reads=[B_odnT], writes=[Buf("dbg3")])
            drive3([p3a_part(1)])
            S.barrier()

        if stage >= 6:
            RB = Region([(10, 174)])
            acc = tile(RB, [16, DM]); B_acct = [Buf(f"acc{t}") for t in range(16)]
            x1T = tile(RB, [8, T], BF16); B_x1T = Buf("x1T")
            comb = tile(RB, [16, 32]); B_comb = Buf("comb")
            wout = tile(RB, [8, DM], BF16); B_wout = Buf("wout")
            ln1gb = tile(RB, [2 * DM]); B_ln1 = Buf("ln1gb")
            wrf = tile(RB, [8, 36]); wrh = tile(RB, [8, 36], BF16); wrl = tile(RB, [8, 36], BF16); brb = tile(RB, [36]); B_wr = Buf("wr")
            WLb = WLoader(RB, nslots=2, ncols=128)
            xt_ring = Ring("xt", [tile(RB, [DM]) for _ in range(2)])
            r_ring = Ring("r", [tile(RB, [DM]) for _ in range(1)])
            x1_ring = Ring("x1", [tile(RB, [DM]) for _ in range(1)])
            x1h_ring = Ring("x1h", [tile(RB, [DM], BF16) for _ in range(2)])
            x1l_ring = Ring("x1l", [tile(RB, [DM], BF16) for _ in range(2)])
            x1Tl_ring = Ring("x1Tl", [tile(RB, [8, 128], BF16) for _ in range(1)])
            st_ring = Ring("st", [tile(RB, [8]) for _ in range(2)])
            sm_ring = Ring("sm", [tile(RB, [128]) for _ in range(2)])
            pm2_ring = Ring("pm2", [psum[:, 0:1024], psum[:, 1024:2048]])
            pm2_bufs = [[BANKB[0], BANKB[1]], [BANKB[2], BANKB[3]]]
            pth_b = [BANKB[4], BANKB[5]]
            ptl_b = [BANKB[6], BANKB[7]]
            pth = psum[:, 2048:3072]
            ptl = psum[:, 3072:4096]

            for g in range(8):
                WLb.load(D["w_out"], g * 128, dest=(wout[:, :, g * 128:(g + 1) * 128], B_wout), cast_eng="scalar")
            S.dma("sync", lambda h: h.dma_start(out=ln1gb, in_=D["ln1"].partition_broadcast(128)), writes=[B_ln1])
            S.dma("sync", lambda h: h.dma_start(out=brb, in_=D["b_r"].partition_broadcast(128)), writes=[B_wr])
            S.dma("sync", lambda h: h.dma_start(out=wrf, in_=D["w_r"].rearrange("(c p) n -> p c n", p=128)), writes=[B_wr])
            S.op("vector", lambda h: h.tensor_copy(out=wrh, in_=wrf), reads=[B_wr], writes=[B_wr])
            S.op("vector", lambda h: h.tensor_tensor(out=wrl, in0=wrf, in1=wrh, op=ALU.subtract), reads=[B_wr], writes=[B_wr])

            x1h_of = {}

            def p3b_A(t, k):
                xt, xtb = xt_ring.next()
                S.dma("sync", lambda h: h.dma_start(out=xt, in_=D["x"][t * 128:(t + 1) * 128, :]), writes=[xtb])
                psm = pm2_ring.views[k % 2]
                pmb = pm2_bufs[k % 2]
                r, rb = r_ring.next()
                for half in range(2):
                    for c in range(8):
                        S.op("tensor", lambda h, c=c, half=half: mm_(h, psm[:, half * 512:(half + 1) * 512], lhsT=mergedT[:, c, t * 128:(t + 1) * 128],
                                                                     rhs=wout[:, c, half * 512:(half + 1) * 512], start=(c == 0), stop=(c == 7)),
                             reads=[B_mT, B_wout], writes=[pmb[half]])
                    S.op("vector", lambda h, half=half: h.scalar_tensor_tensor(
                        out=r[:, half * 512:(half + 1) * 512], in0=xt[:, half * 512:(half + 1) * 512], scalar=ALPHA,
                        in1=psm[:, half * 512:(half + 1) * 512], op0=ALU.mult, op1=ALU.add), reads=[xtb, pmb[half]], writes=[rb])
                    yield
                x1, x1b = x1_ring.next()
                st_, stb = st_ring.next()
                S.op("vector", lambda h: h.memset(st_[:, 0:2], 0.0), writes=[stb])
                S.op("scalar", lambda h: h.activation(out=x1, in_=r, func=AF.Identity, accum_out=st_[:, 0:1]), reads=[rb, stb], writes=[x1b, stb])
                yield
                S.op("scalar", lambda h: h.activation(out=x1, in_=r, func=AF.Square, accum_out=st_[:, 1:2]), reads=[rb, stb], writes=[x1b, stb])
                yield
                S.op("vector", lambda h: h.tensor_scalar(out=st_[:, 2:3], in0=st_[:, 0:1], scalar1=1.0 / DM, scalar2=None, op0=ALU.mult), reads=[stb], writes=[stb])
                S.op("vector", lambda h: h.tensor_tensor(out=st_[:, 3:4], in0=st_[:, 2:3], in1=st_[:, 2:3], op=ALU.mult), reads=[stb], writes=[stb])
                yield
                S.op("vector", lambda h: h.scalar_tensor_tensor(out=st_[:, 4:5], in0=st_[:, 1:2], scalar=1.0 / DM, in1=st_[:, 3:4], op0=ALU.mult, op1=ALU.subtract),
                     reads=[stb], writes=[stb])
                S.op("scalar", lambda h: h.activation(out=st_[:, 5:6], in_=st_[:, 4:5], func=AF.Sqrt, bias=LN_EPS), reads=[stb], writes=[stb])
                yield
                S.op("vector", lambda h: h.reciprocal(out=st_[:, 6:7], in_=st_[:, 5:6]), reads=[stb], writes=[stb])
                S.op("vector", lambda h: h.scalar_tensor_tensor(out=st_[:, 7:8], in0=st_[:, 2:3], scalar=-1.0, in1=st_[:, 6:7], op0=ALU.mult, op1=ALU.mult),
                     reads=[stb], writes=[stb])
                yield
                S.op("scalar", lambda h: h.activation(out=x1, in_=r, func=AF.Identity, scale=st_[:, 6:7], bias=st_[:, 7:8]), reads=[rb, stb], writes=[x1b])
                yield
                S.op("vector", lambda h: h.tensor_tensor(out=x1, in0=x1, in1=ln1gb[:, 0:DM], op=ALU.mult), reads=[x1b, B_ln1], writes=[x1b])
                yield
                S.op("vector", lambda h: h.tensor_tensor(out=x1, in0=x1, in1=ln1gb[:, DM:2 * DM], op=ALU.add), reads=[x1b, B_ln1], writes=[x1b])
                yield
                S.op("scalar", lambda h: h.mul(out=acc[:, t, :], in_=x1, mul=ALPHA), reads=[x1b], writes=[B_acct[t]])
                x1h, x1hb = x1h_ring.next()
                x1l, x1lb = x1l_ring.next()
                S.op("scalar", lambda h: h.copy(out=x1h, in_=x1), reads=[x1b], writes=[x1hb])
                yield
                S.op("vector", lambda h: h.tensor_tensor(out=x1l, in0=x1, in1=x1h, op=ALU.subtract), reads=[x1b, x1hb], writes=[x1lb])
                x1h_of[t] = (x1h, x1hb, x1l, x1lb)
                yield

            def p3b_B(t):
                x1h, x1hb, x1l, x1lb = x1h_of[t]
                for c in range(8):
                    S.op("tensor", lambda h, c=c: mm_(h, pth[:, c * 128:(c + 1) * 128], lhsT=x1h[:, c * 128:(c + 1) * 128], rhs=identb, start=True, stop=True),
                         reads=[x1hb, B_cb], writes=[pth_b[c // 4]])
                for hb in range(2):
                    S.op("scalar", lambda h, hb=hb: h.copy(out=x1T[:, hb * 4:(hb + 1) * 4, t * 128:(t + 1) * 128], in_=v3(pth[:, hb * 512:(hb + 1) * 512], 4)),
                         reads=[pth_b[hb]], writes=[B_x1T])
                yield
                for c in range(8):
                    S.op("tensor", lambda h, c=c: mm_(h, ptl[:, c * 128:(c + 1) * 128], lhsT=x1l[:, c * 128:(c + 1) * 128], rhs=identb, start=True, stop=True),
                         reads=[x1lb, B_cb], writes=[ptl_b[c // 4]])
                x1Tl, x1Tlb = x1Tl_ring.next()
                for hb in range(2):
                    S.op("vector", lambda h, hb=hb: h.tensor_copy(out=x1Tl[:, hb * 4:(hb + 1) * 4, :], in_=v3(ptl[:, hb * 512:(hb + 1) * 512], 4)),
                         reads=[ptl_b[hb]], writes=[x1Tlb])
                yield
                psr = ptl[:, 512:548]
                n_ = 0
                for c in range(8):
                    for (lh, rh, lb_) in ((x1T[:, c, t * 128:(t + 1) * 128], wrh[:, c, :], B_x1T), (x1T[:, c, t * 128:(t + 1) * 128], wrl[:, c, :], B_x1T),
                                          (x1Tl[:, c, :], wrh[:, c, :], x1Tlb)):
                        S.op("tensor", lambda h, lh=lh, rh=rh, n_=n_: mm_(h, psr, lhsT=lh, rhs=rh, start=(n_ == 0), stop=(n_ == 23)),
                             reads=[lb_, B_wr], writes=[ptl_b[1]])
                        n_ += 1
                yield
                sm, smb = sm_ring.next()
                R_ = [smb]
                lg = sm[:, 0:36]
                S.op("vector", lambda h: h.tensor_tensor(out=lg, in0=psr, in1=brb, op=ALU.add), reads=[ptl_b[1], B_wr], writes=R_)
                m_, negm, eg, sgs, gp, og = sm[:, 36:37], sm[:, 37:38], sm[:, 38:42], sm[:, 42:43], sm[:, 43:44], sm[:, 44:48]
                tmp32, sel, m1, mask1 = sm[:, 48:80], sm[:, 80:88], sm[:, 88:89], sm[:, 89:97]
                sel2, m2, mask2, dif, e21, den, p1, p2, c8 = (sm[:, 97:105], sm[:, 105:106], sm[:, 106:114], sm[:, 114:115], sm[:, 115:116],
                                                               sm[:, 116:117], sm[:, 117:118], sm[:, 118:119], sm[:, 119:127])
                V = lambda fn: S.op("vector", fn, reads=R_, writes=R_)
                A_ = lambda fn: S.op("scalar", fn, reads=R_, writes=R_)
                V(lambda h: h.tensor_reduce(out=m_, in_=lg[:, 0:4], axis=AX.X, op=ALU.max))
                V(lambda h: h.tensor_scalar(out=negm, in0=m_, scalar1=-1.0, scalar2=None, op0=ALU.mult))
                V(lambda h: h.memset(sgs, 0.0))
                yield
                A_(lambda h: h.activation(out=eg, in_=lg[:, 0:4], func=AF.Exp, bias=negm, accum_out=sgs))
                yield
                V(lambda h: h.reciprocal(out=gp, in_=sgs))
                V(lambda h: h.tensor_scalar(out=og, in0=lg[:, 0:4], scalar1=m_, scalar2=None, op0=ALU.is_equal))
                yield
                V(lambda h: h.tensor_tensor(out=v3(tmp32, 4), in0=v3(lg[:, 4:36], 4), in1=og.unsqueeze(2).broadcast_to([128, 4, 8]), op=ALU.mult))
                V(lambda h: h.tensor_reduce(out=sel, in_=tmp32.rearrange("p (g e) -> p e g", g=4), axis=AX.X, op=ALU.add))
                yield
                V(lambda h: h.tensor_reduce(out=m1, in_=sel, axis=AX.X, op=ALU.max))
                V(lambda h: h.tensor_scalar(out=mask1, in0=sel, scalar1=m1, scalar2=None, op0=ALU.is_equal))
                yield
                V(lambda h: h.scalar_tensor_tensor(out=sel2, in0=mask1, scalar=-1e30, in1=sel, op0=ALU.mult, op1=ALU.add))
                V(lambda h: h.tensor_reduce(out=m2, in_=sel2, axis=AX.X, op=ALU.max))
                yield
                V(lambda h: h.tensor_scalar(out=mask2, in0=sel2, scalar1=m2, scalar2=None, op0=ALU.is_equal))
                V(lambda h: h.tensor_tensor(out=dif, in0=m2, in1=m1, op=ALU.subtract))
                yield
                A_(lambda h: h.activation(out=e21, in_=dif, func=AF.Exp))
                yield
                V(lambda h: h.tensor_scalar(out=den, in0=e21, scalar1=1.0, scalar2=None, op0=ALU.add))
                V(lambda h: h.reciprocal(out=p1, in_=den))
                yield
                V(lambda h: h.tensor_tensor(out=p2, in0=e21, in1=p1, op=ALU.mult))
                V(lambda h: h.tensor_tensor(out=p1, in0=p1, in1=gp, op=ALU.mult))
                yield
                V(lambda h: h.tensor_tensor(out=p2, in0=p2, in1=gp, op=ALU.mult))
                V(lambda h: h.tensor_scalar(out=c8, in0=mask1, scalar1=p1, scalar2=None, op0=ALU.mult))
                yield
                V(lambda h: h.scalar_tensor_tensor(out=c8, in0=mask2, scalar=p2, in1=c8, op0=ALU.mult, op1=ALU.add))
                V(lambda h: h.tensor_copy(out=v3(tmp32, 4), in_=c8.unsqueeze(1).broadcast_to([128, 4, 8])))
                yield
                S.op("vector", lambda h: h.tensor_tensor(out=v3(comb[:, t, :], 4), in0=v3(tmp32, 4), in1=og.unsqueeze(2).broadcast_to([128, 4, 8]), op=ALU.mult),
                     reads=R_, writes=[B_comb])
                yield

            def drive2(gens):
                live = list(gens)
                while live:
                    for g_ in list(live):
                        try:
                            next(g_)
                        except StopIteration:
                            live.remove(g_)

            for t in range(17):
                gens = []
                if t < 16:
                    gens.append(p3b_A(t, t))
                if t >= 1:
                    gens.append(p3b_B(t - 1))
                drive2(gens)
            if "dbg_x1" in D:
                S.dma("sync", lambda h: h.dma_start(out=D["dbg_x1"], in_=acc), reads=B_acct, writes=[Buf("dbg4")])
            if "dbg_comb" in D:
                S.dma("sync", lambda h: h.dma_start(out=D["dbg_comb"], in_=comb), reads=[B_comb], writes=[Buf("dbg5")])
            S.barrier()

        if stage >= 7:
            RE = Region([(108, 207)])
            NSLOT = 4
            wgu_v = [tile(RE, [8, 512], BF16) for _ in range(NSLOT)]
            wdn_v = [tile(RE, [2, DM], BF16) for _ in range(NSLOT)]
            wexp_b = [Buf(f"wexp{i}") for i in range(NSLOT)]
            stg_ring = Ring("stg", [tile(RE, [2048]) for _ in range(2)])
            ytmp_ring = Ring("ytmp", [tile(RE, [DM]) for _ in range(3)])
            sgm_ring = Ring("sgm", [tile(RE, [256]) for _ in range(3)])
            hid_ring = Ring("hid", [tile(RE, [256], BF16) for _ in range(3)])
            hidT_ring = Ring("hidT", [tile(RE, [2, 128], BF16) for _ in range(3)])
            ln2gb = tile(RE, [2 * DM]); B_ln2 = Buf("ln2gb")
            outst_ring = Ring("outst", [tile(RE, [DM]) for _ in range(2)])
            st2_ring = Ring("st2", [tile(RE, [8]) for _ in range(2)])
            ph_ring = bank_ring("ph", [0, 1, 2])
            pT2_ring = Ring("pT2", [bank(3, 256, 0), bank(3, 256, 256)])
            pT2_ring.bufs = [BANKB[3], BANKB[3]]
            y_views = [psum[:, 2048:3072], psum[:, 3072:4096]]
            y_bufs = [[BANKB[4], BANKB[5]], [BANKB[6], BANKB[7]]]
            S.dma("sync", lambda h: h.dma_start(out=ln2gb, in_=D["ln2"].partition_broadcast(128)), writes=[B_ln2])

            def load_expert(e, slot):
                wb_ = wexp_b[slot]
                if os.environ.get("MOE_NODMA", "") == "1" and e >= 4:
                    return
                for half in range(2):
                    sv, sb_ = stg_ring.next()
                    S.dma("sync", lambda h, sv=sv, half=half: h.dma_start(
                        out=v3(sv, 8), in_=D["w_gu"][e, :, half * 256:(half + 1) * 256].rearrange("(c p) n -> p c n", p=128)), writes=[sb_])
                    S.op("gpsimd", lambda h, sv=sv, half=half: h.tensor_copy(out=wgu_v[slot][:, :, half * 256:(half + 1) * 256], in_=v3(sv, 8)),
                         reads=[sb_], writes=[wb_])
                sv, sb_ = stg_ring.next()
                S.dma("sync", lambda h, sv=sv: h.dma_start(out=v3(sv, 2), in_=D["w_dn"][e].rearrange("(c p) n -> p c n", p=128)), writes=[sb_])
                S.op("gpsimd", lambda h, sv=sv: h.tensor_copy(out=wdn_v[slot], in_=v3(sv, 2)), reads=[sb_], writes=[wb_])

            def hgu(t, e, slot):
                ps, pb = ph_ring.next()
                for c in range(8):
                    S.op("tensor", lambda h, c=c: mm_(h, ps, lhsT=x1T[:, c, t * 128:(t + 1) * 128], rhs=wgu_v[slot][:, c, :],
                                                      start=(c == 0), stop=(c == 7)), reads=[B_x1T, wexp_b[slot]], writes=[pb])
                return ps, pb

            def stage_b(t, e, ps, pb):
                sg, sgb = sgm_ring.next()
                S.op("scalar", lambda h: h.activation(out=sg, in_=ps[:, 0:256], func=AF.Silu), reads=[pb], writes=[sgb])
                hid, hidb = hid_ring.next()
                S.op("vector", lambda h: h.scalar_tensor_tensor(out=hid, in0=ps[:, 256:512], scalar=comb[:, t, e:e + 1], in1=sg,
                                                                op0=ALU.mult, op1=ALU.mult), reads=[pb, B_comb, sgb], writes=[hidb])
                pT, pTb = pT2_ring.next()
                for f in range(2):
                    S.op("tensor", lambda h, f=f: mm_(h, pT[:, f * 128:(f + 1) * 128], lhsT=hid[:, f * 128:(f + 1) * 128], rhs=identb, start=True, stop=True),
                         reads=[hidb, B_cb], writes=[pTb])
                hT, hTb = hidT_ring.next()
                S.op("scalar", lambda h: h.copy(out=hT, in_=v3(pT[:, 0:256], 2)), reads=[pTb], writes=[hTb])
                return hT, hTb

            def stage_c(slot, hT, hTb, yv, yb, first, last):
                for half in range(2):
                    for f in range(2):
                        S.op("tensor", lambda h, f=f, half=half: mm_(h, yv[:, half * 512:(half + 1) * 512], lhsT=hT[:, f, :],
                                                                     rhs=wdn_v[slot][:, f, half * 512:(half + 1) * 512],
                                                                     start=(first and f == 0), stop=(last and f == 1)),
                             reads=[hTb, wexp_b[slot]], writes=[yb[half]])

            def flush_copy(t, yv, yb):
                yt, ytb = ytmp_ring.next()
                S.op("scalar", lambda h: h.copy(out=yt[:, 0:512], in_=yv[:, 0:512]), reads=[yb[0]], writes=[ytb])
                S.op("scalar", lambda h: h.copy(out=yt[:, 512:1024], in_=yv[:, 512:1024]), reads=[yb[1]], writes=[ytb])
                return t, yt, ytb

            def flush_add(t, yt, ytb):
                S.op("vector", lambda h: h.tensor_tensor(out=acc[:, t, :], in0=acc[:, t, :], in1=yt, op=ALU.add), reads=[ytb, B_acct[t]], writes=[B_acct[t]])

            def final_tile(t):
                ot_, otb_ = outst_ring.next()
                st_, stb = st2_ring.next()
                layer_norm_tile(acc[:, t, :], ln2gb, ot_, st_, ot_, [B_acct[t]], B_ln2, otb_, stb, otb_)
                S.dma("sync", lambda h: h.dma_start(out=D["out"][t * 128:(t + 1) * 128, :], in_=ot_), reads=[otb_], writes=[Buf(f"outd{t}")], sem_buf=otb_)

            NE = int(os.environ.get("N_EXPERTS", "32"))
            load_expert(0, 0)
            load_expert(1, 1)
            items = [(grp, t, j) for grp in range(NE // 2) for t in range(16) for j in range(2)]
            n_it = len(items)
            Hs, Ts = {}, {}
            yk = 0
            pending = []
            pending2 = []
            fin_cnt = [0] * 16
            for step in range(n_it + 4):
                if step < n_it:
                    grp, t_, j_ = items[step]
                    if t_ == 2 and j_ == 0 and grp + 1 < NE // 2:
                        load_expert(2 * grp + 2, (2 * grp + 2) % NSLOT)
                        load_expert(2 * grp + 3, (2 * grp + 3) % NSLOT)
                    Hs[step] = hgu(t_, 2 * grp + j_, (2 * grp + j_) % NSLOT)
                i1_ = step - 1
                if 0 <= i1_ < n_it:
                    grp, t_, j_ = items[i1_]
                    Ts[i1_] = stage_b(t_, 2 * grp + j_, Hs[i1_][0], Hs[i1_][1])
                    del Hs[i1_]
                for args in pending2:
                    flush_add(*args)
                    fin_cnt[args[0]] += 1
                    if fin_cnt[args[0]] == NE // 2:
                        final_tile(args[0])
                pending2 = [flush_copy(*args) for args in pending]
                pending = []
                i2_ = step - 2
                if 0 <= i2_ < n_it:
                    grp, t_, j_ = items[i2_]
                    yv, yb = y_views[yk % 2], y_bufs[yk % 2]
                    stage_c((2 * grp + j_) % NSLOT, Ts[i2_][0], Ts[i2_][1], yv, yb, j_ == 0, j_ == 1)
                    del Ts[i2_]
                    if j_ == 1:
                        pending.append((t_, yv, yb))
                        yk += 1

            S.barrier()

        build_program.last_sched = S
        S.emit()
    return nc


def make_in_maps(inputs):
    f = lambda a: np.ascontiguousarray(np.asarray(a, dtype=np.float32))
    x = f(inputs["x"])
    w_in = f(inputs["w_in"])[0]
    shared = {
        "w_in": w_in,
        "nab": na_bias_tiles(f(inputs["na_rpb"])[0]),
        "consts": np.ascontiguousarray(CONST_ARR),
        "convT": np.ascontiguousarray(f(inputs["dn_conv_w"])[0].T.reshape(12, 128, 5).transpose(1, 0, 2).reshape(128, 60)),
        "gvec": np.concatenate([f(inputs["dn_a_log_f"])[0], f(inputs["dn_a_log_b"])[0],
                                f(inputs["dn_dt_bias_f"])[0], f(inputs["dn_dt_bias_b"])[0]])[None, :].copy(),
        "norm_w": f(inputs["dn_norm_w"]).reshape(1, 64),
        "w_proj_na": f(inputs["w_proj_na"])[0], "w_proj_dn": f(inputs["w_proj_dn"])[0], "w_out": f(inputs["w_out"])[0],
        "ln1": np.concatenate([f(inputs["ln1_g"])[0], f(inputs["ln1_b"])[0]])[None, :].copy(),
        "ln2": np.concatenate([f(inputs["ln2_g"])[0], f(inputs["ln2_b"])[0]])[None, :].copy(),
        "w_r": np.ascontiguousarray(np.concatenate([f(inputs["w_router_group"])[0], f(inputs["w_router_expert"])[0]], axis=1)),
        "b_r": np.concatenate([f(inputs["b_router_group"])[0], f(inputs["b_router_expert"])[0]])[None, :].copy(),
        "w_gu": f(inputs["w_expert_gate_up"])[0], "w_dn": f(inputs["w_expert_down"])[0],
    }
    maps = []
    for b in range(8):
        m = dict(shared)
        m["x"] = np.ascontiguousarray(x[b])
        m["xT"] = np.ascontiguousarray(x[b].T)
        maps.append(m)
    return maps


def kernel(**inputs):
    nc = build_program()
    in_maps = make_in_maps(inputs)
    res = run_bass_kernel_spmd(nc, in_maps, core_ids=list(range(8)))
    return np.stack([np.asarray(r["out"], dtype=np.float32) for r in res.results], axis=0)
```

```python
from contextlib import ExitStack
import numpy as np
import concourse.bass as bass
import concourse.mybir as mybir
from concourse.bass_utils import run_bass_kernel_spmd

F32 = mybir.dt.float32
BF16 = mybir.dt.bfloat16
AF = mybir.ActivationFunctionType
ALU = mybir.AluOpType
AX = mybir.AxisListType

ENGS = ("tensor", "vector", "scalar", "gpsimd", "sync")
SAME_ENGINE_SYNC = True
NEG = -30000.0
T = 2048
DM = 1024
ARENA_KB = 207


class Buf:
    __slots__ = ("name", "w", "r", "dsem", "dcnt", "excl")

    def __init__(self, name, excl=False):
        self.name = name
        self.excl = excl
        self.w = None
        self.r = []
        self.dsem = None
        self.dcnt = 0


class Op:
    __slots__ = ("eng", "idx", "fn", "waits", "signal", "sigval", "dma", "dsem", "dval")

    def __init__(self, eng, idx, fn):
        self.eng = eng
        self.idx = idx
        self.fn = fn
        self.waits = []
        self.signal = False
        self.sigval = None
        self.dma = False
        self.dsem = None
        self.dval = 0


class Sched:
    def __init__(self, nc, stack):
        self.nc = nc
        self.stack = stack
        self.ops = {e: [] for e in ENGS}
        self.seen = {e: {p: -1 for p in ENGS} for e in ENGS}
        self.seen_dma = {e: {} for e in ENGS}
        self.nsem = 0

    def new_sem(self, name):
        self.nsem += 1
        return self.stack.enter_context(self.nc.semaphore(f"{name}_{self.nsem}"))

    def _dep(self, op, dep):
        if dep is None or dep is op:
            return
        if dep.dma:
            key = id(dep.dsem)
            if self.seen_dma[op.eng].get(key, 0) >= dep.dval:
                return
            self.seen_dma[op.eng][key] = dep.dval
            op.waits.append(dep)
            return
        if dep.eng == op.eng:
            if dep.eng == "tensor" or not SAME_ENGINE_SYNC:
                return
        if self.seen[op.eng][dep.eng] >= dep.idx:
            return
        self.seen[op.eng][dep.eng] = dep.idx
        dep.signal = True
        op.waits.append(dep)

    def _deps(self, o, deps):
        best = {}
        for dep in deps:
            if dep is None or dep is o:
                continue
            key = ("d", id(dep.dsem)) if dep.dma else ("e", dep.eng)
            val = dep.dval if dep.dma else dep.idx
            if key not in best or val > best[key][0]:
                best[key] = (val, dep)
        for _, dep in best.values():
            self._dep(o, dep)

    def op(self, eng, fn, reads=(), writes=()):
        lst = self.ops[eng]
        o = Op(eng, len(lst), fn)
        ex = [b for b in reads if b.excl and b not in writes]
        if ex:
            reads = [b for b in reads if not b.excl]
            writes = list(writes) + ex
        deps = [b.w for b in reads]
        for b in writes:
            deps.append(b.w)
            deps.extend(b.r)
        self._deps(o, deps)
        for b in reads:
            b.r.append(o)
        for b in writes:
            b.w = o
            b.r = []
        lst.append(o)
        return o

    def dma(self, eng, fn, reads=(), writes=(), sem_buf=None):
        lst = self.ops[eng]
        o = Op(eng, len(lst), fn)
        o.dma = True
        sb = sem_buf or (writes[0] if writes else reads[0])
        if sb.dsem is None:
            sb.dsem = self.new_sem("d_" + sb.name)
        deps = [b.w for b in reads]
        for b in writes:
            if b.w is not None and not (b.w.dma and b.w.dsem is sb.dsem):
                deps.append(b.w)
            deps.extend(b.r)
        self._deps(o, deps)
        sb.dcnt += 16
        o.dsem = sb.dsem
        o.dval = sb.dcnt
        for b in reads:
            b.r.append(o)
        for b in writes:
            b.w = o
            b.r = []
        lst.append(o)
        return o

    def barrier(self):
        lasts = []
        for e in ENGS:
            for o in reversed(self.ops[e]):
                if not o.dma:
                    lasts.append(o)
                    break
        latest = {}
        for e in ENGS:
            for o in self.ops[e]:
                if o.dma:
                    latest[id(o.dsem)] = o
        for e in ENGS:
            o = Op(e, len(self.ops[e]), None)
            for d in lasts:
                self._dep(o, d)
            for d in latest.values():
                self._dep(o, d)
            self.ops[e].append(o)

    def emit(self):
        nc = self.nc
        CH = 1000
        esems = {}
        for e in ENGS:
            n = sum(1 for o in self.ops[e] if o.signal)
            esems[e] = [self.new_sem(f"e_{e}_{i}") for i in range(n // CH + 1)]
            c = 0
            for o in self.ops[e]:
                if o.signal:
                    o.sigval = c
                    c += 1

        def run(e, h):
            for o in self.ops[e]:
                for d in o.waits:
                    if d.dma:
                        h.wait_ge(d.dsem, d.dval)
                    else:
                        h.wait_ge(esems[d.eng][d.sigval // CH], d.sigval % CH + 1)
                if o.fn is None:
                    if o.signal:
                        h.nop().then_inc(esems[e][o.sigval // CH], 1)
                    continue
                ins = o.fn(h)
                if o.dma:
                    ins.then_inc(o.dsem, 16)
                elif o.signal:
                    ins.then_inc(esems[e][o.sigval // CH], 1)

        with nc.Block() as block:
            @block.tensor
            def _(h):
                run("tensor", h)

            @block.vector
            def _(h):
                run("vector", h)

            @block.scalar
            def _(h):
                run("scalar", h)

            @block.gpsimd
            def _(h):
                run("gpsimd", h)

            @block.sync
            def _(h):
                run("sync", h)


class Ring:
    def __init__(self, name, views):
        self.views = views
        self.bufs = [Buf(f"{name}{i}") for i in range(len(views))]
        self.i = 0

    def next(self):
        k = self.i % len(self.views)
        self.i += 1
        return self.views[k], self.bufs[k]


def _rs(r):
    return min(max(r - 4, 0), 24)


def na_blocks(i):
    res = []
    for j in range(16):
        valid = [[_rs(2 * i + b) <= 2 * j + a < _rs(2 * i + b) + 8 for b in (0, 1)] for a in (0, 1)]
        if not (valid[0][0] or valid[0][1] or valid[1][0] or valid[1][1]):
            continue
        delta = j - i
        if all(valid[0]) and all(valid[1]):
            var = delta + 3
        elif delta == -2 and valid == [[True, False], [True, True]]:
            var = 7
        elif delta == 2 and valid == [[False, True], [False, False]]:
            var = 8
        else:
            raise AssertionError((i, j, valid))
        assert 0 <= var < 9
        res.append((j, var))
    return res


def na_bias_tiles(rpb):
    c = np.arange(64)
    col_start = np.clip(c - 8, 0, 48)
    col_mask = (c[None, :] >= col_start[:, None]) & (c[None, :] < col_start[:, None] + 16)
    dc_idx = np.clip(c[None, :] - c[:, None], -15, 15) + 15
    tiles = np.full((8, 9, 128, 128), NEG, np.float32)
    for v in range(9):
        for a in (0, 1):
            for b in (0, 1):
                if v < 7:
                    delta, ok = v - 3, True
                elif v == 7:
                    delta, ok = -2, not (a == 0 and b == 1)
                else:
                    delta, ok = 2, (a == 0 and b == 1)
                dr = 2 * delta + a - b + 7
                if not ok or not (0 <= dr <= 14):
                    continue
                vals = rpb[:, dr][:, dc_idx]
                vals = np.where(col_mask[None], vals, np.float32(NEG))
                tiles[:, v, a * 64:(a + 1) * 64, b * 64:(b + 1) * 64] = vals.transpose(0, 2, 1)
    return np.ascontiguousarray(tiles.transpose(2, 0, 1, 3)).reshape(128, 72 * 128)


def dn_consts():
    p = np.arange(128)
    same = (p[:, None] // 64) == (p[None, :] // 64)
    A1 = (same & (p[:, None] <= p[None, :])).astype(np.float32)
    A2 = (same & (p[:, None] > p[None, :])).astype(np.float32)
    ones = same.astype(np.float32)
    out = {
        "A1": A1, "A2": A2, "A1T": A1.T.copy(), "A2T": A2.T.copy(),
        "C3f": ones - A1, "C3b": ones - A1.T,
        "HA0": np.repeat((p < 64).astype(np.float32)[:, None], 128, 1),
        "HA1": np.repeat((p >= 64).astype(np.float32)[:, None], 128, 1),
        "NEGSf": np.where(same & (p[None, :] < p[:, None]), 0.0, NEG).astype(np.float32),
        "NEG2f": np.where(same & (p[:, None] <= p[None, :]), 0.0, NEG).astype(np.float32),
        "NEGSb": np.where(same & (p[None, :] > p[:, None]), 0.0, NEG).astype(np.float32),
        "NEG2b": np.where(same & (p[:, None] >= p[None, :]), 0.0, NEG).astype(np.float32),
        "IDENT": np.eye(128, dtype=np.float32),
        "H": ones,
    }
    names = ["A1", "A2", "A1T", "A2T", "C3f", "C3b", "HA0", "HA1", "NEGSf", "NEG2f", "NEGSb", "NEG2b", "IDENT", "H"]
    return names, np.concatenate([out[n] for n in names], axis=1)


CONST_NAMES, CONST_ARR = dn_consts()
NCONST = len(CONST_NAMES)

OFF_QNA, OFF_KNA, OFF_VNA = 0, 512, 1024
OFF_QDN, OFF_KDN, OFF_VDN, OFF_ZDN = 1536, 2048, 2560, 3072
OFF_G = 3584
OFF_GNA, OFF_GDN = 3616, 4640


class Region:
    def __init__(self, ranges):
        self.free = [[int(a * 1024), int(b * 1024)] for a, b in ranges]

    def alloc(self, nbytes):
        nbytes = (nbytes + 63) // 64 * 64
        for r in self.free:
            if r[1] - r[0] >= nbytes:
                off = r[0]
                r[0] += nbytes
                return off
        raise MemoryError(f"arena region exhausted: need {nbytes}, free {self.free}")


import os
LP = int(os.environ.get("LOCAL_PARTS", "9"))
SP = int(os.environ.get("SCAN_PARTS", "9"))
def mm_(h, out, **kw):
    return h.matmul(out, skip_group_check=True, **kw)


ALPHA = 2.0 ** 0.25
LN_EPS = 1e-5
RMS_EPS = 1e-6


def build_program(stage=99, dbg=()):
    nc = bass.Bass("TRN2", target_bir_lowering=False)
    D = {}

    def din(name, shape, dt=F32):
        D[name] = nc.dram_tensor(name, list(shape), dt, kind="ExternalInput").ap()

    def dout(name, shape, dt=F32):
        D[name] = nc.dram_tensor(name, list(shape), dt, kind="ExternalOutput").ap()

    din("x", [T, DM]); din("xT", [DM, T]); din("w_in", [DM, 5664]); din("nab", [128, 72 * 128])
    din("consts", [128, NCONST * 128])
    din("convT", [128, 12 * 5]); din("gvec", [1, 32]); din("norm_w", [1, 64])
    din("w_proj_na", [512, DM]); din("w_proj_dn", [512, DM]); din("w_out", [DM, DM])
    din("ln1", [1, 2 * DM]); din("ln2", [1, 2 * DM])
    din("w_r", [DM, 36]); din("b_r", [1, 36])
    din("w_gu", [32, DM, 512]); din("w_dn", [32, 256, DM])
    dout("out", [T, DM])
    for name, shape, dt in dbg:
        dout(name, shape, dt)

    with ExitStack() as st:
        S = Sched(nc, st)
        arena = st.enter_context(nc.sbuf_tensor("arena", [128, ARENA_KB * 256], F32))
        psum = st.enter_context(nc.psum_tensor("psum", [128, 4096], F32))

        def tile(reg, dims, dt=F32):
            n = 1
            for d_ in dims:
                n *= d_
            nbytes = n * (4 if dt == F32 else 2)
            off = reg.alloc(nbytes)
            v = arena[:, off // 4:(off + nbytes + 3) // 4]
            if dt != F32:
                v = v.bitcast(dt)[:, 0:n]
            if len(dims) > 1:
                names = " ".join(f"d{i}" for i in range(len(dims)))
                kw = {f"d{i}": dims[i] for i in range(1, len(dims))}
                v = v.rearrange(f"p ({names}) -> p {names}", **kw)
            return v

        def bank(b, n=512, off=0):
            return psum[:, b * 512 + off:b * 512 + off + n]

        BANKB = [Buf(f"bank{b}", excl=True) for b in range(8)]

        def bank_ring(name, banks):
            r = Ring(name, [bank(b) for b in banks])
            r.bufs = [BANKB[b] for b in banks]
            return r

        def v3(ap, h):
            return ap.rearrange("p (h j) -> p h j", h=h)

        R0 = Region([(0, 10)])
        RN = Region([(26, 206)])
        xT = tile(RN, [8, T], BF16)
        cst = tile(RN, [NCONST * 128])
        B_cst = Buf("cst")
        S.dma("sync", lambda h: h.dma_start(out=cst, in_=D["consts"]), writes=[B_cst])
        cb = tile(R0, [NCONST * 128], BF16)
        identf = tile(R0, [128])
        B_cb = Buf("cb")
        S.op("vector", lambda h: h.tensor_copy(out=cb, in_=cst), reads=[B_cst], writes=[B_cb])
        i_id = CONST_NAMES.index("IDENT")
        S.op("vector", lambda h: h.tensor_copy(out=identf, in_=cst[:, i_id * 128:(i_id + 1) * 128]), reads=[B_cst], writes=[B_cb])
        CBn = {n: cb[:, i * 128:(i + 1) * 128] for i, n in enumerate(CONST_NAMES)}
        C = CBn
        B_cst = B_cb
        identb = CBn["IDENT"]

        onaT = tile(Region([(10, 26)]), [4, T], BF16)
        B_onaT = Buf("onaT")

        evac_flip = [0]

        def copy_alt(out, in_, reads, writes, scale=None):
            evac_flip[0] ^= 1
            if evac_flip[0]:
                if scale is None:
                    S.op("scalar", lambda h: h.copy(out=out, in_=in_), reads=reads, writes=writes)
                else:
                    S.op("scalar", lambda h: h.mul(out=out, in_=in_, mul=scale), reads=reads, writes=writes)
            else:
                if scale is None:
                    S.op("vector", lambda h: h.tensor_copy(out=out, in_=in_), reads=reads, writes=writes)
                else:
                    S.op("vector", lambda h: h.tensor_scalar(out=out, in0=in_, scalar1=scale, scalar2=None, op0=ALU.mult),
                         reads=reads, writes=writes)

        class WLoader:
            def __init__(self, reg, nslots=2, ncols=256):
                self.ncols = ncols
                self.wst = Ring("wst", [tile(reg, [8, ncols]) for _ in range(nslots)])
                self.wbf = Ring("wbf", [tile(reg, [8, ncols], BF16) for _ in range(nslots)])

            def load(self, src, col0, ncols=None, kchunks=8, cast_eng="gpsimd", dest=None):
                ncols = ncols or self.ncols
                sv, sb_ = self.wst.next()
                if dest is None:
                    wv, wb = self.wbf.next()
                    wv = wv[:, 0:kchunks, 0:ncols]
                else:
                    wv, wb = dest
                S.dma("sync", lambda h: h.dma_start(out=sv[:, 0:kchunks, 0:ncols],
                                                    in_=src[:, col0:col0 + ncols].rearrange("(c p) n -> p c n", p=128)),
                      writes=[sb_])
                if cast_eng == "scalar":
                    S.op("scalar", lambda h: h.copy(out=wv, in_=sv[:, 0:kchunks, 0:ncols]), reads=[sb_], writes=[wb])
                else:
                    S.op(cast_eng, lambda h: h.tensor_copy(out=wv, in_=sv[:, 0:kchunks, 0:ncols]), reads=[sb_], writes=[wb])
                return wv, wb

        def proj_fm(pring, wv, wb, ncols, evac, act, B_act, kchunks=8):
            for fc in range(ncols // 128):
                for tb in range(4):
                    ps, pb = pring.next()
                    for c in range(kchunks):
                        S.op("tensor", lambda h, ps=ps, c=c, fc=fc, tb=tb: mm_(h,
                            ps, lhsT=wv[:, c, fc * 128:(fc + 1) * 128], rhs=act[:, c, tb * 512:(tb + 1) * 512],
                            start=(c == 0), stop=(c == kchunks - 1)), reads=[wb, B_act], writes=[pb])
                    evac(ps, pb, fc, tb)

        def proj_tm(pring, wv, wb, ncols, evac, act, B_act, kchunks=8):
            for t in range(16):
                ps, pb = pring.next()
                for c in range(kchunks):
                    S.op("tensor", lambda h, ps=ps, c=c, t=t: mm_(h,
                        ps[:, 0:ncols], lhsT=act[:, c, t * 128:(t + 1) * 128], rhs=wv[:, c, 0:ncols],
                        start=(c == 0), stop=(c == kchunks - 1)), reads=[wb, B_act], writes=[pb])
                evac(ps, pb, t)

        def load_xT(xT, B_xT, ring):
            for c in range(8):
                sv, sb_ = ring.next()
                svf = sv.rearrange("p c n -> p (c n)") if len(sv.shape) == 3 else sv
                S.dma("sync", lambda h, svf=svf, c=c: h.dma_start(out=svf[:, 0:T], in_=D["xT"][c * 128:(c + 1) * 128, :]),
                      writes=[sb_])
                if c % 2 == 0:
                    S.op("vector", lambda h, svf=svf, c=c: h.tensor_copy(out=xT[:, c, :], in_=svf[:, 0:T]), reads=[sb_], writes=[B_xT])
                else:
                    S.op("scalar", lambda h, svf=svf, c=c: h.copy(out=xT[:, c, :], in_=svf[:, 0:T]), reads=[sb_], writes=[B_xT])

        B_xT = Buf("xT")
        qnaT = tile(RN, [4, T], BF16)
        knaT = tile(RN, [4, T], BF16)
        B_qnaT, B_knaT = Buf("qnaT"), Buf("knaT")
        vaug = tile(RN, [16, 8, 128], BF16)
        B_vaug = Buf("vaug")
        nab = tile(RN, [8, 9, 128], BF16)
        B_nab = Buf("nab")
        pT_ring = Ring("pT", [tile(RN, [640], BF16) for _ in range(3)])
        rden_ring = Ring("rden", [tile(RN, [128]) for _ in range(2)])
        WL = WLoader(RN)
        xst_ring = Ring("xst", [tile(RN, [T]) for _ in range(2)])
        pin_ring = bank_ring("pin", [0, 1, 2])

        load_xT(xT, B_xT, xst_ring)

        nabf = nab.rearrange("p h v q -> p (h v q)")
        for k in range(6):
            sv, sb_ = WL.wst.next()
            svf = sv.rearrange("p c n -> p (c n)")
            w = 12 * 128
            S.dma("sync", lambda h, svf=svf, k=k, w=w: h.dma_start(out=svf[:, 0:w], in_=D["nab"][:, k * w:(k + 1) * w]), writes=[sb_])
            S.op("gpsimd", lambda h, svf=svf, k=k, w=w: h.tensor_copy(out=nabf[:, k * w:(k + 1) * w], in_=svf[:, 0:w]), reads=[sb_], writes=[B_nab])

        for g in range(2):
            wv, wb = WL.load(D["w_in"], OFF_QNA + g * 256)
            proj_fm(pin_ring, wv, wb, 256, lambda ps, pb, fc, tb, g=g: copy_alt(
                qnaT[:, g * 2 + fc, tb * 512:(tb + 1) * 512], ps, [pb], [B_qnaT], scale=0.125), xT, B_xT)
        for g in range(2):
            wv, wb = WL.load(D["w_in"], OFF_KNA + g * 256)
            proj_fm(pin_ring, wv, wb, 256, lambda ps, pb, fc, tb, g=g: copy_alt(
                knaT[:, g * 2 + fc, tb * 512:(tb + 1) * 512], ps, [pb], [B_knaT]), xT, B_xT)
        S.op("gpsimd", lambda h: h.memset(vaug[:, :, :, 64:128], 1.0), writes=[B_vaug])
        for g in range(2):
            wv, wb = WL.load(D["w_in"], OFF_VNA + g * 256)
            proj_tm(pin_ring, wv, wb, 256, lambda ps, pb, t, g=g: copy_alt(
                vaug[:, t, g * 4:(g + 1) * 4, 0:64], v3(ps[:, 0:256], 4), [pb], [B_vaug]), xT, B_xT)

        sc_ring = Ring("sc", [psum[:, 1024:2048], psum[:, 2048:3072], psum[:, 3072:4096]])
        sc_ring.bufs = [BANKB[2], BANKB[4], BANKB[6]]
        sc_b2 = [BANKB[3], BANKB[5], BANKB[7]]
        po_ring = bank_ring("po", [0, 1])
        def na_scores(i, hd):
            blocks = na_blocks(i)
            nb = len(blocks)
            hc, hp = hd // 2, (hd % 2) * 64
            sc, scb = sc_ring.next()
            scb2 = sc_b2[sc_ring.bufs.index(scb)]
            for bi, (j, var) in enumerate(blocks):
                S.op("tensor", lambda h, bi=bi, j=j: mm_(h,
                    sc[:, bi * 128:(bi + 1) * 128], lhsT=knaT[hp:hp + 64, hc, j * 128:(j + 1) * 128],
                    rhs=qnaT[hp:hp + 64, hc, i * 128:(i + 1) * 128], start=True, stop=False),
                    reads=[B_knaT, B_qnaT], writes=[scb if bi < 4 else scb2])
                S.op("tensor", lambda h, bi=bi, var=var: mm_(h,
                    sc[:, bi * 128:(bi + 1) * 128], lhsT=identb, rhs=nab[:, hd, var, :], start=False, stop=True),
                    reads=[B_cb, B_nab], writes=[scb if bi < 4 else scb2])
            pT, pTb = pT_ring.next()
            n0 = min(nb, 4) * 128
            S.op("scalar", lambda h: h.activation(out=pT[:, 0:n0], in_=sc[:, 0:n0], func=AF.Exp), reads=[scb], writes=[pTb])
            if nb > 4:
                S.op("scalar", lambda h: h.activation(out=pT[:, 512:640], in_=sc[:, 512:640], func=AF.Exp), reads=[scb2], writes=[pTb])
            return blocks, pT, pTb

        def na_pv(i, hd, blocks, pT, pTb):
            nb = len(blocks)
            hc, hp = hd // 2, (hd % 2) * 64
            po, pob = po_ring.next()
            for bi, (j, var) in enumerate(blocks):
                S.op("tensor", lambda h, bi=bi, j=j: mm_(h,
                    po[:, 0:128], lhsT=vaug[:, j, hd, :], rhs=pT[:, bi * 128:(bi + 1) * 128],
                    start=(bi == 0), stop=(bi == nb - 1)), reads=[B_vaug, pTb], writes=[pob])
            rd, rdb = rden_ring.next()
            S.op("vector", lambda h: h.reciprocal(out=rd[64:128, :], in_=po[64:128, 0:128]), reads=[pob], writes=[rdb])
            S.op("vector", lambda h: h.tensor_tensor(
                out=onaT[hp:hp + 64, hc, i * 128:(i + 1) * 128], in0=po[0:64, 0:128], in1=rd[64:128, :], op=ALU.mult),
                reads=[pob, rdb], writes=[B_onaT])

        na_items = [(i, hd) for i in range(16) for hd in range(8)]
        pend = []
        for k_, (i, hd) in enumerate(na_items):
            pend.append((i, hd) + na_scores(i, hd))
            if len(pend) > 1:
                na_pv(*pend.pop(0))
        while pend:
            na_pv(*pend.pop(0))

        if "dbg_onaT" in D:
            S.dma("sync", lambda h: h.dma_start(out=D["dbg_onaT"], in_=onaT), reads=[B_onaT], writes=[Buf("dbg1")])
        S.barrier()

        if stage >= 2:
            RR = Region([(58, 142)])
            RT = Region([(142, 207)])
            qT = tile(RR, [4, T], BF16); kT = tile(RR, [4, T], BF16)
            ktok = tile(RR, [16, 512], BF16); vtok = tile(RR, [16, 512], BF16)
            B_qT, B_kT, B_ktok, B_vtok = Buf("qT"), Buf("kT"), Buf("ktok"), Buf("vtok")
            graw = tile(RR, [16, 32]); B_graw = Buf("graw")
            gv = tile(RR, [32]); nega = tile(RR, [16]); convT = tile(RR, [12, 5])
            tx = tile(RR, [16, 16]); tax = tile(RR, [16, 16]); te = tile(RR, [16, 16]); tsp = tile(RR, [16, 16])
            betat = tile(RR, [16, 16]); gt = tile(RR, [16, 16])
            gD = tile(RR, [2, 128]); betaD = tile(RR, [2, 128]); nbetaD = tile(RR, [2, 128])
            egc = tile(RR, [2, 128]); egd = tile(RR, [2, 128]); bg = tile(RR, [2, 128])
            EGL = tile(RR, [2, 2, 128])
            gDh = tile(RR, [2, 128], BF16); gDl = tile(RR, [2, 128], BF16); gDhf = tile(RR, [2, 128]); gDlf = tile(RR, [2, 128])
            B_g = Buf("gates")
            B_small = Buf("dnsmall")
            WL = WLoader(RT)
            cst_ring = Ring("cst", [tile(RT, [T + 4], BF16) for _ in range(2)])
            diagw = tile(RT, [12, 5, 128], BF16); B_diag = Buf("diagw")
            sil_ring = Ring("sil", [tile(RT, [T], BF16) for _ in range(2)])
            sq_ring = Ring("sq", [tile(RT, [512], BF16) for _ in range(2)])
            tmpf_ring = Ring("tmpf", [tile(RT, [512]) for _ in range(2)])

            S.dma("sync", lambda h: h.dma_start(out=gv, in_=D["gvec"].partition_broadcast(128)), writes=[B_small])
            S.dma("sync", lambda h: h.dma_start(out=convT.rearrange("p a b -> p (a b)"), in_=D["convT"]), writes=[B_small])
            for cs_ in cst_ring.views:
                pass
            for k_, (cs_, csb_) in enumerate(zip(cst_ring.views, cst_ring.bufs)):
                S.op("gpsimd", lambda h, cs_=cs_: h.memset(cs_[:, 0:2], 0.0), writes=[csb_])
                S.op("gpsimd", lambda h, cs_=cs_: h.memset(cs_[:, T + 2:T + 4], 0.0), writes=[csb_])

            wv, wb = WL.load(D["w_in"], OFF_G, ncols=32)
            proj_tm(pin_ring, wv, wb, 32, lambda ps, pb, t: copy_alt(graw[:, t, :], ps[:, 0:32], [pb], [B_graw]), xT, B_xT)
            G_ = [B_graw, B_small, B_g]
            S.op("scalar", lambda h: h.activation(out=betat, in_=graw[:, :, 0:16], func=AF.Sigmoid), reads=G_, writes=[B_g])
            S.op("vector", lambda h: h.tensor_tensor(out=tx, in0=graw[:, :, 16:32],
                                                     in1=gv[:, 16:32].unsqueeze(1).broadcast_to([128, 16, 16]), op=ALU.add),
                 reads=G_, writes=[B_g])
            S.op("vector", lambda h: h.scalar_tensor_tensor(out=tax, in0=tx, scalar=-1.0, in1=tx, op0=ALU.mult, op1=ALU.max), reads=G_, writes=[B_g])
            S.op("scalar", lambda h: h.activation(out=te, in_=tax, func=AF.Exp, scale=-1.0), reads=G_, writes=[B_g])
            S.op("scalar", lambda h: h.activation(out=te, in_=te, func=AF.Ln, bias=1.0), reads=G_, writes=[B_g])
            S.op("vector", lambda h: h.scalar_tensor_tensor(out=tsp, in0=tx, scalar=0.0, in1=te, op0=ALU.max, op1=ALU.add),
                 reads=G_, writes=[B_g])
            S.op("scalar", lambda h: h.activation(out=nega, in_=gv[:, 0:16], func=AF.Exp), reads=G_, writes=[B_g])
            S.op("vector", lambda h: h.tensor_scalar(out=nega, in0=nega, scalar1=-1.0, scalar2=None, op0=ALU.mult), reads=G_, writes=[B_g])
            S.op("vector", lambda h: h.tensor_tensor(out=gt, in0=tsp, in1=nega.unsqueeze(1).broadcast_to([128, 16, 16]), op=ALU.mult),
                 reads=G_, writes=[B_g])
            for d in range(2):
                S.op("vector", lambda h, d=d: h.tensor_copy(out=gD[:, d, :].rearrange("p (t h) -> p t h", t=16),
                                                            in_=gt[:, :, d * 8:(d + 1) * 8]), reads=G_, writes=[B_g])
                S.op("vector", lambda h, d=d: h.tensor_copy(out=betaD[:, d, :].rearrange("p (t h) -> p t h", t=16),
                                                            in_=betat[:, :, d * 8:(d + 1) * 8]), reads=G_, writes=[B_g])
            S.op("vector", lambda h: h.tensor_scalar(out=nbetaD, in0=betaD, scalar1=-1.0, scalar2=None, op0=ALU.mult), reads=G_, writes=[B_g])
            S.op("vector", lambda h: h.tensor_copy(out=gDh, in_=gD), reads=G_, writes=[B_g])
            S.op("vector", lambda h: h.tensor_copy(out=gDhf, in_=gDh), reads=G_, writes=[B_g])
            S.op("vector", lambda h: h.tensor_tensor(out=gDlf, in0=gD, in1=gDhf, op=ALU.subtract), reads=G_, writes=[B_g])
            S.op("vector", lambda h: h.tensor_copy(out=gDl, in_=gDlf), reads=G_, writes=[B_g])
            for d in range(2):
                C1 = C["A1"] if d == 0 else C["A1T"]
                C3 = C["C3f"] if d == 0 else C["C3b"]
                for lhs, dst in ((C1, egc[:, d, :]), (C3, egd[:, d, :]), (C["HA0"], EGL[:, 0, d, :]), (C["HA1"], EGL[:, 1, d, :])):
                    ps, pb = pin_ring.next()
                    S.op("tensor", lambda h, ps=ps, lhs=lhs, d=d: mm_(h, ps[:, 0:128], lhsT=lhs, rhs=gDh[:, d, :], start=True, stop=False),
                         reads=[B_cst, B_g], writes=[pb])
                    S.op("tensor", lambda h, ps=ps, lhs=lhs, d=d: mm_(h, ps[:, 0:128], lhsT=lhs, rhs=gDl[:, d, :], start=False, stop=True),
                         reads=[B_cst, B_g], writes=[pb])
                    S.op("scalar", lambda h, ps=ps, dst=dst: h.activation(out=dst, in_=ps[:, 0:128], func=AF.Exp), reads=[pb], writes=[B_g])
            S.op("vector", lambda h: h.tensor_tensor(out=bg, in0=betaD, in1=egc, op=ALU.mult), reads=G_, writes=[B_g])

            for ch12 in range(12):
                S.op("vector", lambda h, ch12=ch12: h.tensor_tensor(
                    out=diagw[:, ch12, :, :], in0=identb.unsqueeze(1).broadcast_to([128, 5, 128]),
                    in1=convT[:, ch12, :].unsqueeze(2).broadcast_to([128, 5, 128]), op=ALU.mult), reads=[B_cb, B_small], writes=[B_diag])
            chunks = [(grp, kind, off, g, fc) for grp, (off, kind) in enumerate([(OFF_QDN, "q"), (OFF_KDN, "k"), (OFF_VDN, "v")])
                      for g in range(2) for fc in range(2)]
            conv_ring = bank_ring("cv", [4, 5])
            aux_ring = bank_ring("aux", [6, 7])
            pin4 = bank_ring("pin4", [0, 1, 2, 3])
            wcur = {}
            cs_of, sil_of = {}, {}

            def st_A(ci):
                grp, kind, off, g, fc = chunks[ci]
                if fc == 0:
                    wcur[(grp, g)] = WL.load(D["w_in"], off + g * 256)
                wv, wb = wcur[(grp, g)]
                cs, csb = cst_ring.next()
                cs_of[ci] = (cs, csb)
                for tb in range(4):
                    ps, pb = pin4.next()
                    for c in range(8):
                        S.op("tensor", lambda h, ps=ps, c=c, tb=tb: mm_(h,
                            ps, lhsT=wv[:, c, fc * 128:(fc + 1) * 128], rhs=xT[:, c, tb * 512:(tb + 1) * 512],
                            start=(c == 0), stop=(c == 7)), reads=[wb, B_xT], writes=[pb])
                    copy_alt(cs[:, 2 + tb * 512:2 + (tb + 1) * 512], ps, [pb], [csb])

            def st_B(ci):
                grp, kind, off, g, fc = chunks[ci]
                ch12 = grp * 4 + g * 2 + fc
                cs, csb = cs_of[ci]
                sl, slb = sil_ring.next()
                sil_of[ci] = (sl, slb)
                for tb in range(4):
                    ps, pb = conv_ring.next()
                    for tau in range(5):
                        S.op("tensor", lambda h, ps=ps, tau=tau, tb=tb: mm_(h,
                            ps, lhsT=diagw[:, ch12, tau, :], rhs=cs[:, tb * 512 + tau:tb * 512 + tau + 512],
                            start=(tau == 0), stop=(tau == 4)), reads=[B_diag, csb], writes=[pb])
                    S.op("scalar", lambda h, ps=ps, tb=tb: h.activation(out=sl[:, tb * 512:(tb + 1) * 512], in_=ps, func=AF.Silu),
                         reads=[pb], writes=[slb])

            def st_C(ci):
                grp, kind, off, g, fc = chunks[ci]
                fcg = g * 2 + fc
                if kind == "v":
                    return
                sl, slb = sil_of[ci]
                for tb in range(4):
                    sq_, sqb_ = sq_ring.next()
                    S.op("scalar", lambda h, sq_=sq_, tb=tb: h.activation(out=sq_, in_=sl[:, tb * 512:(tb + 1) * 512], func=AF.Square),
                         reads=[slb], writes=[sqb_])
                    ps, pb = aux_ring.next()
                    S.op("tensor", lambda h, ps=ps, sq_=sq_: mm_(h, ps, lhsT=CBn["H"], rhs=sq_, start=True, stop=True),
                         reads=[B_cb, sqb_], writes=[pb])
                    tf_, tfb_ = tmpf_ring.next()
                    S.op("scalar", lambda h, ps=ps, tf_=tf_: h.activation(out=tf_, in_=ps, func=AF.Sqrt, bias=RMS_EPS), reads=[pb], writes=[tfb_])
                    S.op("vector", lambda h, tf_=tf_: h.reciprocal(out=tf_, in_=tf_), reads=[tfb_], writes=[tfb_])
                    if kind == "q":
                        S.op("vector", lambda h, tf_=tf_, tb=tb: h.scalar_tensor_tensor(
                            out=qT[:, fcg, tb * 512:(tb + 1) * 512], in0=sl[:, tb * 512:(tb + 1) * 512], scalar=0.125, in1=tf_,
                            op0=ALU.mult, op1=ALU.mult), reads=[slb, tfb_], writes=[B_qT])
                    else:
                        S.op("vector", lambda h, tf_=tf_, tb=tb: h.tensor_tensor(
                            out=kT[:, fcg, tb * 512:(tb + 1) * 512], in0=sl[:, tb * 512:(tb + 1) * 512], in1=tf_, op=ALU.mult),
                            reads=[slb, tfb_], writes=[B_kT])

            def st_D(ci):
                grp, kind, off, g, fc = chunks[ci]
                fcg = g * 2 + fc
                if kind == "q":
                    return
                if kind == "v":
                    sl, slb = sil_of[ci]
                    src_, srcb, dst_, dstb = (lambda a, b: sl[:, a:b]), slb, vtok, B_vtok
                else:
                    src_, srcb, dst_, dstb = (lambda a, b: kT[:, fcg, a:b]), B_kT, ktok, B_ktok
                for t4 in range(4):
                    ps, pb = aux_ring.next()
                    for u in range(4):
                        tt_ = t4 * 4 + u
                        S.op("tensor", lambda h, ps=ps, u=u, tt_=tt_: mm_(h,
                            ps[:, u * 128:(u + 1) * 128], lhsT=src_(tt_ * 128, (tt_ + 1) * 128), rhs=identb,
                            start=True, stop=True), reads=[srcb, B_cb], writes=[pb])
                    copy_alt(dst_[:, t4 * 4:(t4 + 1) * 4, fcg * 128:(fcg + 1) * 128], v3(ps, 4), [pb], [dstb])

            nch = len(chunks)
            for step in range(nch + 3):
                if 0 <= step - 3 < nch:
                    st_D(step - 3)
                if 0 <= step - 2 < nch:
                    st_C(step - 2)
                if 0 <= step - 1 < nch:
                    st_B(step - 1)
                if step < nch:
                    st_A(step)
            S.barrier()

        if stage >= 3:
            RO = Region([(26, 42)])
            RM = Region([(42, 58), (142, 207)])
            o_dn = tile(RO, [16, 512], BF16)
            B_odn = [Buf(f"odn{t}") for t in range(16)]
            TMP = []
            for d_ in range(2):
                TMP.append(dict(
                    rhsDh=tile(RM, [8, 128], BF16), rhsDl=tile(RM, [8, 128], BF16), B_rhsD=Buf(f"rhsD{d_}"),
                    Eb=tile(RM, [8, 128]), B_E=Buf(f"E{d_}"),
                    P_ring=Ring(f"P{d_}", [tile(RM, [8, 128], BF16) for _ in range(2)]),
                    PT_ring=Ring(f"PT{d_}", [tile(RM, [8, 128], BF16) for _ in range(2)]),
                    X=tile(RM, [8, 128], BF16), B_X=Buf(f"X{d_}"),
                    vb=tile(RM, [512], BF16), kbe=tile(RM, [512], BF16), B_vk=Buf(f"vbkbe{d_}")))
            sets = []
            for k_ in range(4):
                sets.append(dict(WT=tile(RM, [8, 128], BF16), U=tile(RM, [512], BF16), IT=tile(RM, [8, 128], BF16),
                                 KD=tile(RM, [512], BF16), b=Buf(f"set{k_}")))
            Sst = tile(RM, [2, 4, 64]); Sbf = tile(RM, [2, 4, 64], BF16)
            B_S = [Buf("S0"), Buf("S1")]; B_Sbf = [Buf("Sbf0"), Buf("Sbf1")]
            vnew = tile(RM, [2, 512], BF16); B_vnew = [Buf("vn0"), Buf("vn1")]
            ot_ring = Ring("ot", [tile(RM, [512]) for _ in range(2)])
            lring = bank_ring("lps", [0, 1, 2, 3])
            sring = bank_ring("sps", [4, 5, 6, 7])
            EGL5 = EGL.rearrange("q a d (t hc hp) -> q a d t hc hp", t=16, hc=4, hp=2)

            S.op("gpsimd", lambda h: h.memset(Sst, 0.0), writes=B_S)
            S.op("gpsimd", lambda h: h.memset(Sbf, 0.0), writes=B_Sbf)

            def local(t, d, st_):
                T_ = TMP[d]
                rhsDh, rhsDl, B_rhsD, Eb, B_E = T_["rhsDh"], T_["rhsDl"], T_["B_rhsD"], T_["Eb"], T_["B_E"]
                P_ring, PT_ring, X, B_X, vb, kbe, B_vk = T_["P_ring"], T_["PT_ring"], T_["X"], T_["B_X"], T_["vb"], T_["kbe"], T_["B_vk"]
                C1 = C["A1"] if d == 0 else C["A1T"]
                C2 = C["A2"] if d == 0 else C["A2T"]
                NEGS = CBn["NEGSf"] if d == 0 else CBn["NEGSb"]
                NEG2 = CBn["NEG2f"] if d == 0 else CBn["NEG2b"]
                col = lambda h_: t * 8 + h_
                sb_ = st_["b"]

                def decay(Cl, Cr, NEGm):
                    if os.environ.get("RHSD_ACT", "0") == "1":
                        for h_ in range(8):
                            for dst, src in ((rhsDh, gDhf), (rhsDl, gDlf)):
                                S.op("scalar", lambda h, dst=dst, src=src, h_=h_: h.activation(
                                    out=dst[:, h_, :], in_=Cr, func=AF.Identity, scale=src[:, d, col(h_):col(h_) + 1]),
                                    reads=[B_cst, B_g], writes=[B_rhsD])
                    else:
                        for dst, src in ((rhsDh, gDhf), (rhsDl, gDlf)):
                            S.op("gpsimd", lambda h, dst=dst, src=src: h.tensor_tensor(
                                out=dst, in0=Cr.unsqueeze(1).broadcast_to([128, 8, 128]),
                                in1=src[:, d, t * 8:(t + 1) * 8].unsqueeze(2).broadcast_to([128, 8, 128]), op=ALU.mult),
                                reads=[B_cst, B_g], writes=[B_rhsD])
                    for half in range(2):
                        ps, pb = lring.next()
                        for hh in range(4):
                            h_ = half * 4 + hh
                            S.op("tensor", lambda h, ps=ps, hh=hh: mm_(h, ps[:, hh * 128:(hh + 1) * 128], lhsT=identb, rhs=NEGm,
                                                                       start=True, stop=False), reads=[B_cb], writes=[pb])
                            S.op("tensor", lambda h, ps=ps, hh=hh, h_=h_: mm_(h, ps[:, hh * 128:(hh + 1) * 128], lhsT=Cl, rhs=rhsDh[:, h_, :],
                                                                              start=False, stop=False), reads=[B_cst, B_rhsD], writes=[pb])
                            S.op("tensor", lambda h, ps=ps, hh=hh, h_=h_: mm_(h, ps[:, hh * 128:(hh + 1) * 128], lhsT=Cl, rhs=rhsDl[:, h_, :],
                                                                              start=False, stop=True), reads=[B_cst, B_rhsD], writes=[pb])
                        S.op("scalar", lambda h, ps=ps, half=half: h.activation(out=Eb[:, half * 4:(half + 1) * 4, :], in_=v3(ps, 4), func=AF.Exp),
                             reads=[pb], writes=[B_E])

                decay(C1, C2, NEGS)
                yield
                Pm, Pmb = P_ring.next()
                for par in range(2):
                    ps, pb = lring.next()
                    hp = par * 64
                    for hh in range(4):
                        S.op("tensor", lambda h, ps=ps, hh=hh, hp=hp: mm_(h,
                            ps[:, hh * 128:(hh + 1) * 128], lhsT=kT[hp:hp + 64, hh, t * 128:(t + 1) * 128],
                            rhs=kT[hp:hp + 64, hh, t * 128:(t + 1) * 128], start=True, stop=True), reads=[B_kT], writes=[pb])
                    for hh in range(4):
                        h_ = hh * 2 + par
                        S.op("vector", lambda h, ps=ps, hh=hh, h_=h_, Pm=Pm: h.scalar_tensor_tensor(
                            out=Pm[:, h_, :], in0=ps[:, hh * 128:(hh + 1) * 128], scalar=nbetaD[:, d, col(h_):col(h_) + 1],
                            in1=Eb[:, h_, :], op0=ALU.mult, op1=ALU.mult), reads=[pb, B_g, B_E], writes=[Pmb])
                yield
                PTm, PTmb = PT_ring.next()
                for half in range(2):
                    ps, pb = lring.next()
                    for hh in range(4):
                        h_ = half * 4 + hh
                        S.op("tensor", lambda h, ps=ps, hh=hh, h_=h_, Pm=Pm: mm_(h, ps[:, hh * 128:(hh + 1) * 128], lhsT=Pm[:, h_, :], rhs=identb,
                                                                                      start=True, stop=True), reads=[Pmb, B_cb], writes=[pb])
                    S.op("scalar", lambda h, ps=ps, half=half, PTm=PTm: h.copy(out=PTm[:, half * 4:(half + 1) * 4, :], in_=v3(ps, 4)),
                         reads=[pb], writes=[PTmb])
                    S.op("vector", lambda h, ps=ps, half=half: h.tensor_tensor(
                        out=X[:, half * 4:(half + 1) * 4, :], in0=v3(ps, 4), in1=identf.unsqueeze(1).broadcast_to([128, 4, 128]), op=ALU.add),
                        reads=[pb, B_cst], writes=[B_X])
                yield
                for m in range(6):
                    last = (m == 5)
                    if not last:
                        Pn, Pnb = P_ring.next()
                        PTn, PTnb = PT_ring.next()
                    for half in range(2):
                        hs = [half * 4 + hh for hh in range(4)]
                        if not last:
                            psA, pbA = lring.next()
                            for hh, h_ in enumerate(hs):
                                S.op("tensor", lambda h, psA=psA, hh=hh, h_=h_, Pm=Pm, PTm=PTm: mm_(h,
                                    psA[:, hh * 128:(hh + 1) * 128], lhsT=PTm[:, h_, :], rhs=Pm[:, h_, :], start=True, stop=True),
                                    reads=[Pmb, PTmb], writes=[pbA])
                            S.op("scalar", lambda h, psA=psA, half=half, Pn=Pn: h.copy(out=Pn[:, half * 4:(half + 1) * 4, :], in_=v3(psA, 4)),
                                 reads=[pbA], writes=[Pnb])
                            psB, pbB = lring.next()
                            for hh, h_ in enumerate(hs):
                                S.op("tensor", lambda h, psB=psB, hh=hh, h_=h_, Pm=Pm, PTm=PTm: mm_(h,
                                    psB[:, hh * 128:(hh + 1) * 128], lhsT=Pm[:, h_, :], rhs=PTm[:, h_, :], start=True, stop=True),
                                    reads=[Pmb, PTmb], writes=[pbB])
                            S.op("scalar", lambda h, psB=psB, half=half, PTn=PTn: h.copy(out=PTn[:, half * 4:(half + 1) * 4, :], in_=v3(psB, 4)),
                                 reads=[pbB], writes=[PTnb])
                        if m >= 1:
                            psC, pbC = lring.next()
                            for hh, h_ in enumerate(hs):
                                S.op("tensor", lambda h, psC=psC, hh=hh, h_=h_, Pm=Pm: mm_(h,
                                    psC[:, hh * 128:(hh + 1) * 128], lhsT=Pm[:, h_, :], rhs=X[:, h_, :], start=True, stop=True),
                                    reads=[Pmb, B_X], writes=[pbC])
                            S.op("vector", lambda h, psC=psC, half=half: h.tensor_tensor(
                                out=X[:, half * 4:(half + 1) * 4, :], in0=v3(psC, 4), in1=X[:, half * 4:(half + 1) * 4, :], op=ALU.add),
                                reads=[pbC, B_X], writes=[B_X])
                    if not last:
                        Pm, Pmb, PTm, PTmb = Pn, Pnb, PTn, PTnb
                    yield
                for dst, src, sc_, wbuf in ((vb, vtok, betaD, B_vk), (kbe, ktok, bg, B_vk), (st_["KD"], ktok, egd, sb_)):
                    S.op("gpsimd", lambda h, dst=dst, src=src, sc_=sc_: h.tensor_tensor(
                        out=v3(dst, 8), in0=v3(src[:, t, :], 8),
                        in1=sc_[:, d, t * 8:(t + 1) * 8].unsqueeze(2).broadcast_to([128, 8, 64]), op=ALU.mult),
                        reads=[B_vtok, B_ktok, B_g], writes=[wbuf])
                psU, pbU = lring.next()
                for h_ in range(8):
                    S.op("tensor", lambda h, psU=psU, h_=h_: mm_(h, psU[:, h_ * 64:(h_ + 1) * 64], lhsT=X[:, h_, :], rhs=vb[:, h_ * 64:(h_ + 1) * 64],
                                                                      start=True, stop=True), reads=[B_X, B_vk], writes=[pbU])
                S.op("scalar", lambda h, psU=psU: h.copy(out=st_["U"], in_=psU), reads=[pbU], writes=[sb_])
                WT4 = st_["WT"].rearrange("p (hc hp) c -> p hc hp c", hp=2)
                for half in range(2):
                    psW, pbW = lring.next()
                    for hh in range(4):
                        h_ = half * 4 + hh
                        hc = h_ // 2
                        S.op("tensor", lambda h, psW=psW, hh=hh, h_=h_, hc=hc: mm_(h,
                            psW[:, hh * 128:(hh + 1) * 128], lhsT=kbe[:, hc * 128:(hc + 1) * 128], rhs=X[:, h_, :], start=True, stop=True),
                            reads=[B_vk, B_X], writes=[pbW])
                    pw4 = psW.rearrange("p (hc hp c) -> p hc hp c", hc=2, hp=2)
                    for p_ in range(2):
                        S.op("vector", lambda h, pw4=pw4, p_=p_, half=half: h.tensor_copy(
                            out=WT4[p_ * 64:(p_ + 1) * 64, half * 2:(half + 1) * 2, p_, :], in_=pw4[p_ * 64:(p_ + 1) * 64, :, p_, :]),
                            reads=[pbW], writes=[sb_])
                yield
                decay(C2, C1, NEG2)
                IT4 = st_["IT"].rearrange("p (hc hp) c -> p hc hp c", hp=2)
                Eb4 = Eb.rearrange("p (hc hp) c -> p hc hp c", hp=2)
                for par in range(2):
                    ps, pb = lring.next()
                    hp = par * 64
                    for hh in range(4):
                        S.op("tensor", lambda h, ps=ps, hh=hh, hp=hp: mm_(h,
                            ps[:, hh * 128:(hh + 1) * 128], lhsT=kT[hp:hp + 64, hh, t * 128:(t + 1) * 128],
                            rhs=qT[hp:hp + 64, hh, t * 128:(t + 1) * 128], start=True, stop=True), reads=[B_kT, B_qT], writes=[pb])
                    S.op("vector", lambda h, ps=ps, par=par: h.tensor_tensor(
                        out=IT4[:, :, par, :], in0=v3(ps, 4), in1=Eb4[:, :, par, :], op=ALU.mult),
                        reads=[pb, B_E], writes=[sb_])

            def scan_d(tt, k, d):
                if True:
                    a = k if d == 0 else 1 - k
                    t = tt if d == 0 else 15 - tt
                    st_ = sets[(tt % 2) * 2 + d]
                    sb_ = st_["b"]
                    r0 = 64 * a
                    rows = slice(r0, r0 + 64)
                    pe_, peb = sring.next()
                    po_, pob_ = sring.next()
                    pbk = [(pe_, peb), (po_, pob_)]
                    for par in range(2):
                        hp = par * 64
                        bk, bkb = pbk[par]
                        for hc in range(4):
                            h_ = hc * 2 + par
                            S.op("tensor", lambda h, bk=bk, h_=h_, hc=hc, hp=hp: mm_(h,
                                bk[:, hc * 64:(hc + 1) * 64], lhsT=st_["WT"][hp:hp + 64, h_, :], rhs=Sbf[hp:hp + 64, d, hc, :],
                                start=True, stop=True), reads=[sb_, B_Sbf[d]], writes=[bkb])
                        for hc in range(4):
                            S.op("tensor", lambda h, bk=bk, hc=hc, hp=hp: mm_(h,
                                bk[:, 256 + hc * 64:256 + (hc + 1) * 64], lhsT=qT[hp:hp + 64, hc, t * 128:(t + 1) * 128], rhs=Sbf[hp:hp + 64, d, hc, :],
                                start=True, stop=True), reads=[B_qT, B_Sbf[d]], writes=[bkb])
                    vnew5 = vnew.rearrange("p d (hc hp v) -> p d hc hp v", hc=4, hp=2)
                    U4 = st_["U"].rearrange("p (hc hp v) -> p hc hp v", hc=4, hp=2)
                    for par in range(2):
                        bk, bkb = pbk[par]
                        S.op("vector", lambda h, bk=bk, par=par: h.tensor_tensor(
                            out=vnew5[rows, d, :, par, :], in0=U4[rows, :, par, :], in1=v3(bk[rows, 0:256], 4), op=ALU.subtract),
                            reads=[sb_, bkb], writes=[B_vnew[d]])
                    yield
                    pi, pib = sring.next()
                    for h_ in range(8):
                        S.op("tensor", lambda h, pi=pi, h_=h_: mm_(h,
                            pi[:, h_ * 64:(h_ + 1) * 64], lhsT=st_["IT"][rows, h_, :], rhs=vnew[rows, d, h_ * 64:(h_ + 1) * 64],
                            start=True, stop=True), reads=[sb_, B_vnew[d]], writes=[pib])
                    ot, otb = ot_ring.next()
                    ot4 = ot.rearrange("p (hc hp v) -> p hc hp v", hc=4, hp=2)
                    egc5 = egc.rearrange("p d (t hc hp) -> p d t hc hp", t=16, hc=4, hp=2)
                    for par in range(2):
                        bk, bkb = pbk[par]
                        S.op("vector", lambda h, ot4=ot4, bk=bk, par=par: h.tensor_tensor(
                            out=ot4[rows, :, par, :], in0=v3(bk[rows, 256:512], 4),
                            in1=egc5[rows, d, t, :, par].unsqueeze(2).broadcast_to([64, 4, 64]), op=ALU.mult),
                            reads=[bkb, B_g], writes=[otb])
                    first = (d == 0) == (t < 8)
                    if first:
                        S.op("vector", lambda h, ot=ot, pi=pi: h.tensor_tensor(out=o_dn[rows, t, :], in0=ot[rows, :], in1=pi[rows, :], op=ALU.add),
                             reads=[otb, pib], writes=[B_odn[t]])
                    else:
                        S.op("vector", lambda h, ot=ot, pi=pi: h.tensor_tensor(out=ot[rows, :], in0=ot[rows, :], in1=pi[rows, :], op=ALU.add),
                             reads=[otb, pib], writes=[otb])
                        S.op("gpsimd", lambda h, ot=ot: h.tensor_tensor(out=o_dn[rows, t, :], in0=o_dn[rows, t, :], in1=ot[rows, :], op=ALU.add),
                             reads=[otb, B_odn[t]], writes=[B_odn[t]])
                    yield
                    psu, psub = sring.next()
                    for h_ in range(8):
                        hc = h_ // 2
                        S.op("tensor", lambda h, psu=psu, h_=h_, hc=hc: mm_(h,
                            psu[:, h_ * 64:(h_ + 1) * 64], lhsT=st_["KD"][rows, hc * 128:(hc + 1) * 128], rhs=vnew[rows, d, h_ * 64:(h_ + 1) * 64],
                            start=True, stop=True), reads=[sb_, B_vnew[d]], writes=[psub])
                    psu4 = psu.rearrange("q (hc hp v) -> q hc hp v", hc=4, hp=2)
                    for p_ in range(2):
                        pr = slice(p_ * 64, (p_ + 1) * 64)
                        S.op("vector", lambda h, pr=pr, p_=p_: h.tensor_tensor(
                            out=Sst[pr, d, :, :], in0=Sst[pr, d, :, :],
                            in1=EGL5[pr, a, d, t, :, p_].unsqueeze(2).broadcast_to([64, 4, 64]), op=ALU.mult),
                            reads=[B_S[d], B_g], writes=[B_S[d]])
                        S.op("vector", lambda h, pr=pr, p_=p_, psu4=psu4: h.tensor_tensor(
                            out=Sst[pr, d, :, :], in0=Sst[pr, d, :, :], in1=psu4[pr, :, p_, :], op=ALU.add),
                            reads=[B_S[d], psub], writes=[B_S[d]])
                        S.op("scalar", lambda h, pr=pr: h.copy(out=Sbf[pr, d, :, :], in_=Sst[pr, d, :, :]), reads=[B_S[d]], writes=[B_Sbf[d]])

            import itertools

            def drive(gens):
                live = list(gens)
                while live:
                    for g_ in list(live):
                        try:
                            next(g_)
                        except StopIteration:
                            live.remove(g_)

            def scan_gen(tt):
                return itertools.chain(scan_d(tt, 0, 0), scan_d(tt, 0, 1), scan_d(tt, 1, 0), scan_d(tt, 1, 1))

            for tt in range(16):
                gens = [local(tt, 0, sets[(tt % 2) * 2 + 0]), local(15 - tt, 1, sets[(tt % 2) * 2 + 1])]
                if tt > 0:
                    gens.insert(0, scan_gen(tt - 1))
                drive(gens)
            drive([scan_gen(15)])

            if "dbg_odn" in D:
                S.dma("sync", lambda h: h.dma_start(out=D["dbg_odn"], in_=o_dn), reads=B_odn, writes=[Buf("dbg2")])
            S.barrier()


        def layer_norm_tile(r, gb, out, st, junk, Bs_r, B_gb, B_out, B_st, B_junk, eng_mul="vector"):
            S.op("vector", lambda h: h.memset(st[:, 0:2], 0.0), writes=[B_st])
            S.op("scalar", lambda h: h.activation(out=junk, in_=r, func=AF.Identity, accum_out=st[:, 0:1]), reads=Bs_r + [B_st], writes=[B_junk, B_st])
            S.op("scalar", lambda h: h.activation(out=junk, in_=r, func=AF.Square, accum_out=st[:, 1:2]), reads=Bs_r + [B_st], writes=[B_junk, B_st])
            S.op("vector", lambda h: h.tensor_scalar(out=st[:, 2:3], in0=st[:, 0:1], scalar1=1.0 / DM, scalar2=None, op0=ALU.mult), reads=[B_st], writes=[B_st])
            S.op("vector", lambda h: h.tensor_tensor(out=st[:, 3:4], in0=st[:, 2:3], in1=st[:, 2:3], op=ALU.mult), reads=[B_st], writes=[B_st])
            S.op("vector", lambda h: h.scalar_tensor_tensor(out=st[:, 4:5], in0=st[:, 1:2], scalar=1.0 / DM, in1=st[:, 3:4], op0=ALU.mult, op1=ALU.subtract),
                 reads=[B_st], writes=[B_st])
            S.op("scalar", lambda h: h.activation(out=st[:, 5:6], in_=st[:, 4:5], func=AF.Sqrt, bias=LN_EPS), reads=[B_st], writes=[B_st])
            S.op("vector", lambda h: h.reciprocal(out=st[:, 6:7], in_=st[:, 5:6]), reads=[B_st], writes=[B_st])
            S.op("vector", lambda h: h.tensor_scalar(out=out, in0=r, scalar1=st[:, 2:3], scalar2=st[:, 6:7], op0=ALU.subtract, op1=ALU.mult),
                 reads=Bs_r + [B_st], writes=[B_out])
            S.op(eng_mul, lambda h: h.tensor_tensor(out=out, in0=out, in1=gb[:, 0:DM], op=ALU.mult), reads=[B_out, B_gb], writes=[B_out])
            S.op(eng_mul, lambda h: h.tensor_tensor(out=out, in0=out, in1=gb[:, DM:2 * DM], op=ALU.add), reads=[B_out, B_gb], writes=[B_out])

        if stage >= 5:
            RP = Region([(58, 174)])
            mergedT = tile(Region([(174, 206)]), [8, T], BF16); B_mT = Buf("mergedT")
            xT2 = tile(RP, [8, T], BF16); B_xT2 = Buf("xT2")
            odnT = tile(RP, [4, T], BF16); B_odnT = Buf("odnT")
            wz = tile(RP, [8, 512], BF16); B_wz = Buf("wz")
            WL = WLoader(RP, nslots=3)
            normw = tile(RP, [64]); B_nw = Buf("normw")
            sz_ring = Ring("sz", [tile(RP, [512]) for _ in range(2)])
            sq_ring = Ring("sq", [tile(RP, [512]) for _ in range(2)])
            of_ring = Ring("of", [tile(RP, [512], BF16) for _ in range(2)])
            ss_ring = Ring("ss", [tile(RP, [16]) for _ in range(2)])
            tA_ring = Ring("tA", [tile(RP, [512]) for _ in range(2)])
            tB_ring = Ring("tB", [tile(RP, [512]) for _ in range(2)])
            pz_ring = bank_ring("pz", [0, 1])
            pt_ring = bank_ring("pt", [2, 3])
            pm_ring = bank_ring("pm", [4, 5, 6, 7])

            load_xT(xT2, B_xT2, WL.wst)
            S.dma("sync", lambda h: h.dma_start(out=normw, in_=D["norm_w"].partition_broadcast(128)), writes=[B_nw])
            for g in range(2):
                WL.load(D["w_in"], OFF_ZDN + g * 256, dest=(wz[:, :, g * 256:(g + 1) * 256], B_wz))

            def dn_post(t):
                psz, pzb = pz_ring.next()
                for c in range(8):
                    S.op("tensor", lambda h, c=c: mm_(h, psz, lhsT=xT2[:, c, t * 128:(t + 1) * 128], rhs=wz[:, c, :],
                                                      start=(c == 0), stop=(c == 7)), reads=[B_xT2, B_wz], writes=[pzb])
                yield
                sz, szb = sz_ring.next()
                S.op("scalar", lambda h: h.activation(out=sz, in_=psz, func=AF.Silu), reads=[pzb], writes=[szb])
                sq, sqb_ = sq_ring.next()
                ss, ssb = ss_ring.next()
                o_t = o_dn[:, t, :]
                S.op("vector", lambda h: h.tensor_tensor(out=sq, in0=o_t, in1=o_t, op=ALU.mult), reads=[B_odn[t]], writes=[sqb_])
                yield
                S.op("vector", lambda h: h.tensor_reduce(out=ss[:, 0:8], in_=v3(sq, 8), axis=AX.X, op=ALU.add), reads=[sqb_], writes=[ssb])
                S.op("vector", lambda h: h.tensor_scalar(out=ss[:, 0:8], in0=ss[:, 0:8], scalar1=1.0 / 64, scalar2=RMS_EPS, op0=ALU.mult, op1=ALU.add),
                     reads=[ssb], writes=[ssb])
                yield
                S.op("scalar", lambda h: h.activation(out=ss[:, 8:16], in_=ss[:, 0:8], func=AF.Sqrt), reads=[ssb], writes=[ssb])
                yield
                S.op("vector", lambda h: h.reciprocal(out=ss[:, 8:16], in_=ss[:, 8:16]), reads=[ssb], writes=[ssb])
                S.op("vector", lambda h: h.tensor_tensor(out=v3(sq, 8), in0=v3(o_t, 8), in1=ss[:, 8:16].unsqueeze(2).broadcast_to([128, 8, 64]), op=ALU.mult),
                     reads=[B_odn[t], ssb], writes=[sqb_])
                S.op("vector", lambda h: h.tensor_tensor(out=v3(sq, 8), in0=v3(sq, 8), in1=normw.unsqueeze(1).broadcast_to([128, 8, 64]), op=ALU.mult),
                     reads=[B_nw], writes=[sqb_])
                yield
                of, ofb = of_ring.next()
                S.op("vector", lambda h: h.tensor_tensor(out=of, in0=sq, in1=sz, op=ALU.mult), reads=[sqb_, szb], writes=[ofb])
                yield
                pst, ptb = pt_ring.next()
                for fc in range(4):
                    S.op("tensor", lambda h, fc=fc: mm_(h, pst[:, fc * 128:(fc + 1) * 128], lhsT=of[:, fc * 128:(fc + 1) * 128], rhs=identb,
                                                        start=True, stop=True), reads=[ofb, B_cb], writes=[ptb])
                S.op("scalar", lambda h: h.copy(out=odnT[:, :, t * 128:(t + 1) * 128], in_=v3(pst, 4)), reads=[ptb], writes=[B_odnT])

            def merged_block(part, wp, wpb, wg, wgb, fc, fcg, tb, oT, B_oT):
                ps1, p1b = pm_ring.next()
                for c in range(4):
                    S.op("tensor", lambda h, c=c: mm_(h, ps1, lhsT=wp[:, c, fc * 128:(fc + 1) * 128], rhs=oT[:, c, tb * 512:(tb + 1) * 512],
                                                      start=(c == 0), stop=(c == 3)), reads=[wpb, B_oT], writes=[p1b])
                ps2, p2b = pm_ring.next()
                for c in range(8):
                    S.op("tensor", lambda h, c=c: mm_(h, ps2, lhsT=wg[:, c, fc * 128:(fc + 1) * 128], rhs=xT2[:, c, tb * 512:(tb + 1) * 512],
                                                      start=(c == 0), stop=(c == 7)), reads=[wgb, B_xT2], writes=[p2b])
                tA, tAb = tA_ring.next()
                S.op("scalar", lambda h: h.activation(out=tA, in_=ps2, func=AF.Sigmoid), reads=[p2b], writes=[tAb])
                dst = mergedT[:, fcg, tb * 512:(tb + 1) * 512]
                if part == 0:
                    S.op("vector", lambda h: h.tensor_tensor(out=dst, in0=ps1, in1=tA, op=ALU.mult), reads=[p1b, tAb], writes=[B_mT])
                else:
                    tB, tBb = tB_ring.next()
                    S.op("vector", lambda h: h.tensor_tensor(out=tB, in0=ps1, in1=tA, op=ALU.mult), reads=[p1b, tAb], writes=[tBb])
                    S.op("vector", lambda h: h.tensor_tensor(out=dst, in0=dst, in1=tB, op=ALU.add), reads=[tBb, B_mT], writes=[B_mT])

            def p3a_part(part):
                for g in range(4):
                    wp, wpb = WL.load(D["w_proj_na"] if part == 0 else D["w_proj_dn"], g * 256, kchunks=4, cast_eng="scalar")
                    wg, wgb = WL.load(D["w_in"], (OFF_GNA if part == 0 else OFF_GDN) + g * 256, cast_eng="scalar")
                    for fc in range(2):
                        for tb in range(4):
                            merged_block(part, wp, wpb, wg, wgb, fc, g * 2 + fc, tb,
                                         onaT if part == 0 else odnT, B_onaT if part == 0 else B_odnT)
                            yield

            def dn_post_all():
                for t in range(16):
                    yield from dn_post(t)

            def drive3(gens):
                live = list(gens)
                while live:
                    for g_ in list(live):
                        try:
                            next(g_)
                        except StopIteration:
                            live.remove(g_)

            drive3([dn_post_all(), p3a_part(0)])
            if "dbg_odnT" in D:
                S.dma("sync", lambda h: h.dma_start(out=D["dbg_odnT"], in_=odnT), reads=[B_odnT], writes=[Buf("dbg3")])
            drive3([p3a_part(1)])
            S.barrier()

        if stage >= 6:
            RB = Region([(10, 174)])
            acc = tile(RB, [16, DM]); B_acct = [Buf(f"acc{t}") for t in range(16)]
            x1T = tile(RB, [8, T], BF16); B_x1T = Buf("x1T")
            comb = tile(RB, [16, 32]); B_comb = Buf("comb")
            wout = tile(RB, [8, DM], BF16); B_wout = Buf("wout")
            ln1gb = tile(RB, [2 * DM]); B_ln1 = Buf("ln1gb")
            wrf = tile(RB, [8, 36]); wrh = tile(RB, [8, 36], BF16); wrl = tile(RB, [8, 36], BF16); brb = tile(RB, [36]); B_wr = Buf("wr")
            WLb = WLoader(RB, nslots=2, ncols=128)
            xt_ring = Ring("xt", [tile(RB, [DM]) for _ in range(2)])
            r_ring = Ring("r", [tile(RB, [DM]) for _ in range(1)])
            x1_ring = Ring("x1", [tile(RB, [DM]) for _ in range(1)])
            x1h_ring = Ring("x1h", [tile(RB, [DM], BF16) for _ in range(2)])
            x1l_ring = Ring("x1l", [tile(RB, [DM], BF16) for _ in range(2)])
            x1Tl_ring = Ring("x1Tl", [tile(RB, [8, 128], BF16) for _ in range(1)])
            st_ring = Ring("st", [tile(RB, [8]) for _ in range(2)])
            sm_ring = Ring("sm", [tile(RB, [128]) for _ in range(2)])
            pm2_ring = Ring("pm2", [psum[:, 0:1024], psum[:, 1024:2048]])
            pm2_bufs = [[BANKB[0], BANKB[1]], [BANKB[2], BANKB[3]]]
            pth_b = [BANKB[4], BANKB[5]]
            ptl_b = [BANKB[6], BANKB[7]]
            pth = psum[:, 2048:3072]
            ptl = psum[:, 3072:4096]

            for g in range(8):
                WLb.load(D["w_out"], g * 128, dest=(wout[:, :, g * 128:(g + 1) * 128], B_wout), cast_eng="scalar")
            S.dma("sync", lambda h: h.dma_start(out=ln1gb, in_=D["ln1"].partition_broadcast(128)), writes=[B_ln1])
            S.dma("sync", lambda h: h.dma_start(out=brb, in_=D["b_r"].partition_broadcast(128)), writes=[B_wr])
            S.dma("sync", lambda h: h.dma_start(out=wrf, in_=D["w_r"].rearrange("(c p) n -> p c n", p=128)), writes=[B_wr])
            S.op("vector", lambda h: h.tensor_copy(out=wrh, in_=wrf), reads=[B_wr], writes=[B_wr])
            S.op("vector", lambda h: h.tensor_tensor(out=wrl, in0=wrf, in1=wrh, op=ALU.subtract), reads=[B_wr], writes=[B_wr])

            x1h_of = {}

            def p3b_A(t, k):
                xt, xtb = xt_ring.next()
                S.dma("sync", lambda h: h.dma_start(out=xt, in_=D["x"][t * 128:(t + 1) * 128, :]), writes=[xtb])
                psm = pm2_ring.views[k % 2]
                pmb = pm2_bufs[k % 2]
                r, rb = r_ring.next()
                for half in range(2):
                    for c in range(8):
                        S.op("tensor", lambda h, c=c, half=half: mm_(h, psm[:, half * 512:(half + 1) * 512], lhsT=mergedT[:, c, t * 128:(t + 1) * 128],
                                                                     rhs=wout[:, c, half * 512:(half + 1) * 512], start=(c == 0), stop=(c == 7)),
                             reads=[B_mT, B_wout], writes=[pmb[half]])
                    S.op("vector", lambda h, half=half: h.scalar_tensor_tensor(
                        out=r[:, half * 512:(half + 1) * 512], in0=xt[:, half * 512:(half + 1) * 512], scalar=ALPHA,
                        in1=psm[:, half * 512:(half + 1) * 512], op0=ALU.mult, op1=ALU.add), reads=[xtb, pmb[half]], writes=[rb])
                    yield
                x1, x1b = x1_ring.next()
                st_, stb = st_ring.next()
                S.op("vector", lambda h: h.memset(st_[:, 0:2], 0.0), writes=[stb])
                S.op("scalar", lambda h: h.activation(out=x1, in_=r, func=AF.Identity, accum_out=st_[:, 0:1]), reads=[rb, stb], writes=[x1b, stb])
                yield
                S.op("scalar", lambda h: h.activation(out=x1, in_=r, func=AF.Square, accum_out=st_[:, 1:2]), reads=[rb, stb], writes=[x1b, stb])
                yield
                S.op("vector", lambda h: h.tensor_scalar(out=st_[:, 2:3], in0=st_[:, 0:1], scalar1=1.0 / DM, scalar2=None, op0=ALU.mult), reads=[stb], writes=[stb])
                S.op("vector", lambda h: h.tensor_tensor(out=st_[:, 3:4], in0=st_[:, 2:3], in1=st_[:, 2:3], op=ALU.mult), reads=[stb], writes=[stb])
                yield
                S.op("vector", lambda h: h.scalar_tensor_tensor(out=st_[:, 4:5], in0=st_[:, 1:2], scalar=1.0 / DM, in1=st_[:, 3:4], op0=ALU.mult, op1=ALU.subtract),
                     reads=[stb], writes=[stb])
                S.op("scalar", lambda h: h.activation(out=st_[:, 5:6], in_=st_[:, 4:5], func=AF.Sqrt, bias=LN_EPS), reads=[stb], writes=[stb])
                yield
                S.op("vector", lambda h: h.reciprocal(out=st_[:, 6:7], in_=st_[:, 5:6]), reads=[stb], writes=[stb])
                S.op("vector", lambda h: h.scalar_tensor_tensor(out=st_[:, 7:8], in0=st_[:, 2:3], scalar=-1.0, in1=st_[:, 6:7], op0=ALU.mult, op1=ALU.mult),
                     reads=[stb], writes=[stb])
                yield
                S.op("scalar", lambda h: h.activation(out=x1, in_=r, func=AF.Identity, scale=st_[:, 6:7], bias=st_[:, 7:8]), reads=[rb, stb], writes=[x1b])
                yield
                S.op("vector", lambda h: h.tensor_tensor(out=x1, in0=x1, in1=ln1gb[:, 0:DM], op=ALU.mult), reads=[x1b, B_ln1], writes=[x1b])
                yield
                S.op("vector", lambda h: h.tensor_tensor(out=x1, in0=x1, in1=ln1gb[:, DM:2 * DM], op=ALU.add), reads=[x1b, B_ln1], writes=[x1b])
                yield
                S.op("scalar", lambda h: h.mul(out=acc[:, t, :], in_=x1, mul=ALPHA), reads=[x1b], writes=[B_acct[t]])
                x1h, x1hb = x1h_ring.next()
                x1l, x1lb = x1l_ring.next()
                S.op("scalar", lambda h: h.copy(out=x1h, in_=x1), reads=[x1b], writes=[x1hb])
                yield
                S.op("vector", lambda h: h.tensor_tensor(out=x1l, in0=x1, in1=x1h, op=ALU.subtract), reads=[x1b, x1hb], writes=[x1lb])
                x1h_of[t] = (x1h, x1hb, x1l, x1lb)
                yield

            def p3b_B(t):
                x1h, x1hb, x1l, x1lb = x1h_of[t]
                for c in range(8):
                    S.op("tensor", lambda h, c=c: mm_(h, pth[:, c * 128:(c + 1) * 128], lhsT=x1h[:, c * 128:(c + 1) * 128], rhs=identb, start=True, stop=True),
                         reads=[x1hb, B_cb], writes=[pth_b[c // 4]])
                for hb in range(2):
                    S.op("scalar", lambda h, hb=hb: h.copy(out=x1T[:, hb * 4:(hb + 1) * 4, t * 128:(t + 1) * 128], in_=v3(pth[:, hb * 512:(hb + 1) * 512], 4)),
                         reads=[pth_b[hb]], writes=[B_x1T])
                yield
                for c in range(8):
                    S.op("tensor", lambda h, c=c: mm_(h, ptl[:, c * 128:(c + 1) * 128], lhsT=x1l[:, c * 128:(c + 1) * 128], rhs=identb, start=True, stop=True),
                         reads=[x1lb, B_cb], writes=[ptl_b[c // 4]])
                x1Tl, x1Tlb = x1Tl_ring.next()
                for hb in range(2):
                    S.op("vector", lambda h, hb=hb: h.tensor_copy(out=x1Tl[:, hb * 4:(hb + 1) * 4, :], in_=v3(ptl[:, hb * 512:(hb + 1) * 512], 4)),
                         reads=[ptl_b[hb]], writes=[x1Tlb])
                yield
                psr = ptl[:, 512:548]
                n_ = 0
                for c in range(8):
                    for (lh, rh, lb_) in ((x1T[:, c, t * 128:(t + 1) * 128], wrh[:, c, :], B_x1T), (x1T[:, c, t * 128:(t + 1) * 128], wrl[:, c, :], B_x1T),
                                          (x1Tl[:, c, :], wrh[:, c, :], x1Tlb)):
                        S.op("tensor", lambda h, lh=lh, rh=rh, n_=n_: mm_(h, psr, lhsT=lh, rhs=rh, start=(n_ == 0), stop=(n_ == 23)),
                             reads=[lb_, B_wr], writes=[ptl_b[1]])
                        n_ += 1
                yield
                sm, smb = sm_ring.next()
                R_ = [smb]
                lg = sm[:, 0:36]
                S.op("vector", lambda h: h.tensor_tensor(out=lg, in0=psr, in1=brb, op=ALU.add), reads=[ptl_b[1], B_wr], writes=R_)
                m_, negm, eg, sgs, gp, og = sm[:, 36:37], sm[:, 37:38], sm[:, 38:42], sm[:, 42:43], sm[:, 43:44], sm[:, 44:48]
                tmp32, sel, m1, mask1 = sm[:, 48:80], sm[:, 80:88], sm[:, 88:89], sm[:, 89:97]
                sel2, m2, mask2, dif, e21, den, p1, p2, c8 = (sm[:, 97:105], sm[:, 105:106], sm[:, 106:114], sm[:, 114:115], sm[:, 115:116],
                                                               sm[:, 116:117], sm[:, 117:118], sm[:, 118:119], sm[:, 119:127])
                V = lambda fn: S.op("vector", fn, reads=R_, writes=R_)
                A_ = lambda fn: S.op("scalar", fn, reads=R_, writes=R_)
                V(lambda h: h.tensor_reduce(out=m_, in_=lg[:, 0:4], axis=AX.X, op=ALU.max))
                V(lambda h: h.tensor_scalar(out=negm, in0=m_, scalar1=-1.0, scalar2=None, op0=ALU.mult))
                V(lambda h: h.memset(sgs, 0.0))
                yield
                A_(lambda h: h.activation(out=eg, in_=lg[:, 0:4], func=AF.Exp, bias=negm, accum_out=sgs))
                yield
                V(lambda h: h.reciprocal(out=gp, in_=sgs))
                V(lambda h: h.tensor_scalar(out=og, in0=lg[:, 0:4], scalar1=m_, scalar2=None, op0=ALU.is_equal))
                yield
                V(lambda h: h.tensor_tensor(out=v3(tmp32, 4), in0=v3(lg[:, 4:36], 4), in1=og.unsqueeze(2).broadcast_to([128, 4, 8]), op=ALU.mult))
                V(lambda h: h.tensor_reduce(out=sel, in_=tmp32.rearrange("p (g e) -> p e g", g=4), axis=AX.X, op=ALU.add))
                yield
                V(lambda h: h.tensor_reduce(out=m1, in_=sel, axis=AX.X, op=ALU.max))
                V(lambda h: h.tensor_scalar(out=mask1, in0=sel, scalar1=m1, scalar2=None, op0=ALU.is_equal))
                yield
                V(lambda h: h.scalar_tensor_tensor(out=sel2, in0=mask1, scalar=-1e30, in1=sel, op0=ALU.mult, op1=ALU.add))
                V(lambda h: h.tensor_reduce(out=m2, in_=sel2, axis=AX.X, op=ALU.max))
                yield
                V(lambda h: h.tensor_scalar(out=mask2, in0=sel2, scalar1=m2, scalar2=None, op0=ALU.is_equal))
                V(lambda h: h.tensor_tensor(out=dif, in0=m2, in1=m1, op=ALU.subtract))
                yield
                A_(lambda h: h.activation(out=e21, in_=dif, func=AF.Exp))
                yield
                V(lambda h: h.tensor_scalar(out=den, in0=e21, scalar1=1.0, scalar2=None, op0=ALU.add))
                V(lambda h: h.reciprocal(out=p1, in_=den))
                yield
                V(lambda h: h.tensor_tensor(out=p2, in0=e21, in1=p1, op=ALU.mult))
                V(lambda h: h.tensor_tensor(out=p1, in0=p1, in1=gp, op=ALU.mult))
                yield
                V(lambda h: h.tensor_tensor(out=p2, in0=p2, in1=gp, op=ALU.mult))
                V(lambda h: h.tensor_scalar(out=c8, in0=mask1, scalar1=p1, scalar2=None, op0=ALU.mult))
                yield
                V(lambda h: h.scalar_tensor_tensor(out=c8, in0=mask2, scalar=p2, in1=c8, op0=ALU.mult, op1=ALU.add))
                V(lambda h: h.tensor_copy(out=v3(tmp32, 4), in_=c8.unsqueeze(1).broadcast_to([128, 4, 8])))
                yield
                S.op("vector", lambda h: h.tensor_tensor(out=v3(comb[:, t, :], 4), in0=v3(tmp32, 4), in1=og.unsqueeze(2).broadcast_to([128, 4, 8]), op=ALU.mult),
                     reads=R_, writes=[B_comb])
                yield

            def drive2(gens):
                live = list(gens)
                while live:
                    for g_ in list(live):
                        try:
                            next(g_)
                        except StopIteration:
                            live.remove(g_)

            for t in range(17):
                gens = []
                if t < 16:
                    gens.append(p3b_A(t, t))
                if t >= 1:
                    gens.append(p3b_B(t - 1))
                drive2(gens)
            if "dbg_x1" in D:
                S.dma("sync", lambda h: h.dma_start(out=D["dbg_x1"], in_=acc), reads=B_acct, writes=[Buf("dbg4")])
            if "dbg_comb" in D:
                S.dma("sync", lambda h: h.dma_start(out=D["dbg_comb"], in_=comb), reads=[B_comb], writes=[Buf("dbg5")])
            S.barrier()

        if stage >= 7:
            RE = Region([(108, 207)])
            NSLOT = 4
            wgu_v = [tile(RE, [8, 512], BF16) for _ in range(NSLOT)]
            wdn_v = [tile(RE, [2, DM], BF16) for _ in range(NSLOT)]
            wexp_b = [Buf(f"wexp{i}") for i in range(NSLOT)]
            stg_ring = Ring("stg", [tile(RE, [2048]) for _ in range(2)])
            ytmp_ring = Ring("ytmp", [tile(RE, [DM]) for _ in range(3)])
            sgm_ring = Ring("sgm", [tile(RE, [256]) for _ in range(3)])
            hid_ring = Ring("hid", [tile(RE, [256], BF16) for _ in range(3)])
            hidT_ring = Ring("hidT", [tile(RE, [2, 128], BF16) for _ in range(3)])
            ln2gb = tile(RE, [2 * DM]); B_ln2 = Buf("ln2gb")
            outst_ring = Ring("outst", [tile(RE, [DM]) for _ in range(2)])
            st2_ring = Ring("st2", [tile(RE, [8]) for _ in range(2)])
            ph_ring = bank_ring("ph", [0, 1, 2])
            pT2_ring = Ring("pT2", [bank(3, 256, 0), bank(3, 256, 256)])
            pT2_ring.bufs = [BANKB[3], BANKB[3]]
            y_views = [psum[:, 2048:3072], psum[:, 3072:4096]]
            y_bufs = [[BANKB[4], BANKB[5]], [BANKB[6], BANKB[7]]]
            S.dma("sync", lambda h: h.dma_start(out=ln2gb, in_=D["ln2"].partition_broadcast(128)), writes=[B_ln2])

            def load_expert(e, slot):
                wb_ = wexp_b[slot]
                if os.environ.get("MOE_NODMA", "") == "1" and e >= 4:
                    return
                for half in range(2):
                    sv, sb_ = stg_ring.next()
                    S.dma("sync", lambda h, sv=sv, half=half: h.dma_start(
                        out=v3(sv, 8), in_=D["w_gu"][e, :, half * 256:(half + 1) * 256].rearrange("(c p) n -> p c n", p=128)), writes=[sb_])
                    S.op("gpsimd", lambda h, sv=sv, half=half: h.tensor_copy(out=wgu_v[slot][:, :, half * 256:(half + 1) * 256], in_=v3(sv, 8)),
                         reads=[sb_], writes=[wb_])
                sv, sb_ = stg_ring.next()
                S.dma("sync", lambda h, sv=sv: h.dma_start(out=v3(sv, 2), in_=D["w_dn"][e].rearrange("(c p) n -> p c n", p=128)), writes=[sb_])
                S.op("gpsimd", lambda h, sv=sv: h.tensor_copy(out=wdn_v[slot], in_=v3(sv, 2)), reads=[sb_], writes=[wb_])

            def hgu(t, e, slot):
                ps, pb = ph_ring.next()
                for c in range(8):
                    S.op("tensor", lambda h, c=c: mm_(h, ps, lhsT=x1T[:, c, t * 128:(t + 1) * 128], rhs=wgu_v[slot][:, c, :],
                                                      start=(c == 0), stop=(c == 7)), reads=[B_x1T, wexp_b[slot]], writes=[pb])
                return ps, pb

            def stage_b(t, e, ps, pb):
                sg, sgb = sgm_ring.next()
                S.op("scalar", lambda h: h.activation(out=sg, in_=ps[:, 0:256], func=AF.Silu), reads=[pb], writes=[sgb])
                hid, hidb = hid_ring.next()
                S.op("vector", lambda h: h.scalar_tensor_tensor(out=hid, in0=ps[:, 256:512], scalar=comb[:, t, e:e + 1], in1=sg,
                                                                op0=ALU.mult, op1=ALU.mult), reads=[pb, B_comb, sgb], writes=[hidb])
                pT, pTb = pT2_ring.next()
                for f in range(2):
                    S.op("tensor", lambda h, f=f: mm_(h, pT[:, f * 128:(f + 1) * 128], lhsT=hid[:, f * 128:(f + 1) * 128], rhs=identb, start=True, stop=True),
                         reads=[hidb, B_cb], writes=[pTb])
                hT, hTb = hidT_ring.next()
                S.op("scalar", lambda h: h.copy(out=hT, in_=v3(pT[:, 0:256], 2)), reads=[pTb], writes=[hTb])
                return hT, hTb

            def stage_c(slot, hT, hTb, yv, yb, first, last):
                for half in range(2):
                    for f in range(2):
                        S.op("tensor", lambda h, f=f, half=half: mm_(h, yv[:, half * 512:(half + 1) * 512], lhsT=hT[:, f, :],
                                                                     rhs=wdn_v[slot][:, f, half * 512:(half + 1) * 512],
                                                                     start=(first and f == 0), stop=(last and f == 1)),
                             reads=[hTb, wexp_b[slot]], writes=[yb[half]])

            def flush_copy(t, yv, yb):
                yt, ytb = ytmp_ring.next()
                S.op("scalar", lambda h: h.copy(out=yt[:, 0:512], in_=yv[:, 0:512]), reads=[yb[0]], writes=[ytb])
                S.op("vector", lambda h: h.tensor_copy(out=yt[:, 512:1024], in_=yv[:, 512:1024]), reads=[yb[1]], writes=[ytb])
                return t, yt, ytb

            def flush_add(t, yt, ytb):
                S.op("vector", lambda h: h.tensor_tensor(out=acc[:, t, :], in0=acc[:, t, :], in1=yt, op=ALU.add), reads=[ytb, B_acct[t]], writes=[B_acct[t]])

            def final_tile(t):
                ot_, otb_ = outst_ring.next()
                st_, stb = st2_ring.next()
                layer_norm_tile(acc[:, t, :], ln2gb, ot_, st_, ot_, [B_acct[t]], B_ln2, otb_, stb, otb_)
                S.dma("sync", lambda h: h.dma_start(out=D["out"][t * 128:(t + 1) * 128, :], in_=ot_), reads=[otb_], writes=[Buf(f"outd{t}")], sem_buf=otb_)

            NE = int(os.environ.get("N_EXPERTS", "32"))
            load_expert(0, 0)
            load_expert(1, 1)
            items = [(grp, t, j) for grp in range(NE // 2) for t in range(16) for j in range(2)]
            n_it = len(items)
            Hs, Ts = {}, {}
            yk = 0
            pending = []
            pending2 = []
            fin_cnt = [0] * 16
            for step in range(n_it + 4):
                if step < n_it:
                    grp, t_, j_ = items[step]
                    if t_ == 2 and j_ == 0 and grp + 1 < NE // 2:
                        load_expert(2 * grp + 2, (2 * grp + 2) % NSLOT)
                        load_expert(2 * grp + 3, (2 * grp + 3) % NSLOT)
                    Hs[step] = hgu(t_, 2 * grp + j_, (2 * grp + j_) % NSLOT)
                i1_ = step - 1
                if 0 <= i1_ < n_it:
                    grp, t_, j_ = items[i1_]
                    Ts[i1_] = stage_b(t_, 2 * grp + j_, Hs[i1_][0], Hs[i1_][1])
                    del Hs[i1_]
                for args in pending2:
                    flush_add(*args)
                    fin_cnt[args[0]] += 1
                    if fin_cnt[args[0]] == NE // 2:
                        final_tile(args[0])
                pending2 = [flush_copy(*args) for args in pending]
                pending = []
                i2_ = step - 2
                if 0 <= i2_ < n_it:
                    grp, t_, j_ = items[i2_]
                    yv, yb = y_views[yk % 2], y_bufs[yk % 2]
                    stage_c((2 * grp + j_) % NSLOT, Ts[i2_][0], Ts[i2_][1], yv, yb, j_ == 0, j_ == 1)
                    del Ts[i2_]
                    if j_ == 1:
                        pending.append((t_, yv, yb))
                        yk += 1

            S.barrier()

        build_program.last_sched = S
        S.emit()
    return nc


def make_in_maps(inputs):
    f = lambda a: np.ascontiguousarray(np.asarray(a, dtype=np.float32))
    x = f(inputs["x"])
    w_in = f(inputs["w_in"])[0]
    shared = {
        "w_in": w_in,
        "nab": na_bias_tiles(f(inputs["na_rpb"])[0]),
        "consts": np.ascontiguousarray(CONST_ARR),
        "convT": np.ascontiguousarray(f(inputs["dn_conv_w"])[0].T.reshape(12, 128, 5).transpose(1, 0, 2).reshape(128, 60)),
        "gvec": np.concatenate([f(inputs["dn_a_log_f"])[0], f(inputs["dn_a_log_b"])[0],
                                f(inputs["dn_dt_bias_f"])[0], f(inputs["dn_dt_bias_b"])[0]])[None, :].copy(),
        "norm_w": f(inputs["dn_norm_w"]).reshape(1, 64),
        "w_proj_na": f(inputs["w_proj_na"])[0], "w_proj_dn": f(inputs["w_proj_dn"])[0], "w_out": f(inputs["w_out"])[0],
        "ln1": np.concatenate([f(inputs["ln1_g"])[0], f(inputs["ln1_b"])[0]])[None, :].copy(),
        "ln2": np.concatenate([f(inputs["ln2_g"])[0], f(inputs["ln2_b"])[0]])[None, :].copy(),
        "w_r": np.ascontiguousarray(np.concatenate([f(inputs["w_router_group"])[0], f(inputs["w_router_expert"])[0]], axis=1)),
        "b_r": np.concatenate([f(inputs["b_router_group"])[0], f(inputs["b_router_expert"])[0]])[None, :].copy(),
        "w_gu": f(inputs["w_expert_gate_up"])[0], "w_dn": f(inputs["w_expert_down"])[0],
    }
    maps = []
    for b in range(8):
        m = dict(shared)
        m["x"] = np.ascontiguousarray(x[b])
        m["xT"] = np.ascontiguousarray(x[b].T)
        maps.append(m)
    return maps


def kernel(**inputs):
    nc = build_program()
    in_maps = make_in_maps(inputs)
    res = run_bass_kernel_spmd(nc, in_maps, core_ids=list(range(8)))
    return np.stack([np.asarray(r["out"], dtype=np.float32) for r in res.results], axis=0)
```
